# Optimizing a Trainium2 kernel written in Bass

```python
import math
import jax
import jax.numpy as jnp
from jax import lax
import numpy as np

D_MODEL = 1024
BATCH = 8
SEQ = 4096
DEPTH = 2

CTX_LEN = 256
GRID_W = 64
EPS = 1e-6

ATT_HEADS = 8
ATT_KV_HEADS = 2
ATT_GROUP = ATT_HEADS // ATT_KV_HEADS
HEAD_DIM = 128
ROPE_PAIRS_PER_AXIS = HEAD_DIM // 4
ROPE_THETA = 10000.0
Q_BLOCK = 128

GLA_HEADS = 4
GLA_DK = 64
GLA_DV = 128
GLA_RANK = 16
GLA_TAU = 16.0
GLA_CHUNK = 64

HY_WIDTH = 512
HY_ORDER = 2
HY_BANDS = 16
HY_EMB = 1 + 2 * HY_BANDS
HY_FFN = 64
HY_DECAY_SLOW = -math.log(1e-2) / 1.5
HY_DECAY_FAST = -math.log(1e-2) / 0.3

N_EXPERTS = 16
N_GROUPS = 4
EXPERTS_PER_GROUP = N_EXPERTS // N_GROUPS
TOP_K = 2
D_EXPERT = 512

N_BRANCH = 3
ATT_Q_W = ATT_HEADS * HEAD_DIM
ATT_KV_W = ATT_KV_HEADS * HEAD_DIM
GLA_K_W = GLA_HEADS * GLA_DK
GLA_V_W = GLA_HEADS * GLA_DV
KV_SPLITS = (ATT_KV_W, ATT_KV_W, GLA_K_W, GLA_V_W, 2 * GLA_RANK)
REST_SPLITS = (ATT_Q_W, GLA_K_W, GLA_V_W, 3 * HY_WIDTH, N_BRANCH * D_MODEL)
N_KV_COLS = sum(KV_SPLITS)
N_IN = N_KV_COLS + sum(REST_SPLITS)

kernel_name = 'hybrid_gated_hyena_gla_gqa_moe_dit'


def rms_norm(x, g):
    xf = x.astype(jnp.float32)
    y = xf * lax.rsqrt(jnp.mean(xf * xf, axis=-1, keepdims=True) + EPS)
    return (y * g.astype(jnp.float32)).astype(x.dtype)


def split_cols(t, sizes):
    cuts = [int(v) for v in np.cumsum(sizes)[:-1]]
    return jnp.split(t, cuts, axis=-1)


def modulate(h, shift, scale):
    return h * (1 + scale[..., None, :]) + shift[..., None, :]


def to_heads(t, n_heads):
    b, n, w = t.shape
    return t.reshape(b, n, n_heads, w // n_heads).transpose(0, 2, 1, 3)


def q_heads(t):
    b, n, _ = t.shape
    return t.reshape(b, n, ATT_KV_HEADS, ATT_GROUP, HEAD_DIM).transpose(0, 2, 3, 1, 4)


def tflip(t):
    return jnp.flip(t, axis=2)


def axial_rope_tables(n_tokens):
    rows = n_tokens // GRID_W
    row = jnp.broadcast_to(jnp.arange(rows)[:, None], (rows, GRID_W)).reshape(-1).astype(jnp.float32)
    col = jnp.broadcast_to(jnp.arange(GRID_W)[None, :], (rows, GRID_W)).reshape(-1).astype(jnp.float32)
    inv_freq = ROPE_THETA ** (-jnp.arange(ROPE_PAIRS_PER_AXIS, dtype=jnp.float32) / ROPE_PAIRS_PER_AXIS)
    ang = jnp.concatenate([row[:, None] * inv_freq, col[:, None] * inv_freq], axis=-1)
    return jnp.cos(ang), jnp.sin(ang)


def apply_rope(x, cos, sin):
    xf = x.astype(jnp.float32)
    x1, x2 = xf[..., 0::2], xf[..., 1::2]
    out = jnp.stack([x1 * cos - x2 * sin, x1 * sin + x2 * cos], axis=-1)
    return out.reshape(x.shape).astype(x.dtype)


def gqa_latent(q, k, v):
    b, hkv, g, s, hd = q.shape
    nb = s // Q_BLOCK
    qb = jnp.moveaxis(q.reshape(b, hkv, g, nb, Q_BLOCK, hd), 3, 0)
    scale = HEAD_DIM ** -0.5

    def one_block(qblk):
        sc = jnp.einsum('bkgqd,bknd->bkgqn', qblk, k).astype(jnp.float32) * scale
        p = jax.nn.softmax(sc, axis=-1).astype(v.dtype)
        return jnp.einsum('bkgqn,bknd->bkgqd', p, v)

    o = lax.map(one_block, qb)
    return o.transpose(1, 0, 4, 2, 3, 5).reshape(b, s, hkv * g * hd)


def gqa_context(q, k, v):
    b, hkv, g, n, hd = q.shape
    sc = jnp.einsum('bkgqd,bknd->bkgqn', q, k).astype(jnp.float32) * HEAD_DIM ** -0.5
    p = jax.nn.softmax(sc, axis=-1).astype(v.dtype)
    o = jnp.einsum('bkgqn,bknd->bkgqd', p, v)
    return o.transpose(0, 3, 1, 2, 4).reshape(b, n, hkv * g * hd)


def gla_q(t):
    return to_heads(t, GLA_HEADS).astype(jnp.float32) * GLA_DK ** -0.5


def gla_log_decay(a_low, w_a2, b_a):
    z = (a_low @ w_a2 + b_a).astype(jnp.float32)
    return to_heads(jax.nn.log_sigmoid(z) / GLA_TAU, GLA_HEADS)


def gla_chunked(q, k, v, log_a, s0):
    b, h, n_tok, dk = q.shape
    dv = v.shape[-1]
    n = n_tok // GLA_CHUNK

    def rs(t):
        return t.reshape(b, h, n, GLA_CHUNK, t.shape[-1])

    q, k, v, log_a = rs(q), rs(k), rs(v), rs(log_a)
    cum = jnp.cumsum(log_a, axis=3)
    cum_last = cum[:, :, :, -1:, :]
    q_dec = q * jnp.exp(cum)
    k_inv = k * jnp.exp(-cum)
    k_to_end = k * jnp.exp(cum_last - cum)
    mask = jnp.tril(jnp.ones((GLA_CHUNK, GLA_CHUNK), dtype=bool))
    att = jnp.where(mask, jnp.einsum('bhnqd,bhnsd->bhnqs', q_dec, k_inv), 0.0)
    o_intra = jnp.einsum('bhnqs,bhnsv->bhnqv', att, v)
    chunk_state = jnp.einsum('bhnsd,bhnsv->bhndv', k_to_end, v)
    chunk_decay = jnp.exp(cum_last[:, :, :, 0, :])

    def step(state, xs):
        dec, cs = xs
        return dec[..., None] * state + cs, state

    s_final, s_prev = lax.scan(step, s0, (jnp.moveaxis(chunk_decay, 2, 0), jnp.moveaxis(chunk_state, 2, 0)))
    s_prev = jnp.moveaxis(s_prev, 0, 2)
    o_inter = jnp.einsum('bhnqd,bhndv->bhnqv', q_dec, s_prev)
    return (o_intra + o_inter).reshape(b, h, n_tok, dv), s_final


def gla_final_state(k, v, log_a):
    cum = jnp.cumsum(log_a, axis=2)
    w = jnp.exp(cum[:, :, -1:, :] - cum)
    return jnp.einsum('bhld,bhlv->bhdv', k * w, v)


def gla_bidir(q, k, v, la_f, la_b, s0_f, s0_b):
    o_f, s_f = gla_chunked(q, k, v, la_f, s0_f)
    o_b, s_b = gla_chunked(tflip(q), tflip(k), tflip(v), tflip(la_b), s0_b)
    return o_f + tflip(o_b), s_f, s_b


def gla_output(o, og, norm_g):
    b, h, n, dv = o.shape
    on = rms_norm(o, norm_g).transpose(0, 2, 1, 3).reshape(b, n, h * dv)
    return (on * jax.nn.silu(og.astype(jnp.float32))).astype(og.dtype)


def hyena_filters(n, P):
    f32 = jnp.float32
    t = jnp.arange(n, dtype=f32)
    t_norm = t / n
    bands = jnp.linspace(1e-4, HY_BANDS - 1, HY_BANDS, dtype=f32)
    phase = (2 * math.pi / n) * t[:, None] * bands[None, :]
    z = jnp.concatenate([t_norm[:, None], jnp.cos(phase), -jnp.sin(phase)], axis=-1)
    freq = P['hy_sin_freq'].astype(f32)
    hid = jnp.sin(freq * (z @ P['hy_pos_w1'].astype(f32) + P['hy_pos_b1'].astype(f32)))
    hid = jnp.sin(freq * (hid @ P['hy_pos_w2'].astype(f32) + P['hy_pos_b2'].astype(f32)))
    filt = (hid @ P['hy_pos_w3'].astype(f32)).reshape(n, HY_ORDER, 2, HY_WIDTH)
    filt = filt * jnp.exp(-t_norm[:, None, None, None] * jnp.abs(P['hy_decay'].astype(f32)))
    fwd, bwd = filt[:, :, 0], filt[:, :, 1]
    taps = jnp.concatenate([fwd, jnp.zeros_like(fwd[:1]), jnp.flip(bwd[1:], axis=0)], axis=0)
    taps = taps / (jnp.sum(jnp.abs(taps), axis=0, keepdims=True) + EPS)
    return jnp.fft.rfft(taps, axis=0)


def short_conv(u, w, b):
    up = jnp.pad(u, ((0, 0), (1, 1), (0, 0)))
    return up[:, :-2] * w[0] + up[:, 1:-1] * w[1] + up[:, 2:] * w[2] + b


def hyena_mix(u, P):
    n = u.shape[1]
    filt = hyena_filters(n, P)
    uc = short_conv(u, P['hy_conv_w'], P['hy_conv_b']).astype(jnp.float32)
    x1, x2, z = jnp.split(uc, 3, axis=-1)
    skip = P['hy_skip'].astype(jnp.float32)
    for o, gate in enumerate((x1, x2)):
        zf = jnp.fft.rfft(z, n=2 * n, axis=1)
        conv = jnp.fft.irfft(zf * filt[None, :, o], n=2 * n, axis=1)[:, :n]
        z = gate * (conv + skip[o] * z)
    return z.astype(u.dtype)


def merge_branches(y_hy, y_gla, y_att, br_g, P):
    g_hy, g_gla, g_att = jnp.split(br_g, N_BRANCH, axis=-1)
    m = (jax.nn.sigmoid(g_hy) * (y_hy @ P['w_br_hy'])
         + jax.nn.sigmoid(g_gla) * (y_gla @ P['w_br_gla'])
         + jax.nn.sigmoid(g_att) * (y_att @ P['w_br_att']))
    return m @ P['w_out']


def token_mixer(h, hc, P, last):
    f32 = jnp.float32
    b, s, _ = h.shape
    cos, sin = axial_rope_tables(s)
    proj = h @ P['w_in']
    a_k, a_v, g_k, g_v, g_a = split_cols(proj[..., :N_KV_COLS], KV_SPLITS)
    a_q, g_q, g_og, hy_u, br_g = split_cols(proj[..., N_KV_COLS:], REST_SPLITS)
    cproj = hc @ (P['w_in'][:, :N_KV_COLS] if last else P['w_in'])
    c_ak, c_av, c_gk, c_gv, c_ga = split_cols(cproj[..., :N_KV_COLS], KV_SPLITS)

    k_ctx = rms_norm(to_heads(c_ak, ATT_KV_HEADS), P['k_norm_g'])
    v_ctx = to_heads(c_av, ATT_KV_HEADS)
    k_lat = apply_rope(rms_norm(to_heads(a_k, ATT_KV_HEADS), P['k_norm_g']), cos, sin)
    q_lat = apply_rope(rms_norm(q_heads(a_q), P['q_norm_g']), cos, sin)
    keys = jnp.concatenate([k_lat, k_ctx], axis=2)
    vals = jnp.concatenate([to_heads(a_v, ATT_KV_HEADS), v_ctx], axis=2)
    y_att = gqa_latent(q_lat, keys, vals)

    c_k = to_heads(c_gk, GLA_HEADS).astype(f32)
    c_v = to_heads(c_gv, GLA_HEADS).astype(f32)
    c_la_f = gla_log_decay(c_ga[..., :GLA_RANK], P['gla_wa2'][0], P['gla_ba'][0])
    c_la_b = gla_log_decay(c_ga[..., GLA_RANK:], P['gla_wa2'][1], P['gla_ba'][1])
    if last:
        s_f = gla_final_state(c_k, c_v, c_la_f)
        s_b = gla_final_state(tflip(c_k), tflip(c_v), tflip(c_la_b))
    else:
        c_aq, c_gq, c_og, c_hy, c_brg = split_cols(cproj[..., N_KV_COLS:], REST_SPLITS)
        s0 = jnp.zeros((b, GLA_HEADS, GLA_DK, GLA_DV), f32)
        c_o, s_f, s_b = gla_bidir(gla_q(c_gq), c_k, c_v, c_la_f, c_la_b, s0, s0)
    la_f = gla_log_decay(g_a[..., :GLA_RANK], P['gla_wa2'][0], P['gla_ba'][0])
    la_b = gla_log_decay(g_a[..., GLA_RANK:], P['gla_wa2'][1], P['gla_ba'][1])
    o_lat, _, _ = gla_bidir(gla_q(g_q), to_heads(g_k, GLA_HEADS).astype(f32),
                            to_heads(g_v, GLA_HEADS).astype(f32), la_f, la_b, s_f, s_b)
    y_gla = gla_output(o_lat, g_og, P['gla_norm_g'])

    y_hy = hyena_mix(hy_u, P)

    out = merge_branches(y_hy, y_gla, y_att, br_g, P)
    if last:
        return out, None
    yc_att = gqa_context(rms_norm(q_heads(c_aq), P['q_norm_g']), k_ctx, v_ctx)
    yc_gla = gla_output(c_o, c_og, P['gla_norm_g'])
    yc_hy = hyena_mix(c_hy, P)
    return out, merge_branches(yc_hy, yc_gla, yc_att, c_brg, P)


def grouped_moe(h, router_w, router_b, w_gate, w_up, w_down):
    f32 = jnp.float32
    shape = h.shape
    ht = h.reshape(-1, shape[-1])
    scores = jax.nn.sigmoid((ht @ router_w).astype(f32))
    sel = scores + router_b.astype(f32)
    group_score = lax.top_k(sel.reshape(-1, N_GROUPS, EXPERTS_PER_GROUP), TOP_K)[0].sum(-1)
    group = jnp.argmax(group_score, axis=-1)
    in_group = (jnp.arange(N_EXPERTS) // EXPERTS_PER_GROUP)[None, :] == group[:, None]
    _, idx = lax.top_k(jnp.where(in_group, sel, -jnp.inf), TOP_K)
    w = jnp.take_along_axis(scores, idx, axis=-1)
    w = w / jnp.sum(w, axis=-1, keepdims=True)
    gates = jnp.sum(jax.nn.one_hot(idx, N_EXPERTS, dtype=f32) * w[..., None], axis=1).astype(h.dtype)
    out = jnp.zeros_like(ht)
    for e in range(N_EXPERTS):
        hid = jax.nn.silu(ht @ w_gate[e]) * (ht @ w_up[e])
        out = out + gates[:, e:e + 1] * (hid @ w_down[e])
    return out.reshape(shape)


def setup_inputs(seed: int = 0) -> dict:
    key = jax.random.key(seed)
    keys = iter(jax.random.split(key, 48))

    def nrm(shape, scale):
        return jax.random.normal(next(keys), shape, jnp.float32) * scale

    D = D_MODEL
    decay_base = jnp.linspace(HY_DECAY_SLOW, HY_DECAY_FAST, HY_WIDTH, dtype=jnp.float32)
    return {
        'x': nrm((BATCH, SEQ, D), 1.0),
        'c': nrm((BATCH, D), 1.0),
        'ctx': nrm((BATCH, CTX_LEN, D), 1.0),
        'c_ctx': nrm((D,), 1.0),
        'w_mod': nrm((DEPTH, D, 6 * D), 0.5 * D ** -0.5),
        'b_mod': nrm((DEPTH, 6 * D), 0.02),
        'norm1_g': 1.0 + nrm((DEPTH, D), 0.05),
        'norm2_g': 1.0 + nrm((DEPTH, D), 0.05),
        'w_in': nrm((DEPTH, D, N_IN), D ** -0.5),
        'q_norm_g': 1.0 + nrm((DEPTH, HEAD_DIM), 0.05),
        'k_norm_g': 1.0 + nrm((DEPTH, HEAD_DIM), 0.05),
        'gla_wa2': nrm((DEPTH, 2, GLA_RANK, GLA_K_W), GLA_RANK ** -0.5),
        'gla_ba': nrm((DEPTH, 2, GLA_K_W), 0.02),
        'gla_norm_g': 1.0 + nrm((DEPTH, GLA_DV), 0.05),
        'hy_conv_w': nrm((DEPTH, 3, 3 * HY_WIDTH), 3 ** -0.5),
        'hy_conv_b': nrm((DEPTH, 3 * HY_WIDTH), 0.02),
        'hy_pos_w1': nrm((DEPTH, HY_EMB, HY_FFN), HY_EMB ** -0.5),
        'hy_pos_b1': nrm((DEPTH, HY_FFN), 0.02),
        'hy_sin_freq': 1.0 + nrm((DEPTH, HY_FFN), 0.05),
        'hy_pos_w2': nrm((DEPTH, HY_FFN, HY_FFN), HY_FFN ** -0.5),
        'hy_pos_b2': nrm((DEPTH, HY_FFN), 0.02),
        'hy_pos_w3': nrm((DEPTH, HY_FFN, HY_ORDER * 2 * HY_WIDTH), HY_FFN ** -0.5),
        'hy_decay': decay_base * (1.0 + nrm((DEPTH, HY_ORDER, 2, HY_WIDTH), 0.05)),
        'hy_skip': nrm((DEPTH, HY_ORDER, HY_WIDTH), 1.0),
        'w_br_hy': nrm((DEPTH, HY_WIDTH, D), HY_WIDTH ** -0.5),
        'w_br_gla': nrm((DEPTH, GLA_V_W, D), GLA_V_W ** -0.5),
        'w_br_att': nrm((DEPTH, ATT_Q_W, D), ATT_Q_W ** -0.5),
        'w_out': nrm((DEPTH, D, D), D ** -0.5),
        'router_w': nrm((D, N_EXPERTS), D ** -0.5),
        'router_b': nrm((N_EXPERTS,), 0.01),
        'moe_w_gate': nrm((DEPTH, N_EXPERTS, D, D_EXPERT), D ** -0.5),
        'moe_w_up': nrm((DEPTH, N_EXPERTS, D, D_EXPERT), D ** -0.5),
        'moe_w_down': nrm((DEPTH, N_EXPERTS, D_EXPERT, D), D_EXPERT ** -0.5),
    }


def reference(x, c, ctx, c_ctx, w_mod, b_mod, norm1_g, norm2_g, w_in, q_norm_g, k_norm_g,
              gla_wa2, gla_ba, gla_norm_g, hy_conv_w, hy_conv_b, hy_pos_w1, hy_pos_b1,
              hy_sin_freq, hy_pos_w2, hy_pos_b2, hy_pos_w3, hy_decay, hy_skip,
              w_br_hy, w_br_gla, w_br_att, w_out, router_w, router_b,
              moe_w_gate, moe_w_up, moe_w_down):
    xc = ctx
    sc = jax.nn.silu(c)
    scc = jax.nn.silu(c_ctx)
    for l in range(DEPTH):
        last = l == DEPTH - 1
        P = {
            'w_in': w_in[l], 'q_norm_g': q_norm_g[l], 'k_norm_g': k_norm_g[l],
            'gla_wa2': gla_wa2[l], 'gla_ba': gla_ba[l], 'gla_norm_g': gla_norm_g[l],
            'hy_conv_w': hy_conv_w[l], 'hy_conv_b': hy_conv_b[l],
            'hy_pos_w1': hy_pos_w1[l], 'hy_pos_b1': hy_pos_b1[l], 'hy_sin_freq': hy_sin_freq[l],
            'hy_pos_w2': hy_pos_w2[l], 'hy_pos_b2': hy_pos_b2[l], 'hy_pos_w3': hy_pos_w3[l],
            'hy_decay': hy_decay[l], 'hy_skip': hy_skip[l],
            'w_br_hy': w_br_hy[l], 'w_br_gla': w_br_gla[l], 'w_br_att': w_br_att[l], 'w_out': w_out[l],
        }
        shift1, scale1, gate1, shift2, scale2, gate2 = jnp.split(sc @ w_mod[l] + b_mod[l], 6, axis=-1)
        n_cmod = 2 if last else 6
        cmods = jnp.split(scc @ w_mod[l][:, :n_cmod * D_MODEL] + b_mod[l][:n_cmod * D_MODEL], n_cmod, axis=-1)

        h = modulate(rms_norm(x, norm1_g[l]), shift1, scale1)
        hc = modulate(rms_norm(xc, norm1_g[l]), cmods[0], cmods[1])
        y, yc = token_mixer(h, hc, P, last)
        x = x + gate1[:, None, :] * y
        h = modulate(rms_norm(x, norm2_g[l]), shift2, scale2)
        x = x + gate2[:, None, :] * grouped_moe(h, router_w, router_b, moe_w_gate[l], moe_w_up[l], moe_w_down[l])
        if not last:
            xc = xc + cmods[2] * yc
            hc = modulate(rms_norm(xc, norm2_g[l]), cmods[3], cmods[4])
            xc = xc + cmods[5] * grouped_moe(hc, router_w, router_b, moe_w_gate[l], moe_w_up[l], moe_w_down[l])
    return x
```

```python
import contextlib
import math
import numpy as np
import ml_dtypes
import concourse.bass as bass
import concourse.mybir as mybir
from concourse.bass_utils import run_bass_kernel_spmd

F32 = mybir.dt.float32
BF16 = mybir.dt.bfloat16
I32 = mybir.dt.int32
AF = mybir.ActivationFunctionType
ALU = mybir.AluOpType
AX = mybir.AxisListType

D = 1024
SEQ = 4096
CTX = 256
DEPTH = 2
NIN = 7712
NKV = 1312
EPS = 1e-6
NEXP = 16
DE = 512
HYW = 512

SEM_LIMIT = 30000


class Buf:
    def __init__(self, name):
        self.name = name
        self.writers = {}
        self.readers = {}
        self.prev = {}


class T:
    def __init__(self, t, name, dram=False):
        self.t = t
        self.buf = Buf(name)
        self.name = name
        self.dram = dram
        self.view = None

    def __getitem__(self, idx):
        if self.dram:
            return self.t.ap()[idx]
        if self.view is not None:
            return self.view[idx]
        return self.t[idx]

    def ap(self):
        return self.t.ap() if self.dram else self.t[:]


class Eng:
    def __init__(self, P, name, handle):
        self.P = P
        self.name = name
        self.h = handle
        self.sem = None
        self.count = 0
        self.seen = {}
        self.nsem = 0

    def new_sem(self):
        self.sem = self.P.alloc_sem(f"{self.name}{self.nsem}")
        self.nsem += 1
        self.count = 0


class Prog:
    def __init__(self, nc):
        self.nc = nc
        self.stack = contextlib.ExitStack()
        self.eng = {}
        for n in ("tensor", "vector", "scalar", "gpsimd", "sync"):
            e = Eng(self, n, getattr(nc, n))
            self.eng[n] = e
        self.nsems = 0
        for e in self.eng.values():
            e.new_sem()
        self.dma_sems = []
        self.dma_rr = 0
        for i in range(24):
            self.dma_sems.append([self.alloc_sem(f"dma{i}"), 0, i])
        self.ndma_gen = 24
        self.all_tokens = {}
        self.uid = 0
        self.pending = []
        self.max_pending = 2

    def alloc_sem(self, name):
        self.nsems += 1
        return self.stack.enter_context(self.nc.semaphore(name))

    def sb(self, shape, dtype, name=None, stack=None):
        self.uid += 1
        nm = f"{name or 't'}_{self.uid}"
        t = (stack or self.stack).enter_context(self.nc.sbuf_tensor(nm, list(shape), dtype))
        return T(t, nm)

    def ps(self, shape, dtype=F32, name=None, stack=None):
        self.uid += 1
        nm = f"{name or 'p'}_{self.uid}"
        full = 512 if dtype == F32 else 1024
        t = (stack or self.stack).enter_context(self.nc.psum_tensor(nm, [128, full], dtype))
        free = 1
        for d_ in shape[1:]:
            free *= d_
        assert free <= full
        v = t[0:shape[0], 0:free]
        if len(shape) == 3:
            v = v.rearrange("p (a b) -> p a b", a=shape[1])
        r = T(t, nm)
        r.view = v
        return r

    def dram(self, name, shape, dtype, kind="Internal"):
        t = self.nc.dram_tensor(name, list(shape), dtype, kind=kind)
        return T(t, name, dram=True)

    def _need(self, E, toks):
        for key, (sem, val) in toks.items():
            if E.seen.get(key, 0) < val:
                E.h.wait_ge(sem, val)
                E.seen[key] = val

    def _deps(self, E, reads, writes, pwrites, skip_same=False):
        need = {}

        def add(d):
            for k, (s, v) in d.items():
                if skip_same and k == id(E.sem):
                    continue
                if k not in need or need[k][1] < v:
                    need[k] = (s, v)
        for b in reads:
            add(b.writers)
        for b in writes:
            add(b.writers)
            add(b.readers)
        for b in pwrites:
            add(b.readers)
            add(b.prev)
        self._need(E, need)

    def _commit(self, tok, reads, writes, pwrites):
        k = id(tok[0])
        for b in writes:
            pv = dict(b.writers)
            for kk, vv in b.readers.items():
                if kk not in pv or pv[kk][1] < vv[1]:
                    pv[kk] = vv
            b.prev = pv
            b.writers = {k: tok}
            b.readers = {}
        for b in pwrites:
            b.writers[k] = tok
        for b in reads:
            b.readers[k] = tok
        self.all_tokens[k] = tok

    def op(self, en, fn, reads=(), writes=(), pwrites=()):
        E = self.eng[en]
        reads = [getattr(r, "buf", r) for r in reads]
        writes = [getattr(r, "buf", r) for r in writes]
        pwrites = [getattr(r, "buf", r) for r in pwrites]
        if E.count >= SEM_LIMIT:
            E.new_sem()
        if self.pending and self._pending_conflict(writes + pwrites, ()):
            self.flush_stores()
        self._deps(E, reads, writes, pwrites, skip_same=(en == "tensor"))
        ins = fn(E.h)
        E.count += 1
        ins.then_inc(E.sem, 1)
        tok = (E.sem, E.count)
        E.seen[id(E.sem)] = max(E.seen.get(id(E.sem), 0), 0)
        self._commit(tok, reads, writes, pwrites)
        return ins

    def _pending_conflict(self, bufs_w, bufs_r):
        if not self.pending:
            return False
        for ent in self.pending:
            src, dst = ent[7], ent[8]
            for b in bufs_w:
                if id(b) in src or id(b) in dst:
                    return True
            for b in bufs_r:
                if id(b) in dst:
                    return True
        return False

    def flush_stores(self, keep=0):
        while len(self.pending) > keep:
            ent = self.pending.pop(0)
            self._dma_emit(*ent[:7])

    def dma(self, out, in_, reads=(), writes=(), pwrites=(), q="sync", **kw):
        is_store = any(getattr(r, "dram", False) for r in list(writes) + list(pwrites)) and not any(getattr(r, "dram", False) for r in reads)
        reads = [getattr(r, "buf", r) for r in reads]
        writes = [getattr(r, "buf", r) for r in writes]
        pwrites = [getattr(r, "buf", r) for r in pwrites]
        if is_store:
            src = {id(b) for b in reads}
            dst = {id(b) for b in writes + pwrites}
            self.pending.append((out, in_, reads, writes, pwrites, q, kw, src, dst))
            self.flush_stores(keep=self.max_pending)
            return None
        if self._pending_conflict(writes + pwrites, reads):
            self.flush_stores()
        return self._dma_emit(out, in_, reads, writes, pwrites, q, kw)

    def _dma_emit(self, out, in_, reads, writes, pwrites, q, kw):
        E = self.eng[q]
        slot = self.dma_sems[self.dma_rr]
        self.dma_rr = (self.dma_rr + 1) % len(self.dma_sems)
        if slot[1] + 16 > SEM_LIMIT:
            self._need(E, {id(slot[0]): (slot[0], slot[1])})
            slot[0] = self.alloc_sem(f"dma{self.ndma_gen}")
            self.ndma_gen += 1
            slot[1] = 0
        sem = slot[0]
        if slot[1] > 0:
            self._need(E, {id(sem): (sem, slot[1])})
        self._deps(E, reads, writes, pwrites)
        ins = E.h.dma_start(out=out, in_=in_, **kw)
        slot[1] += 16
        ins.then_inc(sem, 16)
        tok = (sem, slot[1])
        self._commit(tok, reads, writes, pwrites)
        return ins

    def barrier(self):
        self.flush_stores()
        for E in self.eng.values():
            self._need(E, dict(self.all_tokens))

    def finish(self):
        self.barrier()
        self.stack.close()


def _rr(lst, i):
    return lst[i % len(lst)]


class MK(Prog):
    def __init__(self, nc, dbg=False, layers=(0, 1), phases=None):
        super().__init__(nc)
        self.dbg = dbg
        self.layers = layers
        self.phases = phases
        self.inp = {}
        self.scr = {}

    def din(self, name, shape, dtype=F32):
        t = self.dram(name, shape, dtype, kind="ExternalInput")
        self.inp[name] = t
        return t

    def dscr(self, name, shape, dtype):
        t = self.dram(name, shape, dtype, kind="ExternalOutput" if self.dbg else "Internal")
        self.scr[name] = t
        return t

    def mm(self, ps, out, lhsT, rhs, first, last, reads):
        self.op("tensor", lambda e: e.matmul(out, lhsT, rhs, start=first, stop=last), reads=reads,
                writes=[ps] if first else [], pwrites=[] if first else [ps])

    def tr(self, ps, out, in_, ident, first, reads):
        self.op("tensor", lambda e: e.transpose(out, in_, ident), reads=reads,
                writes=[ps] if first else [], pwrites=[] if first else [ps])

    def act(self, out, in_, func, reads, writes=(), pwrites=(), **kw):
        self.op("scalar", lambda e: e.activation(out=out, in_=in_, func=func, **kw), reads=reads, writes=writes, pwrites=pwrites)

    def tt(self, out, in0, in1, op, reads, writes=(), pwrites=(), eng="vector"):
        self.op(eng, lambda e: e.tensor_tensor(out=out, in0=in0, in1=in1, op=op), reads=reads, writes=writes, pwrites=pwrites)

    def ts(self, out, in0, s1, s2, op0, op1, reads, writes=(), pwrites=(), eng="vector"):
        if op1 is None:
            self.op(eng, lambda e: e.tensor_scalar(out=out, in0=in0, scalar1=s1, scalar2=None, op0=op0), reads=reads, writes=writes, pwrites=pwrites)
        else:
            self.op(eng, lambda e: e.tensor_scalar(out=out, in0=in0, scalar1=s1, scalar2=s2, op0=op0, op1=op1), reads=reads, writes=writes, pwrites=pwrites)

    def stt(self, out, in0, scalar, in1, op0, op1, reads, writes=(), pwrites=()):
        self.op("vector", lambda e: e.scalar_tensor_tensor(out=out, in0=in0, scalar=scalar, in1=in1, op0=op0, op1=op1), reads=reads, writes=writes, pwrites=pwrites)

    def cp(self, out, in_, reads, writes=(), pwrites=(), eng="vector"):
        if eng == "scalar":
            self.op("scalar", lambda e: e.copy(out=out, in_=in_), reads=reads, writes=writes, pwrites=pwrites)
        else:
            self.op(eng, lambda e: e.tensor_copy(out=out, in_=in_), reads=reads, writes=writes, pwrites=pwrites)

    def wstage(self, st):
        self._wst = [self.sb([128, 2048], F32, "wst", st) for _ in range(2)]
        self._wsti = 0

    def wload(self, dstT, src, K, ncols):
        ksub = max(1, 2048 // ncols)
        for k0 in range(0, K, ksub):
            kn = min(ksub, K - k0)
            stg = self._wst[self._wsti % 2]
            self._wsti += 1
            sv = stg[:, 0:kn * ncols].rearrange("p (k n) -> p k n", k=kn)
            self.dma(sv, src[k0 * 128:(k0 + kn) * 128, :].rearrange("(k p) n -> p k n", p=128), reads=[], writes=[stg])
            first = k0 == 0
            self.cp(dstT[:, k0:k0 + kn, :ncols], sv, reads=[stg], writes=[dstT] if first else [], pwrites=[] if first else [dstT], eng="gpsimd")

    def rstd_from_ss(self, out, ps_ap, n, tmp_ap, reads, tmpT, outT):
        self.act(tmp_ap, ps_ap, AF.Sqrt, reads=reads, writes=[tmpT], scale=1.0 / n, bias=self.epsc[:, 0:1])
        self.op("vector", lambda e: e.reciprocal(out=out, in_=tmp_ap), reads=[tmpT], writes=[outT])

    def setup_consts(self):
        c = {}
        self.identF = self.sb([128, 128], F32, "identF")
        self.identB = self.sb([128, 128], BF16, "identB")
        self.onesF = self.sb([128, 128], F32, "onesF")
        self.onesB = self.sb([128, 128], BF16, "onesB")
        self.epsc = self.sb([128, 1], F32, "epsc")
        self.op("vector", lambda e: e.memset(self.epsc[:], EPS), writes=[self.epsc])
        self.op("gpsimd", lambda e: e.memset(self.identF[:], 1.0), writes=[self.identF])
        self.op("gpsimd", lambda e: e.affine_select(out=self.identF[:], in_=self.identF[:], pattern=[[-1, 128]],
                                                     compare_op=ALU.is_equal, fill=0.0, base=0, channel_multiplier=1),
                reads=[self.identF], writes=[self.identF])
        self.cp(self.identB[:], self.identF[:], reads=[self.identF], writes=[self.identB])
        self.op("vector", lambda e: e.memset(self.onesF[:], 1.0), writes=[self.onesF])
        self.op("vector", lambda e: e.memset(self.onesB[:], 1.0), writes=[self.onesB])

    def phase_mods(self, l):
        I = self.inp
        st = contextlib.ExitStack()
        scT = self.sb([128, 8, 2], F32, "scT", st)
        self.dma(scT[:], I["cT"].ap(), reads=[I["cT"]], writes=[scT])
        self.act(scT[:], scT[:], AF.Silu, reads=[scT], writes=[scT])
        wm = [self.sb([128, 8, 512], F32, "wm", st) for _ in range(2)]
        pm = self.ps([128, 96], F32, "pm", st)
        bm = self.sb([128, 48], F32, "bm", st)
        gg = self.sb([128, 2, 8], F32, "gg", st)
        self.dma(bm[:], I["b_modT"][l], reads=[I["b_modT"]], writes=[bm])
        self.dma(gg[:, 0, :], I["g1T"][l], reads=[I["g1T"]], pwrites=[gg])
        self.dma(gg[:, 1, :], I["g2T"][l], reads=[I["g2T"]], pwrites=[gg])
        first = True
        for ob in range(12):
            w = wm[ob % 2]
            self.dma(w[:], I["w_mod"][l, :, ob * 512:(ob + 1) * 512].rearrange("(k p) n -> p k n", p=128),
                     reads=[I["w_mod"]], writes=[w])
            for j in range(4):
                oc = ob * 4 + j
                for kc in range(8):
                    self.mm(pm, pm[:, oc * 2:oc * 2 + 2], w[:, kc, j * 128:(j + 1) * 128], scT[:, kc, :],
                            kc == 0, kc == 7, reads=[w, scT])
        modT = self.modT
        self.tt(modT[:], pm[:].rearrange("p (c t) -> p c t", t=2), bm[:].unsqueeze(2).to_broadcast([128, 48, 2]), ALU.add,
                reads=[pm, bm], writes=[modT])
        AA = self.AA
        for i, sc0 in ((0, 8), (1, 32)):
            self.ts(AA[:, i], modT[:, sc0:sc0 + 8, :], 1.0, None, ALU.add, None, reads=[modT], pwrites=[AA])
            self.tt(AA[:, i], AA[:, i], gg[:, i, :].unsqueeze(2).to_broadcast([128, 8, 2]), ALU.mult, reads=[AA, gg], pwrites=[AA])
        self.barrier()
        st.close()

    def phase_xpose_in(self, src, dstT, S):
        st = contextlib.ExitStack()
        xs = [self.sb([128, 1024], F32, "xs", st) for _ in range(2)]
        stg = [self.sb([128, 8, 128], F32, "stg", st) for _ in range(2)]
        pt = [self.ps([128, 512], F32, "pt", st) for _ in range(4)]
        for tt in range(S // 128):
            x = xs[tt % 2]
            sg = stg[tt % 2]
            self.dma(x[:], src[tt * 128:(tt + 1) * 128, :], reads=[src], writes=[x])
            for half in range(2):
                p = pt[(tt * 2 + half) % 4]
                for k in range(4):
                    kk = half * 4 + k
                    self.tr(p, p[:, k * 128:(k + 1) * 128], x[:, kk * 128:(kk + 1) * 128], self.identF[:], k == 0, reads=[x, self.identF])
                self.cp(sg[:, half * 4:(half + 1) * 4, :], p[:].rearrange("p (k n) -> p k n", k=4), reads=[p],
                        writes=[sg] if half == 0 else [], pwrites=[] if half == 0 else [sg], eng="vector" if half == 0 else "scalar")
            self.dma(dstT[:, tt * 128:(tt + 1) * 128].rearrange("(k p) t -> p k t", p=128), sg[:], reads=[sg], pwrites=[dstT])
        self.barrier()
        st.close()

    def phase_norm(self, xT, S, which, col, hT, st, lgT=None, chmax=512):
        A = self.AA
        B0 = 0 if which == 0 else 24
        ch = min(chmax, S)
        xc = [self.sb([128, 8, ch], F32, "xc", st) for _ in range(2)]
        sq = self.sb([128, 8, ch], F32, "sq", st)
        hf = [self.sb([128, 8, ch], F32, "hf", st) for _ in range(2)]
        tmp = self.sb([128, ch], F32, "ntmp", st)
        rstd = self.sb([128, ch], F32, "rstd", st)
        pss = [self.ps([128, ch], F32, "pss", st) for _ in range(2)]
        if lgT is not None:
            rw = self.sb([128, 8, 16], F32, "rw", st)
            self.dma(rw[:], self.inp["router_w"].ap().rearrange("(k p) e -> p k e", p=128), reads=[self.inp["router_w"]], writes=[rw])
            psr = [self.ps([128, (ch // 128) * 16], F32, "psr", st) for _ in range(2)]
        for c in range(S // ch):
            x = xc[c % 2]
            h = hf[c % 2]
            p = pss[c % 2]
            self.dma(x[:], xT[:, c * ch:(c + 1) * ch].rearrange("(k p) t -> p k t", p=128), reads=[xT], writes=[x])
            self.act(sq[:], x[:], AF.Square, reads=[x], writes=[sq])
            for kc in range(8):
                self.mm(p, p[:], self.onesF[:], sq[:, kc, :], kc == 0, kc == 7, reads=[sq, self.onesF])
            self.rstd_from_ss(rstd[:], p[:], 1024.0, tmp[:], [p], tmp, rstd)
            for kc in range(8):
                self.tt(h[:, kc, :], x[:, kc, :], rstd[:], ALU.mult, reads=[x, rstd], writes=[h] if kc == 0 else [], pwrites=[] if kc == 0 else [h])
                self.act(h[:, kc, :], h[:, kc, :], AF.Identity, reads=[h, A, self.modT], pwrites=[h],
                         scale=A[:, which, kc, col:col + 1], bias=self.modT[:, B0 + kc, col:col + 1])
            self.cp(hT[:, :, c * ch:(c + 1) * ch], h[:], reads=[h], pwrites=[hT], eng="gpsimd")
            if lgT is not None:
                pr = psr[c % 2]
                nj = ch // 128
                for j in range(nj):
                    for kc in range(8):
                        self.mm(pr, pr[:, j * 16:(j + 1) * 16], h[:, kc, j * 128:(j + 1) * 128], rw[:, kc, :], kc == 0, kc == 7, reads=[rw, h])
                self.cp(lgT[:, c * nj:(c + 1) * nj, :], pr[:].rearrange("p (j e) -> p j e", e=16), reads=[pr], pwrites=[lgT], eng="scalar")

    def linear_fm(self, wsrc, wrow_chunks, col0, ncols, acts, S, dst, drow0, evac_dt, st_outer=None, wtiles=None):
        st = contextlib.ExitStack()
        K = wrow_chunks
        ch = min(512, S)
        wb = [self.sb([128, K, 512], BF16, "wb", st) for _ in range(2)]
        self.wstage(st)
        stg = [self.sb([128, ch], evac_dt, "lstg", st) for _ in range(3)]
        pp = [self.ps([128, ch], F32, "lps", st) for _ in range(3)]
        n = 0
        blocks = list(range(0, ncols, 512))
        self.wload(wb[0], wsrc[:, col0:col0 + min(512, ncols)], K, min(512, ncols))
        for bi, b0 in enumerate(blocks):
            bw = min(512, ncols - b0)
            w = wb[bi % 2]
            if bi + 1 < len(blocks):
                nb0 = blocks[bi + 1]
                nbw = min(512, ncols - nb0)
                self.wload(wb[(bi + 1) % 2], wsrc[:, col0 + nb0:col0 + nb0 + nbw], K, nbw)
            for m0 in range(0, bw, 128):
                msz = min(128, bw - m0)
                for c in range(S // ch):
                    p = pp[n % 3]
                    sg = stg[n % 3]
                    for kc in range(K):
                        self.mm(p, p[:msz, :], w[:, kc, m0:m0 + msz], acts[:, kc, c * ch:(c + 1) * ch], kc == 0, kc == K - 1, reads=[w, acts])
                    self.cp(sg[:msz, :], p[:msz, :], reads=[p], writes=[sg], eng="vector" if n % 2 == 0 else "scalar")
                    r0 = drow0 + b0 + m0
                    self.dma(dst[r0:r0 + msz, c * ch:(c + 1) * ch], sg[:msz, :], reads=[sg], pwrites=[dst])
                    n += 1
        self.barrier()
        st.close()

    def linear_tm(self, wsrc, K, col0, ncols, acts, S, dst, drow0, dcol0, evac_dt):
        st = contextlib.ExitStack()
        wb = [self.sb([128, K, 512], BF16, "wbt", st) for _ in range(2)]
        self.wstage(st)
        stg = [self.sb([128, 512], evac_dt, "tstg", st) for _ in range(3)]
        pp = [self.ps([128, 512], F32, "tps", st) for _ in range(3)]
        n = 0
        blocks = list(range(0, ncols, 512))
        self.wload(wb[0], wsrc[:, col0:col0 + min(512, ncols)], K, min(512, ncols))
        for bi, b0 in enumerate(blocks):
            bw = min(512, ncols - b0)
            w = wb[bi % 2]
            if bi + 1 < len(blocks):
                nb0 = blocks[bi + 1]
                nbw = min(512, ncols - nb0)
                self.wload(wb[(bi + 1) % 2], wsrc[:, col0 + nb0:col0 + nb0 + nbw], K, nbw)
            for tt in range(S // 128):
                p = pp[n % 3]
                sg = stg[n % 3]
                for kc in range(K):
                    self.mm(p, p[:, :bw], acts[:, kc, tt * 128:(tt + 1) * 128], w[:, kc, :bw], kc == 0, kc == K - 1, reads=[w, acts])
                self.cp(sg[:, :bw], p[:, :bw], reads=[p], writes=[sg], eng="vector" if n % 2 == 0 else "scalar")
                self.dma(dst[drow0 + tt * 128:drow0 + (tt + 1) * 128, dcol0 + b0:dcol0 + b0 + bw], sg[:, :bw], reads=[sg], pwrites=[dst])
                n += 1
        self.barrier()
        st.close()

    def phase_qkprep(self, l, sm):
        I = self.inp
        S = sm.S
        ch = min(512, S)
        st = contextlib.ExitStack()
        qkg = self.sb([128, 2], F32, "qkg", st)
        self.dma(qkg[:], I["qkg"][l], reads=[I["qkg"]], writes=[qkg])
        RT = self.sb([128, 128], F32, "RT", st)
        self.dma(RT[:], I["ropeRT"].ap(), reads=[I["ropeRT"]], writes=[RT])
        xs = [self.sb([128, ch], F32, "qx", st) for _ in range(3)]
        sqs = [self.sb([128, ch], F32, "qsq", st) for _ in range(3)]
        tmps = [self.sb([128, ch], F32, "qtmp", st) for _ in range(3)]
        rstds = [self.sb([128, ch], F32, "qrstd", st) for _ in range(3)]
        nn = [self.sb([128, ch], F32, "qn", st) for _ in range(3)]
        t1s = [self.sb([128, ch], F32, "qt1", st) for _ in range(3)]
        t2s = [self.sb([128, ch], F32, "qt2", st) for _ in range(3)]
        ob = [self.sb([128, ch], BF16, "qo", st) for _ in range(3)]
        cs = [self.sb([128, 2, ch], F32, "qcs", st) for _ in range(2)]
        p1 = [self.ps([128, ch], F32, "qp1", st) for _ in range(2)]
        p2 = [self.ps([128, ch], F32, "qp2", st) for _ in range(2)]
        n = 0
        for c in range(S // ch):
            if sm.rope:
                cst = cs[c % 2]
                self.dma(cst[:, 0, :], I["ropeC"][:, c * ch:(c + 1) * ch], reads=[], writes=[cst])
                self.dma(cst[:, 1, :], I["ropeS"][:, c * ch:(c + 1) * ch], reads=[], pwrites=[cst])
            for hh in range(10):
                isq = hh < 8
                src = sm.d["qTraw"] if isq else sm.d["kTraw"]
                r0 = hh * 128 if isq else (hh - 8) * 128
                x = xs[n % 3]
                nb = nn[n % 3]
                o = ob[n % 3]
                sq, tmp, rstd, t1, t2 = sqs[n % 3], tmps[n % 3], rstds[n % 3], t1s[n % 3], t2s[n % 3]
                pa = p1[n % 2]
                pb = p2[n % 2]
                self.dma(x[:], src[r0:r0 + 128, c * ch:(c + 1) * ch], reads=[src], writes=[x])
                self.act(sq[:], x[:], AF.Square, reads=[x], writes=[sq])
                self.mm(pa, pa[:], self.onesF[:], sq[:], True, True, reads=[sq, self.onesF])
                self.rstd_from_ss(rstd[:], pa[:], 128.0, tmp[:], [pa], tmp, rstd)
                g = qkg[:, 0:1] if isq else qkg[:, 1:2]
                self.stt(nb[:], x[:], g, rstd[:], ALU.mult, ALU.mult, reads=[x, qkg, rstd], writes=[nb])
                if sm.rope:
                    self.mm(pb, pb[:], RT[:], nb[:], True, True, reads=[RT, nb])
                    self.tt(t1[:], nb[:], cst[:, 0, :], ALU.mult, reads=[nb, cst], writes=[t1], eng="gpsimd")
                    self.tt(t2[:], pb[:], cst[:, 1, :], ALU.mult, reads=[pb, cst], writes=[t2])
                    self.tt(o[:], t1[:], t2[:], ALU.add, reads=[t1, t2], writes=[o])
                else:
                    self.cp(o[:], nb[:], reads=[nb], writes=[o])
                if isq:
                    self.dma(sm.d["qrT"][r0:r0 + 128, c * ch:(c + 1) * ch], o[:], reads=[o], pwrites=[sm.d["qrT"]])
                else:
                    k0 = sm.koff + c * ch
                    self.dma(self.krT[r0:r0 + 128, k0:k0 + ch], o[:], reads=[o], pwrites=[self.krT])
                n += 1
        self.barrier()
        st.close()

    def phase_attn(self, sm):
        S = sm.S
        ch = min(512, S)
        k0, k1 = sm.keys
        nk = k1 - k0
        nkt = nk // 128
        st = contextlib.ExitStack()
        KT = self.sb([128, nk], BF16, "KT", st)
        V = self.sb([128, nkt, 128], BF16, "V", st)
        Qc = [self.sb([128, ch], BF16, "Qc", st) for _ in range(2)]
        pT = [self.sb([128, ch], BF16, "pT", st) for _ in range(3)]
        rden = self.sb([128, ch], F32, "rden", st)
        yo = [self.sb([128, ch], BF16, "yo", st) for _ in range(2)]
        ps_s = [self.ps([128, ch], F32, "ps_s", st) for _ in range(3)]
        ps_o = [self.ps([128, ch], F32, "ps_o", st) for _ in range(2)]
        ps_d = [self.ps([128, ch], F32, "ps_d", st) for _ in range(2)]
        scale = 128.0 ** -0.5
        nq = 0
        ns = 0
        for kv in range(2):
            self.dma(KT[:], self.krT[kv * 128:(kv + 1) * 128, k0:k1], reads=[self.krT], writes=[KT])
            self.dma(V[:], self.av[k0:k1, kv * 128:(kv + 1) * 128].rearrange("(t p) d -> p t d", p=128), reads=[self.av], writes=[V])
            for g in range(4):
                h = kv * 4 + g
                for c in range(S // ch):
                    q = Qc[nq % 2]
                    po = ps_o[nq % 2]
                    pd = ps_d[nq % 2]
                    y = yo[nq % 2]
                    self.dma(q[:], sm.d["qrT"][h * 128:(h + 1) * 128, c * ch:(c + 1) * ch], reads=[sm.d["qrT"]], writes=[q])
                    prev = None
                    for kt in range(nkt + 1):
                        cur_pt = None
                        if kt < nkt:
                            psx = ps_s[ns % 3]
                            cur_pt = pT[ns % 3]
                            ns += 1
                            self.mm(psx, psx[:], KT[:, kt * 128:(kt + 1) * 128], q[:], True, True, reads=[KT, q])
                            self.act(cur_pt[:], psx[:], AF.Exp, reads=[psx], writes=[cur_pt], scale=scale)
                        if prev is not None:
                            pk, ppt = prev
                            self.mm(po, po[:], V[:, pk, :], ppt[:], pk == 0, pk == nkt - 1, reads=[V, ppt])
                            self.mm(pd, pd[:], self.onesB[:], ppt[:], pk == 0, pk == nkt - 1, reads=[self.onesB, ppt])
                        prev = (kt, cur_pt) if kt < nkt else None
                    self.op("vector", lambda e: e.reciprocal(out=rden[:], in_=pd[:]), reads=[pd], writes=[rden])
                    self.tt(y[:], po[:], rden[:], ALU.mult, reads=[po, rden], writes=[y])
                    self.dma(sm.d["yattT"][h * 128:(h + 1) * 128, c * ch:(c + 1) * ch], y[:], reads=[y], pwrites=[sm.d["yattT"]])
                    nq += 1
        self.barrier()
        st.close()

    def phase_gla(self, l, sm):
        I = self.inp
        S = sm.S
        ch = min(512, S)
        nch = S // 64
        ntile = S // 128
        st = contextlib.ExitStack()
        d = sm.d
        X = self.sb([64, S], F32, "gX", st)
        L = self.sb([64, S], F32, "gL", st)
        CUM = self.sb([64, S], F32, "gCUM", st)
        E = self.sb([64, S], F32, "gE", st)
        maskR = self.sb([64, S], F32, "gmaskR", st)
        ktf = self.sb([64, S], BF16, "gktf", st)
        self.op("gpsimd", lambda e: e.memset(maskR[:], 1.0), writes=[maskR])
        self.op("gpsimd", lambda e: e.memset(maskR[:].rearrange("p (c j) -> p c j", j=64)[:, :, 0:1], 0.0), reads=[maskR], writes=[maskR])
        qd = [self.sb([64, S], BF16, "gqd", st) for _ in range(2)]
        ki = [self.sb([64, S], BF16, "gki", st) for _ in range(2)]
        kteT = [self.sb([128, ntile, 64], BF16, "gkteT", st) for _ in range(2)]
        dec = self.sb([64, nch], F32, "gdec", st)
        Sbf = [self.sb([64, nch, 128], BF16, "gSbf", st) for _ in range(2)]
        Sst = [self.sb([64, 128], F32, "gSst", st) for _ in range(2)]
        Vh = self.sb([128, ntile, 128], BF16, "gVh", st)
        wa2 = self.sb([16, 2, 256], F32, "gwa2", st)
        nba = self.sb([64, 2, 4], F32, "gnba", st)
        gg = self.sb([128, 1], F32, "ggn", st)
        mk = self.sb([128, 2, 128], F32, "gmk", st)
        gat = [self.sb([16, ch], F32, "ggat", st) for _ in range(2)]
        am = [self.sb([128, 128], BF16, "gam", st) for _ in range(4)]
        og = [self.sb([128, ch], BF16, "gog", st) for _ in range(2)]
        sg = self.sb([128, ch], F32, "gsg", st)
        osq = self.sb([128, ch], F32, "gosq", st)
        tmp = self.sb([128, ch], F32, "gtmp", st)
        rstd = self.sb([128, ch], F32, "grstd", st)
        t1 = self.sb([128, ch], F32, "gt1", st)
        yb = [self.sb([128, ch], BF16, "gyb", st) for _ in range(2)]
        ptr = self.ps([128, 512], BF16, "gptr", st)
        pcs = [self.ps([64, 4, 128], F32, "gpcs", st) for _ in range(2)]
        pa = [self.ps([128, 128], F32, "gpa", st) for _ in range(2)]
        po = self.ps([128, ch], F32, "gpo", st)
        pss = self.ps([128, ch], F32, "gpss", st)
        pz = [pss, po]
        for dd in range(2):
            self.dma(wa2[:, dd, :], I["gla_wa2"][l, dd], reads=[], pwrites=[wa2])
        self.dma(nba[:], I["gla_baT"][l], reads=[], writes=[nba])
        self.ts(nba[:], nba[:], -1.0, None, ALU.mult, None, reads=[nba], writes=[nba])
        self.dma(gg[:], I["glag"][l], reads=[], writes=[gg])
        self.dma(mk[:], I["gla_mask"].ap(), reads=[], writes=[mk])
        nz = 0
        import os
        cut = int(os.environ.get("GLA_CUT", "99"))

        def bail():
            self.barrier()
            st.close()
        for h in range(4):
            self.dma(Vh[:], d["gv"][:, h * 128:(h + 1) * 128].rearrange("(t p) v -> p t v", p=128), reads=[d["gv"]], writes=[Vh])
            for dd in range(2):
                for c in range(S // ch):
                    ga = gat[nz % 2]
                    p = pz[nz % 2]
                    nz += 1
                    self.dma(ga[:], d["gaT"][dd * 16:(dd + 1) * 16, c * ch:(c + 1) * ch], reads=[d["gaT"]], writes=[ga])
                    self.mm(p, p[0:64, :], wa2[:, dd, h * 64:(h + 1) * 64], ga[:], True, True, reads=[wa2, ga])
                    self.act(E[:, c * ch:(c + 1) * ch], p[0:64, :], AF.Exp, reads=[p, nba], pwrites=[E], scale=-1.0, bias=nba[:, dd, h:h + 1])
                    self.act(L[:, c * ch:(c + 1) * ch], E[:, c * ch:(c + 1) * ch], AF.Ln, reads=[E], pwrites=[L], bias=1.0)
                if cut == 2:
                    return bail()
                self.op("vector", lambda e: e.tensor_tensor_scan(out=CUM[:], data0=maskR[:], data1=L[:], initial=0.0, op0=ALU.mult, op1=ALU.add),
                        reads=[maskR, L], writes=[CUM])
                C3 = CUM[:].rearrange("p (c j) -> p c j", j=64)
                END = C3[:, :, 63:64]
                ENDb = END.to_broadcast([64, nch, 64])
                if dd == 0:
                    ARG = CUM
                else:
                    self.tt(L[:], L[:], CUM[:], ALU.subtract, reads=[L, CUM], writes=[L])
                    L3 = L[:].rearrange("p (c j) -> p c j", j=64)
                    self.tt(L3, L3, ENDb, ALU.add, reads=[L, CUM], writes=[L])
                    ARG = L
                A3 = ARG[:].rearrange("p (c j) -> p c j", j=64)
                self.dma(X[:], d["gqT"][h * 64:(h + 1) * 64, :], reads=[d["gqT"]], writes=[X])
                self.act(E[:], ARG[:], AF.Exp, reads=[ARG], writes=[E], scale=-1.0 / 16)
                self.stt(qd[dd][:], X[:], 0.125, E[:], ALU.mult, ALU.mult, reads=[X, E], writes=[qd[dd]])
                self.dma(X[:], d["gkT"][h * 64:(h + 1) * 64, :], reads=[d["gkT"]], writes=[X])
                self.act(E[:], ARG[:], AF.Exp, reads=[ARG], writes=[E], scale=1.0 / 16)
                self.tt(ki[dd][:], X[:], E[:], ALU.mult, reads=[X, E], writes=[ki[dd]])
                E3 = E[:].rearrange("p (c j) -> p c j", j=64)
                self.tt(E3, ENDb, A3, ALU.subtract, reads=[CUM, ARG], writes=[E])
                self.act(E[:], E[:], AF.Exp, reads=[E], writes=[E], scale=-1.0 / 16)
                self.tt(ktf[:], X[:], E[:], ALU.mult, reads=[X, E], writes=[ktf])
                if cut == 3:
                    return bail()
                for t0 in range(0, ntile, 8):
                    nt = min(8, ntile - t0)
                    for j in range(nt):
                        tt_ = t0 + j
                        self.tr(ptr, ptr[:, j * 64:(j + 1) * 64], ktf[:, tt_ * 128:(tt_ + 1) * 128], self.identB[0:64, 0:64], j == 0, reads=[ktf, self.identB])
                    self.cp(kteT[dd][:, t0:t0 + nt, :], ptr[:, 0:nt * 64].rearrange("p (t k) -> p t k", k=64), reads=[ptr],
                            writes=[kteT[dd]] if t0 == 0 else [], pwrites=[] if t0 == 0 else [kteT[dd]])
                if cut == 4:
                    return bail()
                self.act(dec[:].unsqueeze(2), END, AF.Exp, reads=[CUM], writes=[dec], scale=-1.0 / 16)
                cur = 0
                self.cp(Sst[0][:], sm.gin[:, h, dd, :], reads=[sm.gin], writes=[Sst[0]])
                order = list(range(nch)) if dd == 0 else list(range(nch - 1, -1, -1))
                for i0 in range(0, nch, 4):
                    grp = order[i0:i0 + 4]
                    gi = (i0 // 4) % 2
                    cnt = [0, 0]
                    slots = {}
                    for c in grp:
                        par = c % 2
                        slot = gi * 2 + cnt[par]
                        cnt[par] += 1
                        slots[c] = (pcs[par], slot)
                        tt_, pb = c // 2, par * 64
                        self.mm(pcs[par], pcs[par][:, slot, :], kteT[dd][pb:pb + 64, tt_, :], Vh[pb:pb + 64, tt_, :], True, True, reads=[kteT[dd], Vh])
                    for c in grp:
                        pc, slot = slots[c]
                        self.cp(Sbf[dd][:, c, :], Sst[cur][:], reads=[Sst[cur]], pwrites=[Sbf[dd]], eng="scalar")
                        self.stt(Sst[1 - cur][:], Sst[cur][:], dec[:, c:c + 1], pc[:, slot, :], ALU.mult, ALU.add,
                                 reads=[Sst[cur], dec, pc], writes=[Sst[1 - cur]])
                        cur = 1 - cur
                if sm.gout is not None:
                    self.cp(sm.gout[:, h, dd, :], Sst[cur][:], reads=[Sst[cur]], pwrites=[sm.gout])
            if cut == 5:
                return bail()
            npair = 0
            for c in range(S // ch):
                ntp = ch // 128
                for j in range(ntp):
                    tp = c * ntp + j
                    ams = []
                    for dd in range(2):
                        a = am[(npair % 2) * 2 + dd]
                        p = pa[dd]
                        self.mm(p, p[:], ki[dd][:, tp * 128:(tp + 1) * 128], qd[dd][:, tp * 128:(tp + 1) * 128], True, True, reads=[ki[dd], qd[dd]])
                        self.tt(a[:], p[:], mk[:, dd, :], ALU.mult, reads=[p, mk], writes=[a])
                        ams.append(a)
                    npair += 1
                    for cc in range(2):
                        cidx = tp * 2 + cc
                        reg = po[:, j * 128 + cc * 64:j * 128 + cc * 64 + 64]
                        for dd in range(2):
                            self.mm(po, reg, Vh[:, tp, :], ams[dd][:, cc * 64:(cc + 1) * 64], dd == 0, False, reads=[Vh, ams[dd]])
                            self.mm(po, reg, Sbf[dd][:, cidx, :], qd[dd][:, cidx * 64:(cidx + 1) * 64], False, dd == 1, reads=[Sbf[dd], qd[dd]])
                ogt = og[c % 2]
                y = yb[c % 2]
                self.dma(ogt[:], d["ogT"][h * 128:(h + 1) * 128, c * ch:(c + 1) * ch], reads=[d["ogT"]], writes=[ogt])
                self.act(sg[:], ogt[:], AF.Silu, reads=[ogt], writes=[sg])
                self.act(osq[:], po[:], AF.Square, reads=[po], writes=[osq])
                self.mm(pss, pss[:], self.onesF[:], osq[:], True, True, reads=[self.onesF, osq])
                self.rstd_from_ss(rstd[:], pss[:], 128.0, tmp[:], [pss], tmp, rstd)
                self.tt(t1[:], po[:], rstd[:], ALU.mult, reads=[po, rstd], writes=[t1])
                self.stt(y[:], t1[:], gg[:, 0:1], sg[:], ALU.mult, ALU.mult, reads=[t1, gg, sg], writes=[y])
                self.dma(d["yglaT"][h * 128:(h + 1) * 128, c * ch:(c + 1) * ch], y[:], reads=[y], pwrites=[d["yglaT"]])
        self.barrier()
        st.close()

    def phase_hyena(self, l, sm):
        I = self.inp
        S = sm.S
        n = S
        ntile = n // 128
        nkt = ntile
        d = sm.d
        sfx = "L" if n == SEQ else "C"
        TC, TS = I["dftC" + sfx], I["dftS" + sfx]
        N2 = 2 * n
        st = contextlib.ExitStack()
        ch = min(512, n)
        zemb = self.sb([33, n], F32, "hzemb", st)
        self.dma(zemb[:], I["zemb" + sfx].ap(), reads=[], writes=[zemb])
        w1 = self.sb([33, 64], F32, "hw1", st)
        w2 = self.sb([64, 64], F32, "hw2", st)
        w3 = self.sb([64, 2048], F32, "hw3", st)
        pv = self.sb([64, 4], F32, "hpv", st)
        self.dma(w1[:], I["hy_pos_w1"][l], reads=[], writes=[w1])
        self.dma(w2[:], I["hy_pos_w2"][l], reads=[], writes=[w2])
        self.dma(w3[:], I["hy_pos_w3"][l], reads=[], writes=[w3])
        self.dma(pv[:], I["hy_pv"][l], reads=[], writes=[pv])
        fb = self.sb([64, 2], F32, "hfb", st)
        self.tt(fb[:, 0:1], pv[:, 0:1], pv[:, 1:2], ALU.mult, reads=[pv], writes=[fb])
        self.tt(fb[:, 1:2], pv[:, 2:3], pv[:, 1:2], ALU.mult, reads=[pv], pwrites=[fb])
        hid = [self.sb([64, n], F32, "hhid", st) for _ in range(2)]
        u = self.sb([64, ch], F32, "hu", st)
        kf = self.sb([64, ch], F32, "hkf", st)
        kint = self.sb([64, ch], I32, "hki", st)
        php = [self.ps([64, ch], F32, "hph", st) for _ in range(2)]
        TWO_PI = 2.0 * math.pi
        for layer_i in range(2):
            wsb = w1 if layer_i == 0 else w2
            src = zemb if layer_i == 0 else hid[0]
            K = 33 if layer_i == 0 else 64
            for c in range(n // ch):
                p = php[c % 2]
                self.mm(p, p[:], wsb[0:K, :], src[0:K, c * ch:(c + 1) * ch], True, True, reads=[wsb, src])
                self.ts(u[:], p[:], pv[:, 1:2], fb[:, layer_i:layer_i + 1], ALU.mult, ALU.add, reads=[p, pv, fb], writes=[u])
                self.ts(kf[:], u[:], 1.0 / TWO_PI, None, ALU.mult, None, reads=[u], writes=[kf])
                self.cp(kint[:], kf[:], reads=[kf], writes=[kint])
                self.cp(kf[:], kint[:], reads=[kint], writes=[kf])
                self.stt(u[:], kf[:], -TWO_PI, u[:], ALU.mult, ALU.add, reads=[kf, u], writes=[u])
                self.ts(kf[:], u[:], math.pi, -TWO_PI, ALU.is_gt, ALU.mult, reads=[u], writes=[kf])
                self.tt(u[:], u[:], kf[:], ALU.add, reads=[u, kf], writes=[u])
                self.ts(kf[:], u[:], -math.pi, TWO_PI, ALU.is_lt, ALU.mult, reads=[u], writes=[kf])
                self.tt(u[:], u[:], kf[:], ALU.add, reads=[u, kf], writes=[u])
                self.ts(u[:], u[:], math.pi, -math.pi, ALU.min, ALU.max, reads=[u], writes=[u])
                self.act(hid[layer_i][:, c * ch:(c + 1) * ch], u[:], AF.Sin, reads=[u], pwrites=[hid[layer_i]])
        hid2 = hid[1]
        absd = self.sb([128, 2048], F32, "habsd", st)
        self.dma(absd[:], I["hy_decay"][l].partition_broadcast(128), reads=[], writes=[absd])
        self.act(absd[:], absd[:], AF.Abs, reads=[absd], writes=[absd])
        negtn = self.sb([128, ntile], F32, "hnegtn", st)
        self.dma(negtn[:], I["negtn" + sfx].ap(), reads=[], writes=[negtn])
        wins = [self.sb([128, 2048], F32, "hwin", st) for _ in range(2)]
        tapss = [self.sb([128, 2048], F32, "htaps", st) for _ in range(2)]
        ataps = [self.sb([128, 2048], F32, "hatap", st) for _ in range(2)]
        fs_t = [self.sb([128, 2, 512], BF16, "hfst", st) for _ in range(2)]
        fd_t = [self.sb([128, 2, 512], BF16, "hfdt", st) for _ in range(2)]
        pf = [self.ps([128, 512], F32, "hpf", st) for _ in range(2)]
        pl1 = [self.ps([128, 512], F32, "hpl1", st) for _ in range(4)]
        for tt_ in range(ntile):
            win, taps, atap = wins[tt_ % 2], tapss[tt_ % 2], ataps[tt_ % 2]
            self.act(win[:], absd[:], AF.Exp, reads=[absd, negtn], writes=[win], scale=negtn[:, tt_:tt_ + 1])
            for b in range(4):
                p = pf[b % 2]
                self.mm(p, p[:], hid2[:, tt_ * 128:(tt_ + 1) * 128], w3[:, b * 512:(b + 1) * 512], True, True, reads=[hid2, w3])
                self.tt(taps[:, b * 512:(b + 1) * 512], p[:], win[:, b * 512:(b + 1) * 512], ALU.mult, reads=[p, win],
                        writes=[taps] if b == 0 else [], pwrites=[] if b == 0 else [taps])
            if tt_ == 0:
                t4 = taps[0:1, :].rearrange("p (o d c) -> p o d c", o=2, d=2)
                self.op("vector", lambda e: e.memset(t4[:, :, 1, :], 0.0), reads=[taps], pwrites=[taps])
            self.act(atap[:], taps[:], AF.Abs, reads=[taps], writes=[atap])
            for b in range(4):
                self.mm(pl1[b], pl1[b][:], self.onesF[:], atap[:, b * 512:(b + 1) * 512], tt_ == 0, tt_ == ntile - 1, reads=[self.onesF, atap])
            T4 = taps[:].rearrange("p (o d c) -> p o d c", o=2, d=2)
            fs_ = fs_t[tt_ % 2]
            fd_ = fd_t[tt_ % 2]
            self.tt(fs_[:], T4[:, :, 0, :], T4[:, :, 1, :], ALU.add, reads=[taps], writes=[fs_])
            self.tt(fd_[:], T4[:, :, 0, :], T4[:, :, 1, :], ALU.subtract, reads=[taps], writes=[fd_], eng="gpsimd")
            self.dma(d["fsd"][tt_ * 128:(tt_ + 1) * 128, :], fs_[:].rearrange("p o c -> p (o c)"), reads=[fs_], pwrites=[d["fsd"]])
            self.dma(d["fdd"][tt_ * 128:(tt_ + 1) * 128, :], fd_[:].rearrange("p o c -> p (o c)"), reads=[fd_], pwrites=[d["fdd"]])
        linv = self.linv
        l1 = self.sb([128, 2048], F32, "hl1", st)
        for b in range(4):
            self.cp(l1[:, b * 512:(b + 1) * 512], pl1[b][:], reads=[pl1[b]], writes=[l1] if b == 0 else [], pwrites=[] if b == 0 else [l1])
        L4 = l1[:].rearrange("p (o d c) -> p o d c", o=2, d=2)
        self.tt(linv[:], L4[:, :, 0, :], L4[:, :, 1, :], ALU.add, reads=[l1], writes=[linv])
        self.ts(linv[:], linv[:], EPS, None, ALU.add, None, reads=[linv], writes=[linv])
        self.op("vector", lambda e: e.reciprocal(out=linv[:], in_=linv[:]), reads=[linv], writes=[linv])
        self.barrier()
        st.close()

        st = contextlib.ExitStack()
        wk = self.sb([128, nkt], F32, "hwk", st)
        self.dma(wk[:], I["wk" + sfx].ap(), reads=[], writes=[wk])
        alt = self.sb([128, 2], BF16, "halt", st)
        self.dma(alt[:], I["altcol"].ap(), reads=[], writes=[alt])
        altrow = self.sb([1, 128], BF16, "haltrow", st)
        self.dma(altrow[:], I["altrow"].ap(), reads=[], writes=[altrow])
        tcb = [self.sb([128, ntile, 128], BF16, "htc", st) for _ in range(2)]
        tsb = [self.sb([128, ntile, 128], BF16, "hts", st) for _ in range(2)]
        fsb = self.sb([128, ntile, 512], BF16, "hfsb", st)
        fdb = self.sb([128, ntile, 512], BF16, "hfdb", st)
        pre = [self.ps([128, 512], F32, "hpre", st) for _ in range(2)]
        pim = [self.ps([128, 512], F32, "hpim", st) for _ in range(2)]
        pny = self.ps([1, 512], F32, "hpny", st)
        fo = [self.sb([128, 2, 512], F32, "hfo", st) for _ in range(2)]
        fn = self.sb([1, 512], F32, "hfn", st)
        cw = self.sb([128, 4, 1536], F32, "hcw", st)
        for j in range(3):
            self.dma(cw[:, j, :], I["hy_conv_w"][l, j].partition_broadcast(128), reads=[], writes=[cw] if j == 0 else [], pwrites=[] if j == 0 else [cw])
        self.dma(cw[:, 3, :], I["hy_conv_b"][l].partition_broadcast(128), reads=[], pwrites=[cw])
        us = [self.sb([128, 3, 1536], BF16, "hus", st) for _ in range(2)]
        a1s = [self.sb([128, 1536], F32, "ha1", st) for _ in range(2)]
        a2s = [self.sb([128, 1536], F32, "ha2", st) for _ in range(2)]
        a3s = [self.sb([128, 1536], F32, "ha3", st) for _ in range(2)]
        ucb = [self.sb([128, 1536], BF16, "hucb", st) for _ in range(2)]

        def sc_step(tt_):
            u_ = us[tt_ % 2]
            uo = ucb[tt_ % 2]
            for j in range(3):
                self.dma(u_[:, j, :], d["hyu"][tt_ * 128 + j:tt_ * 128 + j + 128, :], reads=[d["hyu"]], writes=[u_] if j == 0 else [], pwrites=[] if j == 0 else [u_])
            a1, a2, a3 = a1s[tt_ % 2], a2s[tt_ % 2], a3s[tt_ % 2]
            self.tt(a1[:], u_[:, 0, :], cw[:, 0, :], ALU.mult, reads=[u_, cw], writes=[a1])
            self.tt(a2[:], u_[:, 1, :], cw[:, 1, :], ALU.mult, reads=[u_, cw], writes=[a2], eng="gpsimd")
            self.tt(a3[:], u_[:, 2, :], cw[:, 2, :], ALU.mult, reads=[u_, cw], writes=[a3], eng="gpsimd")
            self.tt(a1[:], a1[:], cw[:, 3, :], ALU.add, reads=[a1, cw], writes=[a1])
            self.tt(a2[:], a2[:], a3[:], ALU.add, reads=[a2, a3], writes=[a2], eng="gpsimd")
            self.tt(uo[:], a1[:], a2[:], ALU.add, reads=[a1, a2], writes=[uo])
            self.dma(d["ucd"][tt_ * 128:(tt_ + 1) * 128, :], uo[:], reads=[uo], pwrites=[d["ucd"]])
        sc_todo = list(range(ntile))
        sc_every = max(1, (2 * nkt) // ntile)
        sc_iter = 0
        nld = 0
        for o in range(2):
            self.dma(fsb[:], d["fsd"][:, o * 512:(o + 1) * 512].rearrange("(t p) c -> p t c", p=128), reads=[d["fsd"]], writes=[fsb])
            self.dma(fdb[:], d["fdd"][:, o * 512:(o + 1) * 512].rearrange("(t p) c -> p t c", p=128), reads=[d["fdd"]], writes=[fdb])
            for kt in range(nkt):
                sc_iter += 1
                if sc_todo and sc_iter % sc_every == 0:
                    sc_step(sc_todo.pop(0))
                tc_, ts_ = tcb[nld % 2], tsb[nld % 2]
                pr, pi = pre[nld % 2], pim[nld % 2]
                fo_ = fo[nld % 2]
                nld += 1
                self.dma(tc_[:], TC[kt], reads=[], writes=[tc_])
                self.dma(ts_[:], TS[kt], reads=[], writes=[ts_])
                for tt_ in range(ntile):
                    self.mm(pr, pr[:], tc_[:, tt_, :], fsb[:, tt_, :], tt_ == 0, tt_ == ntile - 1, reads=[tc_, fsb])
                for tt_ in range(ntile):
                    self.mm(pi, pi[:], ts_[:, tt_, :], fdb[:, tt_, :], tt_ == 0, tt_ == ntile - 1, reads=[ts_, fdb])
                self.stt(fo_[:, 0, :], pr[:], wk[:, kt:kt + 1], linv[:, o, :], ALU.mult, ALU.mult, reads=[pr, wk, linv], writes=[fo_])
                self.stt(fo_[:, 1, :], pi[:], wk[:, kt:kt + 1], linv[:, o, :], ALU.mult, ALU.mult, reads=[pi, wk, linv], pwrites=[fo_])
                self.dma(d["Fd"][o, :, kt * 128:(kt + 1) * 128, :].rearrange("r p c -> p r c"), fo_[:], reads=[fo_], pwrites=[d["Fd"]])
            for tt_ in range(ntile):
                self.mm(pny, pny[:], alt[:, 0:1], fsb[:, tt_, :], tt_ == 0, tt_ == ntile - 1, reads=[alt, fsb])
            self.stt(fn[:], pny[:], 1.0 / N2, linv[0:1, o, :], ALU.mult, ALU.mult, reads=[pny, linv], writes=[fn])
            self.dma(d["Fnyq"][o:o + 1, :], fn[:], reads=[fn], pwrites=[d["Fnyq"]])
        while sc_todo:
            sc_step(sc_todo.pop(0))
        self.barrier()
        st.close()

        st = contextlib.ExitStack()
        alt = self.sb([128, 2], BF16, "halt", st)
        self.dma(alt[:], I["altcol"].ap(), reads=[], writes=[alt])
        altrow = self.sb([1, 128], BF16, "haltrow", st)
        self.dma(altrow[:], I["altrow"].ap(), reads=[], writes=[altrow])
        skb = self.sb([128, 2, 512], F32, "hskb", st)
        self.dma(skb[:].rearrange("p o c -> p (o c)"), I["hy_skip"][l].partition_broadcast(128), reads=[], writes=[skb])
        zsb = self.sb([128, ntile, 512], BF16, "hzsb", st)
        Yre = self.sb([128, nkt, 512], BF16, "hYre", st)
        Ys = self.sb([128, nkt, 512], BF16, "hYs", st)
        Yn = self.sb([1, 512], BF16, "hYn", st)
        fn = self.sb([1, 512], F32, "hfn2", st)
        tcb = [self.sb([128, ntile, 128], BF16, "htc2", st) for _ in range(2)]
        tsb = [self.sb([128, ntile, 128], BF16, "hts2", st) for _ in range(2)]
        Ft = [self.sb([128, 2, 512], F32, "hFt", st) for _ in range(2)]
        m = [self.sb([128, 512], F32, "hm", st) for _ in range(4)]
        gt = [self.sb([128, 512], BF16, "hgt", st) for _ in range(2)]
        zo = [self.sb([128, 512], BF16, "hzo", st) for _ in range(2)]
        pre = [self.ps([128, 512], F32, "cpre", st) for _ in range(2)]
        pim = [self.ps([128, 512], F32, "cpim", st) for _ in range(2)]
        pny = self.ps([1, 512], F32, "cpny", st)
        py = [self.ps([128, 512], F32, "cpy", st) for _ in range(2)]
        ptr = self.ps([128, 512], BF16, "cptr", st)
        ytr = [self.sb([128, 4, 128], BF16, "hytr", st) for _ in range(2)]
        nld = 0
        for o in range(2):
            zsrc = d["ucd"][:, 1024:1536] if o == 0 else d["z2d"][:, :]
            zdep = d["ucd"] if o == 0 else d["z2d"]
            self.dma(zsb[:], zsrc.rearrange("(t p) c -> p t c", p=128), reads=[zdep], writes=[zsb])
            self.dma(fn[:], d["Fnyq"][o:o + 1, :], reads=[d["Fnyq"]], writes=[fn])
            for kt in range(nkt):
                tc_, ts_ = tcb[nld % 2], tsb[nld % 2]
                pr, pi = pre[nld % 2], pim[nld % 2]
                F_ = Ft[nld % 2]
                nld += 1
                self.dma(tc_[:], TC[kt], reads=[], writes=[tc_])
                self.dma(ts_[:], TS[kt], reads=[], writes=[ts_])
                self.dma(F_[:], d["Fd"][o, :, kt * 128:(kt + 1) * 128, :].rearrange("r p c -> p r c"), reads=[d["Fd"]], writes=[F_])
                for tt_ in range(ntile):
                    self.mm(pr, pr[:], tc_[:, tt_, :], zsb[:, tt_, :], tt_ == 0, tt_ == ntile - 1, reads=[tc_, zsb])
                for tt_ in range(ntile):
                    self.mm(pi, pi[:], ts_[:, tt_, :], zsb[:, tt_, :], tt_ == 0, tt_ == ntile - 1, reads=[ts_, zsb])
                self.tt(m[0][:], pr[:], F_[:, 0, :], ALU.mult, reads=[pr, F_], writes=[m[0]])
                self.tt(m[1][:], pi[:], F_[:, 1, :], ALU.mult, reads=[pi, F_], writes=[m[1]])
                self.tt(Yre[:, kt, :], m[0][:], m[1][:], ALU.subtract, reads=[m[0], m[1]], pwrites=[Yre], eng="gpsimd")
                self.tt(m[2][:], pr[:], F_[:, 1, :], ALU.mult, reads=[pr, F_], writes=[m[2]])
                self.tt(m[3][:], pi[:], F_[:, 0, :], ALU.mult, reads=[pi, F_], writes=[m[3]])
                self.tt(Ys[:, kt, :], m[2][:], m[3][:], ALU.add, reads=[m[2], m[3]], pwrites=[Ys], eng="gpsimd")
            for tt_ in range(ntile):
                self.mm(pny, pny[:], alt[:, 0:1], zsb[:, tt_, :], tt_ == 0, tt_ == ntile - 1, reads=[alt, zsb])
            self.tt(Yn[:], pny[:], fn[:], ALU.mult, reads=[pny, fn], writes=[Yn])
            for tt_ in range(ntile):
                tc_, ts_ = tcb[nld % 2], tsb[nld % 2]
                p = py[nld % 2]
                g_ = gt[nld % 2]
                z_ = zo[nld % 2]
                nld += 1
                self.dma(tc_[:], TC[tt_], reads=[], writes=[tc_])
                self.dma(ts_[:], TS[tt_], reads=[], writes=[ts_])
                self.dma(g_[:], d["ucd"][tt_ * 128:(tt_ + 1) * 128, o * 512:(o + 1) * 512], reads=[d["ucd"]], writes=[g_])
                for kt in range(nkt):
                    self.mm(p, p[:], tc_[:, kt, :], Yre[:, kt, :], kt == 0, False, reads=[tc_, Yre])
                    self.mm(p, p[:], ts_[:, kt, :], Ys[:, kt, :], False, False, reads=[ts_, Ys])
                self.mm(p, p[:], altrow[:, :], Yn[:], False, True, reads=[altrow, Yn])
                self.tt(m[0][:], zsb[:, tt_, :], skb[:, o, :], ALU.mult, reads=[zsb, skb], writes=[m[0]], eng="gpsimd")
                self.tt(m[1][:], p[:], m[0][:], ALU.add, reads=[p, m[0]], writes=[m[1]])
                self.tt(z_[:], m[1][:], g_[:], ALU.mult, reads=[m[1], g_], writes=[z_])
                if o == 0:
                    self.dma(d["z2d"][tt_ * 128:(tt_ + 1) * 128, :], z_[:], reads=[z_], pwrites=[d["z2d"]])
                else:
                    yt = ytr[tt_ % 2]
                    for j in range(4):
                        self.tr(ptr, ptr[:, j * 128:(j + 1) * 128], z_[:, j * 128:(j + 1) * 128], self.identB[:], j == 0, reads=[z_, self.identB])
                    self.cp(yt[:], ptr[:].rearrange("p (j t) -> p j t", j=4), reads=[ptr], writes=[yt], eng="scalar")
                    self.dma(d["yhyT"][:, tt_ * 128:(tt_ + 1) * 128].rearrange("(j p) t -> p j t", p=128), yt[:], reads=[yt], pwrites=[d["yhyT"]])
            self.barrier()
        self.barrier()
        st.close()

    def phase_merge(self, l, sm, xin, xout):
        I = self.inp
        S = sm.S
        ch = min(512, S)
        d = sm.d
        col = sm.col
        st = contextlib.ExitStack()
        wbh = self.sb([128, 4, 1024], BF16, "mwbh", st)
        wbg = self.sb([128, 4, 1024], BF16, "mwbg", st)
        wba = self.sb([128, 8, 1024], BF16, "mwba", st)
        wo = self.sb([128, 8, 1024], BF16, "mwo", st)
        self.wstage(st)
        for wt, nm, kk in ((wbh, "w_br_hy", 4), (wbg, "w_br_gla", 4), (wba, "w_br_att", 8), (wo, "w_out", 8)):
            self.wload(wt, I[nm][l], kk, 1024)
        yh = [self.sb([128, 4, ch], BF16, "myh", st) for _ in range(2)]
        yg = [self.sb([128, 4, ch], BF16, "myg", st) for _ in range(2)]
        ya = [self.sb([128, 8, ch], BF16, "mya", st) for _ in range(2)]
        mT = self.sb([128, 8, ch], BF16, "mmT", st)
        brg = [self.sb([128, 3, ch], BF16, "mbrg", st) for _ in range(2)]
        sigs = [self.sb([128, 3, ch], F32, "msig", st) for _ in range(2)]
        as_ = [self.sb([128, ch], F32, "ma", st) for _ in range(2)]
        bs_ = [self.sb([128, ch], F32, "mb", st) for _ in range(2)]
        cs_ = [self.sb([128, ch], F32, "mc", st) for _ in range(2)]
        xc = [self.sb([128, ch], F32, "mxc", st) for _ in range(2)]
        xn = [self.sb([128, ch], F32, "mxn", st) for _ in range(2)]
        p1l = [self.ps([128, ch], F32, "mp1", st) for _ in range(2)]
        p2l = [self.ps([128, ch], F32, "mp2", st) for _ in range(2)]
        p3l = [self.ps([128, ch], F32, "mp3", st) for _ in range(2)]
        py = [self.ps([128, ch], F32, "mpy", st) for _ in range(2)]
        brv = d["brT"]
        n = 0
        for c in range(S // ch):
            cs = slice(c * ch, (c + 1) * ch)
            yh_, yg_, ya_ = yh[c % 2], yg[c % 2], ya[c % 2]
            self.dma(yh_[:], d["yhyT"][:, cs].rearrange("(k p) t -> p k t", p=128), reads=[d["yhyT"]], writes=[yh_])
            self.dma(yg_[:], d["yglaT"][:, cs].rearrange("(k p) t -> p k t", p=128), reads=[d["yglaT"]], writes=[yg_])
            self.dma(ya_[:], d["yattT"][:, cs].rearrange("(k p) t -> p k t", p=128), reads=[d["yattT"]], writes=[ya_])
            for fc in range(8):
                fs_ = slice(fc * 128, (fc + 1) * 128)
                bg = brg[n % 2]
                p1, p2, p3 = p1l[n % 2], p2l[n % 2], p3l[n % 2]
                n += 1
                self.dma(bg[:], brv[:, cs].rearrange("(j k p) t -> p k j t", j=3, k=8, p=128)[:, fc], reads=[brv], writes=[bg])
                for k in range(4):
                    self.mm(p1, p1[:], wbh[:, k, fs_], yh_[:, k, :], k == 0, k == 3, reads=[wbh, yh_])
                for k in range(4):
                    self.mm(p2, p2[:], wbg[:, k, fs_], yg_[:, k, :], k == 0, k == 3, reads=[wbg, yg_])
                for k in range(8):
                    self.mm(p3, p3[:], wba[:, k, fs_], ya_[:, k, :], k == 0, k == 7, reads=[wba, ya_])
                sig, a, b, c_ = sigs[fc % 2], as_[fc % 2], bs_[fc % 2], cs_[fc % 2]
                self.act(sig[:], bg[:], AF.Sigmoid, reads=[bg], writes=[sig])
                self.tt(a[:], p1[:], sig[:, 0, :], ALU.mult, reads=[p1, sig], writes=[a])
                self.tt(b[:], p2[:], sig[:, 1, :], ALU.mult, reads=[p2, sig], writes=[b])
                self.tt(c_[:], p3[:], sig[:, 2, :], ALU.mult, reads=[p3, sig], writes=[c_])
                self.tt(a[:], a[:], b[:], ALU.add, reads=[a, b], writes=[a], eng="gpsimd")
                self.tt(mT[:, fc, :], a[:], c_[:], ALU.add, reads=[a, c_], writes=[mT] if fc == 0 else [], pwrites=[] if fc == 0 else [mT], eng="gpsimd")
            for fc in range(8):
                fs_ = slice(fc * 128, (fc + 1) * 128)
                p = py[fc % 2]
                x_ = xc[fc % 2]
                xo = xn[fc % 2]
                self.dma(x_[:], xin[fs_, cs], reads=[xin], writes=[x_])
                for k in range(8):
                    self.mm(p, p[:], wo[:, k, fs_], mT[:, k, :], k == 0, k == 7, reads=[wo, mT])
                self.stt(xo[:], p[:], self.modT[:, 16 + fc, col:col + 1], x_[:], ALU.mult, ALU.add, reads=[p, self.modT, x_], writes=[xo])
                self.dma(xout[fs_, cs], xo[:], reads=[xo], pwrites=[xout])
        self.barrier()
        st.close()

    def phase_moe(self, l, sm, xin, xout):
        I = self.inp
        S = sm.S
        col = sm.col
        half = min(2048, S)
        ch = min(512, half)
        ntt = half // 128
        for hb in range(S // half):
            hs = slice(hb * half, (hb + 1) * half)
            st = contextlib.ExitStack()
            h2T = self.sb([128, 8, half], BF16, "eh2T", st)
            lgT = self.sb([128, ntt, 16], F32, "elg", st)
            G = self.sb([128, ntt, 16], F32, "eG", st)
            acc = self.sb([128, 8, half], F32, "eacc", st)
            self.dma(acc[:], xin[:, hs].rearrange("(k p) t -> p k t", p=128), reads=[xin], writes=[acc])
            st2 = contextlib.ExitStack()
            self.phase_norm(_Slice(xin, hs), half, 1, col, h2T, st2, lgT=lgT, chmax=256)
            self.barrier()
            st2.close()
            st2 = contextlib.ExitStack()
            rb = self.sb([128, 16], F32, "erb", st2)
            self.dma(rb[:], I["router_b"].ap().partition_broadcast(128), reads=[], writes=[rb])
            sc = self.sb([128, ntt, 16], F32, "esc", st2)
            sv = self.sb([128, ntt, 16], F32, "esv", st2)
            t = self.sb([128, ntt, 16], F32, "et", st2)
            t2 = self.sb([128, ntt, 16], F32, "et2", st2)
            i1 = self.sb([128, ntt, 16], F32, "ei1", st2)
            i2 = self.sb([128, ntt, 16], F32, "ei2", st2)
            p6 = self.sb([128, ntt * 4, 6], F32, "ep6", st2)
            gs = self.sb([128, ntt, 4], F32, "egs", st2)
            gm = self.sb([128, ntt], F32, "egm", st2)
            ing = self.sb([128, ntt, 4], F32, "eing", st2)
            self.act(sc[:], lgT[:], AF.Sigmoid, reads=[lgT], writes=[sc])
            self.tt(sv[:], sc[:], rb[:].unsqueeze(1).to_broadcast([128, ntt, 16]), ALU.add, reads=[sc, rb], writes=[sv])
            s4 = sv[:].rearrange("p t (g e) -> p (t g) e", e=4)
            self.tt(p6[:, :, 0:3], s4[:, :, 0:3], s4[:, :, 1:4], ALU.add, reads=[sv], writes=[p6])
            self.tt(p6[:, :, 3:5], s4[:, :, 0:2], s4[:, :, 2:4], ALU.add, reads=[sv], pwrites=[p6])
            self.tt(p6[:, :, 5:6], s4[:, :, 0:1], s4[:, :, 3:4], ALU.add, reads=[sv], pwrites=[p6])
            self.op("vector", lambda e: e.tensor_reduce(out=gs[:].rearrange("p t g -> p (t g)"), in_=p6[:], axis=AX.X, op=ALU.max), reads=[p6], writes=[gs])
            self.op("vector", lambda e: e.tensor_reduce(out=gm[:], in_=gs[:], axis=AX.X, op=ALU.max), reads=[gs], writes=[gm])
            self.tt(ing[:], gs[:], gm[:].unsqueeze(2).to_broadcast([128, ntt, 4]), ALU.is_equal, reads=[gs, gm], writes=[ing])
            self.ts(t[:], sv[:], 2.0, None, ALU.add, None, reads=[sv], writes=[t])
            t4 = t[:].rearrange("p t (g e) -> p t g e", e=4)
            self.tt(t4, t4, ing[:].unsqueeze(3).to_broadcast([128, ntt, 4, 4]), ALU.mult, reads=[t, ing], writes=[t])
            self.ts(t[:], t[:], -2.0, None, ALU.add, None, reads=[t], writes=[t])
            self.op("vector", lambda e: e.tensor_reduce(out=gm[:], in_=t[:], axis=AX.X, op=ALU.max), reads=[t], writes=[gm])
            self.tt(i1[:], t[:], gm[:].unsqueeze(2).to_broadcast([128, ntt, 16]), ALU.is_equal, reads=[t, gm], writes=[i1])
            self.stt(t2[:], i1[:], -4.0, t[:], ALU.mult, ALU.add, reads=[i1, t], writes=[t2])
            self.op("vector", lambda e: e.tensor_reduce(out=gm[:], in_=t2[:], axis=AX.X, op=ALU.max), reads=[t2], writes=[gm])
            self.tt(i2[:], t2[:], gm[:].unsqueeze(2).to_broadcast([128, ntt, 16]), ALU.is_equal, reads=[t2, gm], writes=[i2])
            self.tt(i1[:], i1[:], i2[:], ALU.add, reads=[i1, i2], writes=[i1])
            self.tt(t[:], sc[:], i1[:], ALU.mult, reads=[sc, i1], writes=[t])
            self.op("vector", lambda e: e.tensor_reduce(out=gm[:], in_=t[:], axis=AX.X, op=ALU.add), reads=[t], writes=[gm])
            self.op("vector", lambda e: e.reciprocal(out=gm[:], in_=gm[:]), reads=[gm], writes=[gm])
            self.tt(G[:], t[:], gm[:].unsqueeze(2).to_broadcast([128, ntt, 16]), ALU.mult, reads=[t, gm], writes=[G])
            if self.dbg:
                self.dma(sm.d["gates"][hb * ntt * 128:(hb + 1) * ntt * 128, :].rearrange("(t p) e -> p t e", p=128), G[:], reads=[G], pwrites=[sm.d["gates"]])
            self.barrier()
            st2.close()
            self.wstage(st)
            Gx = [self.sb([128, ntt, 128], F32, "eGx", st) for _ in range(1)]
            wg = [self.sb([128, 8, 512], BF16, "ewg", st) for _ in range(2)]
            wu = [self.sb([128, 8, 512], BF16, "ewu", st) for _ in range(2)]
            wd = [self.sb([128, 4, 1024], BF16, "ewd", st) for _ in range(2)]
            gb = [self.sb([128, ch], F32, "egb", st) for _ in range(2)]
            sgl = [self.sb([128, ch], F32, "esgl", st) for _ in range(2)]
            tm = [self.sb([128, ch], F32, "etm", st) for _ in range(2)]
            hid = [self.sb([128, 4, ch], BF16, "ehid", st) for _ in range(2)]
            pgb = self.ps([128, ch], F32, "epgb", st)
            pg = [self.ps([128, ch], F32, "epg", st) for _ in range(2)]
            pu = [self.ps([128, ch], F32, "epu", st) for _ in range(2)]
            pd = [self.ps([128, ch], F32, "epd", st) for _ in range(2)]
            nn_ = 0
            nd = 0
            def esteps(ei):
                pcs_ = []
                for dstT, nm, K_, nc_ in ((wg[ei % 2], "moe_w_gate", 8, 512), (wu[ei % 2], "moe_w_up", 8, 512), (wd[ei % 2], "moe_w_down", 4, 1024)):
                    ksub = max(1, 2048 // nc_)
                    for k0 in range(0, K_, ksub):
                        pcs_.append((dstT, I[nm][l, ei], k0, min(ksub, K_ - k0), nc_))
                views = {}

                def do_dma(i):
                    dstT, src, k0, kn, nc_ = pcs_[i]
                    stg = self._wst[i % 2]
                    sv = stg[:, 0:kn * nc_].rearrange("p (k n) -> p k n", k=kn)
                    views[i] = (stg, sv)
                    self.dma(sv, src[k0 * 128:(k0 + kn) * 128, :].rearrange("(k p) n -> p k n", p=128), reads=[], writes=[stg])

                def do_cast(i):
                    dstT, src, k0, kn, nc_ = pcs_[i]
                    stg, sv = views[i]
                    self.cp(dstT[:, k0:k0 + kn, :nc_], sv, reads=[stg], pwrites=[dstT], eng="scalar")
                steps = []
                n_ = len(pcs_)
                for i in range(n_ + 2):
                    def st_(i=i):
                        if i >= 2:
                            do_cast(i - 2)
                        if i < n_:
                            do_dma(i)
                    steps.append(st_)
                return steps
            for f_ in esteps(0):
                f_()
            nslots = (half // ch) * 4
            for e_ in range(NEXP):
                g_, u_, d_ = wg[e_ % 2], wu[e_ % 2], wd[e_ % 2]
                nxt = esteps(e_ + 1) if e_ + 1 < NEXP else []
                per_slot = -(-len(nxt) // nslots) if nxt else 0
                gx = Gx[0]
                self.cp(gx[:], G[:, :, e_:e_ + 1].to_broadcast([128, ntt, 128]), reads=[G], writes=[gx])
                for c in range(half // ch):
                    cs = slice(c * ch, (c + 1) * ch)
                    gb_ = gb[c % 2]
                    hd = hid[c % 2]
                    for j in range(ch // 128):
                        self.mm(pgb, pgb[:, j * 128:(j + 1) * 128], gx[:, c * (ch // 128) + j, :], self.identF[:], True, True, reads=[gx, self.identF])
                    self.cp(gb_[:], pgb[:], reads=[pgb], writes=[gb_], eng="scalar")
                    for dc in range(4):
                        for _ in range(per_slot):
                            if nxt:
                                nxt.pop(0)()
                        ds_ = slice(dc * 128, (dc + 1) * 128)
                        a_, b_ = pg[nn_ % 2], pu[nn_ % 2]
                        s_, t_ = sgl[nn_ % 2], tm[nn_ % 2]
                        nn_ += 1
                        for k in range(8):
                            self.mm(a_, a_[:], g_[:, k, ds_], h2T[:, k, cs], k == 0, k == 7, reads=[g_, h2T])
                        for k in range(8):
                            self.mm(b_, b_[:], u_[:, k, ds_], h2T[:, k, cs], k == 0, k == 7, reads=[u_, h2T])
                        self.act(s_[:], a_[:], AF.Silu, reads=[a_], writes=[s_])
                        self.tt(t_[:], b_[:], s_[:], ALU.mult, reads=[b_, s_], writes=[t_])
                        self.tt(hd[:, dc, :], t_[:], gb_[:], ALU.mult, reads=[t_, gb_], writes=[hd] if dc == 0 else [], pwrites=[] if dc == 0 else [hd], eng="gpsimd")
                    for fc in range(8):
                        p_ = pd[nd % 2]
                        nd += 1
                        for dc in range(4):
                            self.mm(p_, p_[:], d_[:, dc, fc * 128:(fc + 1) * 128], hd[:, dc, :], dc == 0, dc == 3, reads=[d_, hd])
                        self.stt(acc[:, fc, cs], p_[:], self.modT[:, 40 + fc, col:col + 1], acc[:, fc, cs], ALU.mult, ALU.add,
                                 reads=[p_, self.modT, acc], pwrites=[acc])
                while nxt:
                    nxt.pop(0)()
            self.dma(xout[:, hs].rearrange("(k p) t -> p k t", p=128), acc[:], reads=[acc], pwrites=[xout])
            self.barrier()
            st.close()

    def phase_xpose_out(self, xT, out, S):
        st = contextlib.ExitStack()
        xs = [self.sb([128, 8, 128], F32, "oxs", st) for _ in range(2)]
        stg = [self.sb([128, 1024], F32, "ostg", st) for _ in range(2)]
        pt = [self.ps([128, 512], F32, "opt", st) for _ in range(4)]
        for tt in range(S // 128):
            x = xs[tt % 2]
            sg = stg[tt % 2]
            self.dma(x[:], xT[:, tt * 128:(tt + 1) * 128].rearrange("(k p) t -> p k t", p=128), reads=[xT], writes=[x])
            for half in range(2):
                p = pt[(tt * 2 + half) % 4]
                for k in range(4):
                    self.tr(p, p[:, k * 128:(k + 1) * 128], x[:, half * 4 + k, :], self.identF[:], k == 0, reads=[x, self.identF])
                self.cp(sg[:, half * 512:(half + 1) * 512], p[:], reads=[p], writes=[sg] if half == 0 else [], pwrites=[] if half == 0 else [sg],
                        eng="vector" if half == 0 else "scalar")
            self.dma(out[tt * 128:(tt + 1) * 128, :], sg[:], reads=[sg], pwrites=[out])
        self.barrier()
        st.close()


class _Slice:
    def __init__(self, t, cols):
        self.t = t
        self.buf = t.buf
        self.cols = cols

    def __getitem__(self, idx):
        r, c = idx
        base = self.cols.start
        c2 = slice(base + (c.start or 0), base + c.stop)
        return self.t[r, c2]


class Stream:
    pass


def build_program(dbg=False, stop_after=None, layers=(0, 1)):
    nc = bass.Bass("TRN2", target_bir_lowering=False)
    P = MK(nc, dbg=dbg)
    din = P.din
    din("x", [SEQ, D]); din("ctx", [CTX, D]); din("cT", [128, 8, 2])
    din("w_mod", [2, D, 6 * D]); din("b_modT", [2, 128, 48]); din("g1T", [2, 128, 8]); din("g2T", [2, 128, 8])
    din("w_in", [2, D, NIN]); din("qkg", [2, 128, 2])
    din("gla_wa2", [2, 2, 16, 256]); din("gla_baT", [2, 64, 2, 4]); din("glag", [2, 128, 1]); din("gla_mask", [128, 2, 128])
    din("ropeC", [128, SEQ]); din("ropeS", [128, SEQ]); din("ropeRT", [128, 128])
    din("hy_pos_w1", [2, 33, 64]); din("hy_pos_w2", [2, 64, 64]); din("hy_pos_w3", [2, 64, 2048]); din("hy_pv", [2, 64, 4])
    din("hy_decay", [2, 2048]); din("hy_skip", [2, 1024]); din("hy_conv_w", [2, 3, 1536]); din("hy_conv_b", [2, 1536])
    din("zembL", [33, SEQ]); din("zembC", [33, CTX]); din("negtnL", [128, 32]); din("negtnC", [128, 2])
    din("wkL", [128, 32]); din("wkC", [128, 2])
    din("dftCL", [32, 128, 32, 128], BF16); din("dftSL", [32, 128, 32, 128], BF16)
    din("dftCC", [2, 128, 2, 128], BF16); din("dftSC", [2, 128, 2, 128], BF16)
    din("altcol", [128, 2], BF16); din("altrow", [1, 128], BF16)
    din("w_br_hy", [2, 512, D]); din("w_br_gla", [2, 512, D]); din("w_br_att", [2, D, D]); din("w_out", [2, D, D])
    din("router_w", [D, 16]); din("router_b", [16]); din("moe_sel", [16, 16, 128])
    din("moe_w_gate", [2, 16, D, DE]); din("moe_w_up", [2, 16, D, DE]); din("moe_w_down", [2, 16, DE, D])
    out = P.dram("out", [SEQ, D], F32, kind="ExternalOutput")

    P.setup_consts()
    P.modT = P.sb([128, 48, 2], F32, "modT")
    P.AA = P.sb([128, 2, 8, 2], F32, "AA")
    P.linv = P.sb([128, 2, 512], F32, "linv")
    g0 = P.sb([64, 4, 2, 128], F32, "gstate0")
    gc = P.sb([64, 4, 2, 128], F32, "gstatec")
    P.op("vector", lambda e: e.memset(g0[:], 0.0), writes=[g0])
    P.krT = P.dscr("krT", [256, SEQ + CTX], BF16)
    P.av = P.dscr("av", [SEQ + CTX, 256], BF16)
    streams = []
    for nm, S, col in (("c", CTX, 1), ("l", SEQ, 0)):
        sm = Stream()
        sm.S, sm.col, sm.nm = S, col, nm
        sm.rope = nm == "l"
        sm.koff = 0 if nm == "l" else SEQ
        sm.keys = (0, SEQ + CTX) if nm == "l" else (SEQ, SEQ + CTX)
        sm.gin = gc if nm == "l" else g0
        sm.gout = None if nm == "l" else gc
        dd = {}
        for k, shp, dt in (("xTa", [D, S], F32), ("xTb", [D, S], F32), ("kTraw", [256, S], F32), ("qTraw", [1024, S], F32),
                           ("gkT", [256, S], F32), ("gqT", [256, S], F32), ("gaT", [32, S], F32), ("ogT", [512, S], BF16),
                           ("brT", [3072, S], BF16), ("gv", [S, 512], BF16), ("hyu", [S + 2, 1536], BF16), ("qrT", [1024, S], BF16),
                           ("yattT", [1024, S], BF16), ("yglaT", [512, S], BF16), ("yhyT", [512, S], BF16),
                           ("fsd", [S, 1024], BF16), ("fdd", [S, 1024], BF16), ("Fd", [2, 2, S, 512], F32), ("Fnyq", [2, 512], F32),
                           ("ucd", [S, 1536], BF16), ("z2d", [S, 512], BF16), ("gates", [S, 16], F32)):
            dd[k] = P.dscr(f"{nm}_{k}", shp, dt)
        sm.d = dd
        streams.append(sm)
    ctxs, lat = streams

    def done(tag):
        return stop_after == tag

    def finish():
        P.finish()
        return nc, P

    P.phase_xpose_in(P.inp["ctx"], ctxs.d["xTa"], CTX)
    P.phase_xpose_in(P.inp["x"], lat.d["xTa"], SEQ)
    if done("xpose"):
        return finish()
    for l in layers:
        last = l == DEPTH - 1
        P.phase_mods(l)
        for sm in (ctxs, lat):
            S = sm.S
            d = sm.d
            st = contextlib.ExitStack()
            hT = P.sb([128, 8, S], BF16, "hT", st)
            st2 = contextlib.ExitStack()
            P.phase_norm(d["xTa"], S, 0, sm.col, hT, st2)
            P.barrier()
            st2.close()
            if P.dbg and l == layers[0]:
                hdbg = P.dscr(f"{sm.nm}_hT", [D, S], BF16)
                P.dma(hdbg.ap().rearrange("(k p) t -> p k t", p=128), hT[:], reads=[hT], writes=[hdbg])
            win = P.inp["w_in"][l]
            zr = P.sb([1, 1536], BF16, "zr", st)
            P.op("vector", lambda e: e.memset(zr[:], 0.0), writes=[zr])
            P.dma(d["hyu"][0:1, :], zr[:], reads=[zr], pwrites=[d["hyu"]])
            P.dma(d["hyu"][S + 1:S + 2, :], zr[:], reads=[zr], pwrites=[d["hyu"]])
            kv_only = last and sm is ctxs
            for (c0, ncol, key, dt) in ((0, 256, "kTraw", F32), (512, 256, "gkT", F32), (1280, 32, "gaT", F32), (1312, 1024, "qTraw", F32),
                                        (2336, 256, "gqT", F32), (2592, 512, "ogT", BF16), (4640, 3072, "brT", BF16)):
                if kv_only and key in ("qTraw", "gqT", "ogT", "brT"):
                    continue
                P.linear_fm(win, 8, c0, ncol, hT, S, d[key], 0, dt)
            P.linear_tm(win, 8, 256, 256, hT, S, P.av, sm.koff, 0, BF16)
            P.linear_tm(win, 8, 768, 512, hT, S, d["gv"], 0, 0, BF16)
            if not kv_only:
                P.linear_tm(win, 8, 3104, 1536, hT, S, d["hyu"], 1, 0, BF16)
            P.barrier()
            st.close()
            if done(sm.nm + ":proj"):
                return finish()
            P.phase_qkprep(l, sm)
            if done(sm.nm + ":qkprep"):
                return finish()
            P.phase_gla(l, sm)
            if done(sm.nm + ":gla"):
                return finish()
            if last and sm is ctxs:
                continue
            P.phase_attn(sm)
            if done(sm.nm + ":attn"):
                return finish()
            P.phase_hyena(l, sm)
            if done(sm.nm + ":hyena"):
                return finish()
            P.phase_merge(l, sm, d["xTa"], d["xTb"])
            if done(sm.nm + ":merge"):
                return finish()
            P.phase_moe(l, sm, d["xTb"], d["xTa"])
            if done(sm.nm + ":moe"):
                return finish()
    P.phase_xpose_out(lat.d["xTa"], out, SEQ)
    return finish()


def host_consts():
    c = {}
    f32 = np.float32
    bf = ml_dtypes.bfloat16
    t = np.arange(SEQ)
    row = (t // 64).astype(f32)
    colv = (t % 64).astype(f32)
    inv = (np.float32(10000.0) ** (-np.arange(32, dtype=f32) / np.float32(32))).astype(f32)
    ang = np.concatenate([row[:, None] * inv[None, :], colv[:, None] * inv[None, :]], axis=-1).astype(f32)
    pidx = np.arange(128) // 2
    c["ropeC"] = np.ascontiguousarray(np.cos(ang)[:, pidx].T.astype(f32))
    c["ropeS"] = np.ascontiguousarray(np.sin(ang)[:, pidx].T.astype(f32))
    RT = np.zeros((128, 128), f32)
    for i in range(64):
        RT[2 * i + 1, 2 * i] = -1.0
        RT[2 * i, 2 * i + 1] = 1.0
    c["ropeRT"] = RT
    s = np.arange(128)[:, None]
    q = np.arange(128)[None, :]
    same = (s // 64) == (q // 64)
    mk = np.zeros((128, 2, 128), f32)
    mk[:, 0, :] = (same & (s <= q)).astype(f32)
    mk[:, 1, :] = (same & (s >= q)).astype(f32)
    c["gla_mask"] = mk
    for sfx, n in (("L", SEQ), ("C", CTX)):
        tt = np.arange(n, dtype=f32)
        tn = (tt / np.float32(n)).astype(f32)
        bands = np.linspace(1e-4, 15, 16, dtype=f32)
        phase = (np.float32(2 * math.pi / n) * tt[:, None] * bands[None, :]).astype(f32)
        z = np.concatenate([tn[:, None], np.cos(phase), -np.sin(phase)], axis=-1).astype(f32)
        c["zemb" + sfx] = np.ascontiguousarray(z.T)
        nt = n // 128
        c["negtn" + sfx] = np.ascontiguousarray((-tn).reshape(nt, 128).T)
        N2 = 2 * n
        wk = np.full(n, 2.0 / N2, f32)
        wk[0] = 1.0 / N2
        c["wk" + sfx] = np.ascontiguousarray(wk.reshape(nt, 128).T)
        idx = np.arange(n, dtype=np.int64)
        prod = (idx[:, None] * idx[None, :]) % N2
        angd = prod.astype(np.float64) * (2 * math.pi / N2)
        for nm, fn in (("dftC", np.cos), ("dftS", np.sin)):
            M = fn(angd).astype(f32)
            T4 = M.reshape(nt, 128, nt, 128).transpose(2, 1, 0, 3)
            c[nm + sfx] = np.ascontiguousarray(T4).astype(bf)
    alt = np.where(np.arange(128) % 2 == 0, 1.0, -1.0).astype(f32)
    c["altcol"] = np.stack([alt, alt], axis=1).astype(bf)
    c["altrow"] = alt[None, :].astype(bf)
    sel = np.zeros((16, 16, 128), f32)
    for e in range(16):
        sel[e, e, :] = 1.0
    c["moe_sel"] = sel
    return c


_CONSTS = None


def host_inputs(inp):
    global _CONSTS
    if _CONSTS is None:
        _CONSTS = host_consts()
    f32 = np.float32
    g = {k: np.asarray(v) for k, v in inp.items()}
    sh = dict(_CONSTS)
    for k in ("w_mod", "w_in", "gla_wa2", "hy_pos_w1", "hy_pos_w2", "hy_pos_w3", "hy_conv_w", "hy_conv_b", "w_br_hy", "w_br_gla",
              "w_br_att", "w_out", "router_w", "router_b", "moe_w_gate", "moe_w_up", "moe_w_down"):
        sh[k] = np.ascontiguousarray(g[k], dtype=f32)
    sh["b_modT"] = np.ascontiguousarray(g["b_mod"].reshape(2, 48, 128).transpose(0, 2, 1))
    sh["g1T"] = np.ascontiguousarray(g["norm1_g"].reshape(2, 8, 128).transpose(0, 2, 1))
    sh["g2T"] = np.ascontiguousarray(g["norm2_g"].reshape(2, 8, 128).transpose(0, 2, 1))
    sh["qkg"] = np.ascontiguousarray(np.stack([g["q_norm_g"], g["k_norm_g"]], axis=-1))
    sh["gla_baT"] = np.ascontiguousarray(g["gla_ba"].reshape(2, 2, 4, 64).transpose(0, 3, 1, 2))
    sh["glag"] = np.ascontiguousarray(g["gla_norm_g"].reshape(2, 128, 1))
    pv = np.zeros((2, 64, 4), f32)
    pv[:, :, 0] = g["hy_pos_b1"]
    pv[:, :, 1] = g["hy_sin_freq"]
    pv[:, :, 2] = g["hy_pos_b2"]
    sh["hy_pv"] = pv
    sh["hy_decay"] = np.ascontiguousarray(g["hy_decay"].reshape(2, 2048))
    sh["hy_skip"] = np.ascontiguousarray(g["hy_skip"].reshape(2, 1024))
    return sh, g


def core_inputs(sh, g, b):
    m = dict(sh)
    m["x"] = np.ascontiguousarray(g["x"][b], dtype=np.float32)
    m["ctx"] = np.ascontiguousarray(g["ctx"][b], dtype=np.float32)
    cT = np.stack([g["c"][b].reshape(8, 128).T, g["c_ctx"].reshape(8, 128).T], axis=-1)
    m["cT"] = np.ascontiguousarray(cT, dtype=np.float32)
    return m


def kernel(**inputs):
    sh, g = host_inputs(inputs)
    nc, P = build_program()
    in_maps = [core_inputs(sh, g, b) for b in range(8)]
    res = run_bass_kernel_spmd(nc, in_maps, core_ids=list(range(8)))
    return np.stack([np.asarray(r["out"]) for r in res.results], axis=0).astype(np.float32)
```

```python
import contextlib
import math
import numpy as np
import ml_dtypes
import concourse.bass as bass
import concourse.mybir as mybir
from concourse.bass_utils import run_bass_kernel_spmd

F32 = mybir.dt.float32
BF16 = mybir.dt.bfloat16
I32 = mybir.dt.int32
AF = mybir.ActivationFunctionType
ALU = mybir.AluOpType
AX = mybir.AxisListType

D = 1024
SEQ = 4096
CTX = 256
DEPTH = 2
NIN = 7712
NKV = 1312
EPS = 1e-6
NEXP = 16
DE = 512
HYW = 512

SEM_LIMIT = 30000


class Buf:
    def __init__(self, name):
        self.name = name
        self.writers = {}
        self.readers = {}
        self.prev = {}


class T:
    def __init__(self, t, name, dram=False):
        self.t = t
        self.buf = Buf(name)
        self.name = name
        self.dram = dram
        self.view = None

    def __getitem__(self, idx):
        if self.dram:
            return self.t.ap()[idx]
        if self.view is not None:
            return self.view[idx]
        return self.t[idx]

    def ap(self):
        return self.t.ap() if self.dram else self.t[:]


class Eng:
    def __init__(self, P, name, handle):
        self.P = P
        self.name = name
        self.h = handle
        self.sem = None
        self.count = 0
        self.seen = {}
        self.nsem = 0

    def new_sem(self):
        self.sem = self.P.alloc_sem(f"{self.name}{self.nsem}")
        self.nsem += 1
        self.count = 0


class Prog:
    def __init__(self, nc):
        self.nc = nc
        self.stack = contextlib.ExitStack()
        self.eng = {}
        for n in ("tensor", "vector", "scalar", "gpsimd", "sync"):
            e = Eng(self, n, getattr(nc, n))
            self.eng[n] = e
        self.nsems = 0
        for e in self.eng.values():
            e.new_sem()
        self.dma_sems = []
        self.dma_rr = 0
        for i in range(24):
            self.dma_sems.append([self.alloc_sem(f"dma{i}"), 0, i])
        self.ndma_gen = 24
        self.all_tokens = {}
        self.uid = 0
        self.pending = []
        self.max_pending = 2

    def alloc_sem(self, name):
        self.nsems += 1
        return self.stack.enter_context(self.nc.semaphore(name))

    def sb(self, shape, dtype, name=None, stack=None):
        self.uid += 1
        nm = f"{name or 't'}_{self.uid}"
        t = (stack or self.stack).enter_context(self.nc.sbuf_tensor(nm, list(shape), dtype))
        return T(t, nm)

    def ps(self, shape, dtype=F32, name=None, stack=None):
        self.uid += 1
        nm = f"{name or 'p'}_{self.uid}"
        full = 512 if dtype == F32 else 1024
        t = (stack or self.stack).enter_context(self.nc.psum_tensor(nm, [128, full], dtype))
        free = 1
        for d_ in shape[1:]:
            free *= d_
        assert free <= full
        v = t[0:shape[0], 0:free]
        if len(shape) == 3:
            v = v.rearrange("p (a b) -> p a b", a=shape[1])
        r = T(t, nm)
        r.view = v
        return r

    def dram(self, name, shape, dtype, kind="Internal"):
        t = self.nc.dram_tensor(name, list(shape), dtype, kind=kind)
        return T(t, name, dram=True)

    def _need(self, E, toks):
        for key, (sem, val) in toks.items():
            if E.seen.get(key, 0) < val:
                E.h.wait_ge(sem, val)
                E.seen[key] = val

    def _deps(self, E, reads, writes, pwrites, skip_same=False):
        need = {}

        def add(d):
            for k, (s, v) in d.items():
                if skip_same and k == id(E.sem):
                    continue
                if k not in need or need[k][1] < v:
                    need[k] = (s, v)
        for b in reads:
            add(b.writers)
        for b in writes:
            add(b.writers)
            add(b.readers)
        for b in pwrites:
            add(b.readers)
            add(b.prev)
        self._need(E, need)

    def _commit(self, tok, reads, writes, pwrites):
        k = id(tok[0])
        for b in writes:
            pv = dict(b.writers)
            for kk, vv in b.readers.items():
                if kk not in pv or pv[kk][1] < vv[1]:
                    pv[kk] = vv
            b.prev = pv
            b.writers = {k: tok}
            b.readers = {}
        for b in pwrites:
            b.writers[k] = tok
        for b in reads:
            b.readers[k] = tok
        self.all_tokens[k] = tok

    def op(self, en, fn, reads=(), writes=(), pwrites=()):
        E = self.eng[en]
        reads = [getattr(r, "buf", r) for r in reads]
        writes = [getattr(r, "buf", r) for r in writes]
        pwrites = [getattr(r, "buf", r) for r in pwrites]
        if E.count >= SEM_LIMIT:
            E.new_sem()
        if self.pending and self._pending_conflict(writes + pwrites, ()):
            self.flush_stores()
        self._deps(E, reads, writes, pwrites, skip_same=(en == "tensor"))
        ins = fn(E.h)
        E.count += 1
        ins.then_inc(E.sem, 1)
        tok = (E.sem, E.count)
        E.seen[id(E.sem)] = max(E.seen.get(id(E.sem), 0), 0)
        self._commit(tok, reads, writes, pwrites)
        return ins

    def _pending_conflict(self, bufs_w, bufs_r):
        if not self.pending:
            return False
        for ent in self.pending:
            src, dst = ent[7], ent[8]
            for b in bufs_w:
                if id(b) in src or id(b) in dst:
                    return True
            for b in bufs_r:
                if id(b) in dst:
                    return True
        return False

    def flush_stores(self, keep=0):
        while len(self.pending) > keep:
            ent = self.pending.pop(0)
            self._dma_emit(*ent[:7])

    def dma(self, out, in_, reads=(), writes=(), pwrites=(), q="sync", **kw):
        is_store = any(getattr(r, "dram", False) for r in list(writes) + list(pwrites)) and not any(getattr(r, "dram", False) for r in reads)
        reads = [getattr(r, "buf", r) for r in reads]
        writes = [getattr(r, "buf", r) for r in writes]
        pwrites = [getattr(r, "buf", r) for r in pwrites]
        if is_store:
            src = {id(b) for b in reads}
            dst = {id(b) for b in writes + pwrites}
            self.pending.append((out, in_, reads, writes, pwrites, q, kw, src, dst))
            self.flush_stores(keep=self.max_pending)
            return None
        if self._pending_conflict(writes + pwrites, reads):
            self.flush_stores()
        return self._dma_emit(out, in_, reads, writes, pwrites, q, kw)

    def _dma_emit(self, out, in_, reads, writes, pwrites, q, kw):
        E = self.eng[q]
        slot = self.dma_sems[self.dma_rr]
        self.dma_rr = (self.dma_rr + 1) % len(self.dma_sems)
        if slot[1] + 16 > SEM_LIMIT:
            self._need(E, {id(slot[0]): (slot[0], slot[1])})
            slot[0] = self.alloc_sem(f"dma{self.ndma_gen}")
            self.ndma_gen += 1
            slot[1] = 0
        sem = slot[0]
        if slot[1] > 0:
            self._need(E, {id(sem): (sem, slot[1])})
        self._deps(E, reads, writes, pwrites)
        ins = E.h.dma_start(out=out, in_=in_, **kw)
        slot[1] += 16
        ins.then_inc(sem, 16)
        tok = (sem, slot[1])
        self._commit(tok, reads, writes, pwrites)
        return ins

    def barrier(self):
        self.flush_stores()
        for E in self.eng.values():
            self._need(E, dict(self.all_tokens))

    def finish(self):
        self.barrier()
        self.stack.close()


def _rr(lst, i):
    return lst[i % len(lst)]


class MK(Prog):
    def __init__(self, nc, dbg=False, layers=(0, 1), phases=None):
        super().__init__(nc)
        self.dbg = dbg
        self.layers = layers
        self.phases = phases
        self.inp = {}
        self.scr = {}

    def din(self, name, shape, dtype=F32):
        t = self.dram(name, shape, dtype, kind="ExternalInput")
        self.inp[name] = t
        return t

    def dscr(self, name, shape, dtype):
        t = self.dram(name, shape, dtype, kind="ExternalOutput" if self.dbg else "Internal")
        self.scr[name] = t
        return t

    def mm(self, ps, out, lhsT, rhs, first, last, reads):
        self.op("tensor", lambda e: e.matmul(out, lhsT, rhs, start=first, stop=last), reads=reads,
                writes=[ps] if first else [], pwrites=[] if first else [ps])

    def tr(self, ps, out, in_, ident, first, reads):
        self.op("tensor", lambda e: e.transpose(out, in_, ident), reads=reads,
                writes=[ps] if first else [], pwrites=[] if first else [ps])

    def act(self, out, in_, func, reads, writes=(), pwrites=(), **kw):
        self.op("scalar", lambda e: e.activation(out=out, in_=in_, func=func, **kw), reads=reads, writes=writes, pwrites=pwrites)

    def tt(self, out, in0, in1, op, reads, writes=(), pwrites=(), eng="vector"):
        self.op(eng, lambda e: e.tensor_tensor(out=out, in0=in0, in1=in1, op=op), reads=reads, writes=writes, pwrites=pwrites)

    def ts(self, out, in0, s1, s2, op0, op1, reads, writes=(), pwrites=(), eng="vector"):
        if op1 is None:
            self.op(eng, lambda e: e.tensor_scalar(out=out, in0=in0, scalar1=s1, scalar2=None, op0=op0), reads=reads, writes=writes, pwrites=pwrites)
        else:
            self.op(eng, lambda e: e.tensor_scalar(out=out, in0=in0, scalar1=s1, scalar2=s2, op0=op0, op1=op1), reads=reads, writes=writes, pwrites=pwrites)

    def stt(self, out, in0, scalar, in1, op0, op1, reads, writes=(), pwrites=()):
        self.op("vector", lambda e: e.scalar_tensor_tensor(out=out, in0=in0, scalar=scalar, in1=in1, op0=op0, op1=op1), reads=reads, writes=writes, pwrites=pwrites)

    def cp(self, out, in_, reads, writes=(), pwrites=(), eng="vector"):
        if eng == "scalar":
            self.op("scalar", lambda e: e.copy(out=out, in_=in_), reads=reads, writes=writes, pwrites=pwrites)
        else:
            self.op(eng, lambda e: e.tensor_copy(out=out, in_=in_), reads=reads, writes=writes, pwrites=pwrites)

    def wstage(self, st):
        self._wst = [self.sb([128, 2048], F32, "wst", st) for _ in range(2)]
        self._wsti = 0

    def wload(self, dstT, src, K, ncols):
        ksub = max(1, 2048 // ncols)
        for k0 in range(0, K, ksub):
            kn = min(ksub, K - k0)
            stg = self._wst[self._wsti % 2]
            self._wsti += 1
            sv = stg[:, 0:kn * ncols].rearrange("p (k n) -> p k n", k=kn)
            self.dma(sv, src[k0 * 128:(k0 + kn) * 128, :].rearrange("(k p) n -> p k n", p=128), reads=[], writes=[stg])
            first = k0 == 0
            self.cp(dstT[:, k0:k0 + kn, :ncols], sv, reads=[stg], writes=[dstT] if first else [], pwrites=[] if first else [dstT], eng="gpsimd")

    def rstd_from_ss(self, out, ps_ap, n, tmp_ap, reads, tmpT, outT):
        self.act(tmp_ap, ps_ap, AF.Sqrt, reads=reads, writes=[tmpT], scale=1.0 / n, bias=self.epsc[:, 0:1])
        self.op("vector", lambda e: e.reciprocal(out=out, in_=tmp_ap), reads=[tmpT], writes=[outT])

    def setup_consts(self):
        c = {}
        self.identF = self.sb([128, 128], F32, "identF")
        self.identB = self.sb([128, 128], BF16, "identB")
        self.onesF = self.sb([128, 128], F32, "onesF")
        self.onesB = self.sb([128, 128], BF16, "onesB")
        self.epsc = self.sb([128, 1], F32, "epsc")
        self.op("vector", lambda e: e.memset(self.epsc[:], EPS), writes=[self.epsc])
        self.op("gpsimd", lambda e: e.memset(self.identF[:], 1.0), writes=[self.identF])
        self.op("gpsimd", lambda e: e.affine_select(out=self.identF[:], in_=self.identF[:], pattern=[[-1, 128]],
                                                     compare_op=ALU.is_equal, fill=0.0, base=0, channel_multiplier=1),
                reads=[self.identF], writes=[self.identF])
        self.cp(self.identB[:], self.identF[:], reads=[self.identF], writes=[self.identB])
        self.op("vector", lambda e: e.memset(self.onesF[:], 1.0), writes=[self.onesF])
        self.op("vector", lambda e: e.memset(self.onesB[:], 1.0), writes=[self.onesB])

    def phase_mods(self, l):
        I = self.inp
        st = contextlib.ExitStack()
        scT = self.sb([128, 8, 2], F32, "scT", st)
        self.dma(scT[:], I["cT"].ap(), reads=[I["cT"]], writes=[scT])
        self.act(scT[:], scT[:], AF.Silu, reads=[scT], writes=[scT])
        wm = [self.sb([128, 8, 512], F32, "wm", st) for _ in range(2)]
        pm = self.ps([128, 96], F32, "pm", st)
        bm = self.sb([128, 48], F32, "bm", st)
        gg = self.sb([128, 2, 8], F32, "gg", st)
        self.dma(bm[:], I["b_modT"][l], reads=[I["b_modT"]], writes=[bm])
        self.dma(gg[:, 0, :], I["g1T"][l], reads=[I["g1T"]], pwrites=[gg])
        self.dma(gg[:, 1, :], I["g2T"][l], reads=[I["g2T"]], pwrites=[gg])
        first = True
        for ob in range(12):
            w = wm[ob % 2]
            self.dma(w[:], I["w_mod"][l, :, ob * 512:(ob + 1) * 512].rearrange("(k p) n -> p k n", p=128),
                     reads=[I["w_mod"]], writes=[w])
            for j in range(4):
                oc = ob * 4 + j
                for kc in range(8):
                    self.mm(pm, pm[:, oc * 2:oc * 2 + 2], w[:, kc, j * 128:(j + 1) * 128], scT[:, kc, :],
                            kc == 0, kc == 7, reads=[w, scT])
        modT = self.modT
        self.tt(modT[:], pm[:].rearrange("p (c t) -> p c t", t=2), bm[:].unsqueeze(2).to_broadcast([128, 48, 2]), ALU.add,
                reads=[pm, bm], writes=[modT])
        AA = self.AA
        for i, sc0 in ((0, 8), (1, 32)):
            self.ts(AA[:, i], modT[:, sc0:sc0 + 8, :], 1.0, None, ALU.add, None, reads=[modT], pwrites=[AA])
            self.tt(AA[:, i], AA[:, i], gg[:, i, :].unsqueeze(2).to_broadcast([128, 8, 2]), ALU.mult, reads=[AA, gg], pwrites=[AA])
        self.barrier()
        st.close()

    def phase_xpose_in(self, src, dstT, S):
        st = contextlib.ExitStack()
        xs = [self.sb([128, 1024], F32, "xs", st) for _ in range(2)]
        stg = [self.sb([128, 8, 128], F32, "stg", st) for _ in range(2)]
        pt = [self.ps([128, 512], F32, "pt", st) for _ in range(4)]
        for tt in range(S // 128):
            x = xs[tt % 2]
            sg = stg[tt % 2]
            self.dma(x[:], src[tt * 128:(tt + 1) * 128, :], reads=[src], writes=[x])
            for half in range(2):
                p = pt[(tt * 2 + half) % 4]
                for k in range(4):
                    kk = half * 4 + k
                    self.tr(p, p[:, k * 128:(k + 1) * 128], x[:, kk * 128:(kk + 1) * 128], self.identF[:], k == 0, reads=[x, self.identF])
                self.cp(sg[:, half * 4:(half + 1) * 4, :], p[:].rearrange("p (k n) -> p k n", k=4), reads=[p],
                        writes=[sg] if half == 0 else [], pwrites=[] if half == 0 else [sg], eng="vector" if half == 0 else "scalar")
            self.dma(dstT[:, tt * 128:(tt + 1) * 128].rearrange("(k p) t -> p k t", p=128), sg[:], reads=[sg], pwrites=[dstT])
        self.barrier()
        st.close()

    def phase_norm(self, xT, S, which, col, hT, st, lgT=None, chmax=512):
        A = self.AA
        B0 = 0 if which == 0 else 24
        ch = min(chmax, S)
        xc = [self.sb([128, 8, ch], F32, "xc", st) for _ in range(2)]
        sq = self.sb([128, 8, ch], F32, "sq", st)
        hf = [self.sb([128, 8, ch], F32, "hf", st) for _ in range(2)]
        tmp = self.sb([128, ch], F32, "ntmp", st)
        rstd = self.sb([128, ch], F32, "rstd", st)
        pss = [self.ps([128, ch], F32, "pss", st) for _ in range(2)]
        if lgT is not None:
            rw = self.sb([128, 8, 16], F32, "rw", st)
            self.dma(rw[:], self.inp["router_w"].ap().rearrange("(k p) e -> p k e", p=128), reads=[self.inp["router_w"]], writes=[rw])
            psr = [self.ps([128, (ch // 128) * 16], F32, "psr", st) for _ in range(2)]
        for c in range(S // ch):
            x = xc[c % 2]
            h = hf[c % 2]
            p = pss[c % 2]
            self.dma(x[:], xT[:, c * ch:(c + 1) * ch].rearrange("(k p) t -> p k t", p=128), reads=[xT], writes=[x])
            self.act(sq[:], x[:], AF.Square, reads=[x], writes=[sq])
            for kc in range(8):
                self.mm(p, p[:], self.onesF[:], sq[:, kc, :], kc == 0, kc == 7, reads=[sq, self.onesF])
            self.rstd_from_ss(rstd[:], p[:], 1024.0, tmp[:], [p], tmp, rstd)
            for kc in range(8):
                self.tt(h[:, kc, :], x[:, kc, :], rstd[:], ALU.mult, reads=[x, rstd], writes=[h] if kc == 0 else [], pwrites=[] if kc == 0 else [h])
                self.act(h[:, kc, :], h[:, kc, :], AF.Identity, reads=[h, A, self.modT], pwrites=[h],
                         scale=A[:, which, kc, col:col + 1], bias=self.modT[:, B0 + kc, col:col + 1])
            self.cp(hT[:, :, c * ch:(c + 1) * ch], h[:], reads=[h], pwrites=[hT], eng="gpsimd")
            if lgT is not None:
                pr = psr[c % 2]
                nj = ch // 128
                for j in range(nj):
                    for kc in range(8):
                        self.mm(pr, pr[:, j * 16:(j + 1) * 16], h[:, kc, j * 128:(j + 1) * 128], rw[:, kc, :], kc == 0, kc == 7, reads=[rw, h])
                self.cp(lgT[:, c * nj:(c + 1) * nj, :], pr[:].rearrange("p (j e) -> p j e", e=16), reads=[pr], pwrites=[lgT], eng="scalar")

    def linear_fm(self, wsrc, wrow_chunks, col0, ncols, acts, S, dst, drow0, evac_dt, st_outer=None, wtiles=None):
        st = contextlib.ExitStack()
        K = wrow_chunks
        ch = min(512, S)
        wb = [self.sb([128, K, 512], BF16, "wb", st) for _ in range(2)]
        self.wstage(st)
        stg = [self.sb([128, ch], evac_dt, "lstg", st) for _ in range(3)]
        pp = [self.ps([128, ch], F32, "lps", st) for _ in range(3)]
        n = 0
        blocks = list(range(0, ncols, 512))
        self.wload(wb[0], wsrc[:, col0:col0 + min(512, ncols)], K, min(512, ncols))
        for bi, b0 in enumerate(blocks):
            bw = min(512, ncols - b0)
            w = wb[bi % 2]
            if bi + 1 < len(blocks):
                nb0 = blocks[bi + 1]
                nbw = min(512, ncols - nb0)
                self.wload(wb[(bi + 1) % 2], wsrc[:, col0 + nb0:col0 + nb0 + nbw], K, nbw)
            for m0 in range(0, bw, 128):
                msz = min(128, bw - m0)
                for c in range(S // ch):
                    p = pp[n % 3]
                    sg = stg[n % 3]
                    for kc in range(K):
                        self.mm(p, p[:msz, :], w[:, kc, m0:m0 + msz], acts[:, kc, c * ch:(c + 1) * ch], kc == 0, kc == K - 1, reads=[w, acts])
                    self.cp(sg[:msz, :], p[:msz, :], reads=[p], writes=[sg], eng="vector" if n % 2 == 0 else "scalar")
                    r0 = drow0 + b0 + m0
                    self.dma(dst[r0:r0 + msz, c * ch:(c + 1) * ch], sg[:msz, :], reads=[sg], pwrites=[dst])
                    n += 1
        self.barrier()
        st.close()

    def linear_tm(self, wsrc, K, col0, ncols, acts, S, dst, drow0, dcol0, evac_dt):
        st = contextlib.ExitStack()
        wb = [self.sb([128, K, 512], BF16, "wbt", st) for _ in range(2)]
        self.wstage(st)
        stg = [self.sb([128, 512], evac_dt, "tstg", st) for _ in range(3)]
        pp = [self.ps([128, 512], F32, "tps", st) for _ in range(3)]
        n = 0
        blocks = list(range(0, ncols, 512))
        self.wload(wb[0], wsrc[:, col0:col0 + min(512, ncols)], K, min(512, ncols))
        for bi, b0 in enumerate(blocks):
            bw = min(512, ncols - b0)
            w = wb[bi % 2]
            if bi + 1 < len(blocks):
                nb0 = blocks[bi + 1]
                nbw = min(512, ncols - nb0)
                self.wload(wb[(bi + 1) % 2], wsrc[:, col0 + nb0:col0 + nb0 + nbw], K, nbw)
            for tt in range(S // 128):
                p = pp[n % 3]
                sg = stg[n % 3]
                for kc in range(K):
                    self.mm(p, p[:, :bw], acts[:, kc, tt * 128:(tt + 1) * 128], w[:, kc, :bw], kc == 0, kc == K - 1, reads=[w, acts])
                self.cp(sg[:, :bw], p[:, :bw], reads=[p], writes=[sg], eng="vector" if n % 2 == 0 else "scalar")
                self.dma(dst[drow0 + tt * 128:drow0 + (tt + 1) * 128, dcol0 + b0:dcol0 + b0 + bw], sg[:, :bw], reads=[sg], pwrites=[dst])
                n += 1
        self.barrier()
        st.close()

    def phase_qkprep(self, l, sm):
        I = self.inp
        S = sm.S
        ch = min(512, S)
        st = contextlib.ExitStack()
        qkg = self.sb([128, 2], F32, "qkg", st)
        self.dma(qkg[:], I["qkg"][l], reads=[I["qkg"]], writes=[qkg])
        RT = self.sb([128, 128], F32, "RT", st)
        self.dma(RT[:], I["ropeRT"].ap(), reads=[I["ropeRT"]], writes=[RT])
        xs = [self.sb([128, ch], F32, "qx", st) for _ in range(3)]
        sqs = [self.sb([128, ch], F32, "qsq", st) for _ in range(3)]
        tmps = [self.sb([128, ch], F32, "qtmp", st) for _ in range(3)]
        rstds = [self.sb([128, ch], F32, "qrstd", st) for _ in range(3)]
        nn = [self.sb([128, ch], F32, "qn", st) for _ in range(3)]
        t1s = [self.sb([128, ch], F32, "qt1", st) for _ in range(3)]
        t2s = [self.sb([128, ch], F32, "qt2", st) for _ in range(3)]
        ob = [self.sb([128, ch], BF16, "qo", st) for _ in range(3)]
        cs = [self.sb([128, 2, ch], F32, "qcs", st) for _ in range(2)]
        p1 = [self.ps([128, ch], F32, "qp1", st) for _ in range(2)]
        p2 = [self.ps([128, ch], F32, "qp2", st) for _ in range(2)]
        n = 0
        for c in range(S // ch):
            if sm.rope:
                cst = cs[c % 2]
                self.dma(cst[:, 0, :], I["ropeC"][:, c * ch:(c + 1) * ch], reads=[], writes=[cst])
                self.dma(cst[:, 1, :], I["ropeS"][:, c * ch:(c + 1) * ch], reads=[], pwrites=[cst])
            for hh in range(10):
                isq = hh < 8
                src = sm.d["qTraw"] if isq else sm.d["kTraw"]
                r0 = hh * 128 if isq else (hh - 8) * 128
                x = xs[n % 3]
                nb = nn[n % 3]
                o = ob[n % 3]
                sq, tmp, rstd, t1, t2 = sqs[n % 3], tmps[n % 3], rstds[n % 3], t1s[n % 3], t2s[n % 3]
                pa = p1[n % 2]
                pb = p2[n % 2]
                self.dma(x[:], src[r0:r0 + 128, c * ch:(c + 1) * ch], reads=[src], writes=[x])
                self.act(sq[:], x[:], AF.Square, reads=[x], writes=[sq])
                self.mm(pa, pa[:], self.onesF[:], sq[:], True, True, reads=[sq, self.onesF])
                self.rstd_from_ss(rstd[:], pa[:], 128.0, tmp[:], [pa], tmp, rstd)
                g = qkg[:, 0:1] if isq else qkg[:, 1:2]
                self.stt(nb[:], x[:], g, rstd[:], ALU.mult, ALU.mult, reads=[x, qkg, rstd], writes=[nb])
                if sm.rope:
                    self.mm(pb, pb[:], RT[:], nb[:], True, True, reads=[RT, nb])
                    self.tt(t1[:], nb[:], cst[:, 0, :], ALU.mult, reads=[nb, cst], writes=[t1], eng="gpsimd")
                    self.tt(t2[:], pb[:], cst[:, 1, :], ALU.mult, reads=[pb, cst], writes=[t2])
                    self.tt(o[:], t1[:], t2[:], ALU.add, reads=[t1, t2], writes=[o])
                else:
                    self.cp(o[:], nb[:], reads=[nb], writes=[o])
                if isq:
                    self.dma(sm.d["qrT"][r0:r0 + 128, c * ch:(c + 1) * ch], o[:], reads=[o], pwrites=[sm.d["qrT"]])
                else:
                    k0 = sm.koff + c * ch
                    self.dma(self.krT[r0:r0 + 128, k0:k0 + ch], o[:], reads=[o], pwrites=[self.krT])
                n += 1
        self.barrier()
        st.close()

    def phase_attn(self, sm):
        S = sm.S
        ch = min(512, S)
        k0, k1 = sm.keys
        nk = k1 - k0
        nkt = nk // 128
        st = contextlib.ExitStack()
        KT = self.sb([128, nk], BF16, "KT", st)
        V = self.sb([128, nkt, 128], BF16, "V", st)
        Qc = [self.sb([128, ch], BF16, "Qc", st) for _ in range(2)]
        pT = [self.sb([128, ch], BF16, "pT", st) for _ in range(3)]
        rden = self.sb([128, ch], F32, "rden", st)
        accA = [self.sb([128, ch], F32, "accA", st) for _ in range(2)]
        accB = [self.sb([128, ch], F32, "accB", st) for _ in range(2)]
        yo = [self.sb([128, ch], BF16, "yo", st) for _ in range(2)]
        ps_s = [self.ps([128, ch], F32, "ps_s", st) for _ in range(3)]
        ps_o = [self.ps([128, ch], F32, "ps_o", st) for _ in range(2)]
        ps_d = [self.ps([128, ch], F32, "ps_d", st) for _ in range(2)]
        scale = 128.0 ** -0.5
        nq = 0
        ns = 0
        for kv in range(2):
            self.dma(KT[:], self.krT[kv * 128:(kv + 1) * 128, k0:k1], reads=[self.krT], writes=[KT])
            self.dma(V[:], self.av[k0:k1, kv * 128:(kv + 1) * 128].rearrange("(t p) d -> p t d", p=128), reads=[self.av], writes=[V])
            for g in range(4):
                h = kv * 4 + g
                for c in range(S // ch):
                    q = Qc[nq % 2]
                    po = ps_o[nq % 2]
                    pd = ps_d[nq % 2]
                    y = yo[nq % 2]
                    self.dma(q[:], sm.d["qrT"][h * 128:(h + 1) * 128, c * ch:(c + 1) * ch], reads=[sm.d["qrT"]], writes=[q])
                    prev = None
                    aA, aB = accA[nq % 2], accB[nq % 2]
                    nA = nB = 0
                    for kt in range(nkt + 1):
                        cur_pt = None
                        if kt < nkt:
                            psx = ps_s[ns % 3]
                            cur_pt = pT[ns % 3]
                            ns += 1
                            self.mm(psx, psx[:], KT[:, kt * 128:(kt + 1) * 128], q[:], True, True, reads=[KT, q])
                            self.act(cur_pt[:], psx[:], AF.Exp, reads=[psx], writes=[cur_pt], scale=scale)
                        if prev is not None:
                            pk, ppt = prev
                            self.mm(po, po[:], V[:, pk, :], ppt[:], pk == 0, pk == nkt - 1, reads=[V, ppt])
                            if pk % 3 == 2 or (nkt < 3 and pk == 1):
                                if nB == 0:
                                    self.cp(aB[:], ppt[:], reads=[ppt], writes=[aB], eng="gpsimd")
                                else:
                                    self.tt(aB[:], aB[:], ppt[:], ALU.add, reads=[aB, ppt], writes=[aB], eng="gpsimd")
                                nB += 1
                            else:
                                if nA == 0:
                                    self.cp(aA[:], ppt[:], reads=[ppt], writes=[aA])
                                else:
                                    self.tt(aA[:], aA[:], ppt[:], ALU.add, reads=[aA, ppt], writes=[aA])
                                nA += 1
                        prev = (kt, cur_pt) if kt < nkt else None
                    self.mm(pd, pd[:], self.onesF[:], aA[:], True, nB == 0, reads=[self.onesF, aA])
                    if nB > 0:
                        self.mm(pd, pd[:], self.onesF[:], aB[:], False, True, reads=[self.onesF, aB])
                    self.op("vector", lambda e: e.reciprocal(out=rden[:], in_=pd[:]), reads=[pd], writes=[rden])
                    self.tt(y[:], po[:], rden[:], ALU.mult, reads=[po, rden], writes=[y])
                    self.dma(sm.d["yattT"][h * 128:(h + 1) * 128, c * ch:(c + 1) * ch], y[:], reads=[y], pwrites=[sm.d["yattT"]])
                    nq += 1
        self.barrier()
        st.close()

    def phase_gla(self, l, sm):
        I = self.inp
        S = sm.S
        ch = min(512, S)
        nch = S // 64
        ntile = S // 128
        st = contextlib.ExitStack()
        d = sm.d
        X = self.sb([64, S], F32, "gX", st)
        L = self.sb([64, S], F32, "gL", st)
        CUM = self.sb([64, S], F32, "gCUM", st)
        E = self.sb([64, S], F32, "gE", st)
        maskR = self.sb([64, S], F32, "gmaskR", st)
        ktf = self.sb([64, S], BF16, "gktf", st)
        self.op("gpsimd", lambda e: e.memset(maskR[:], 1.0), writes=[maskR])
        self.op("gpsimd", lambda e: e.memset(maskR[:].rearrange("p (c j) -> p c j", j=64)[:, :, 0:1], 0.0), reads=[maskR], writes=[maskR])
        qd = [self.sb([64, S], BF16, "gqd", st) for _ in range(2)]
        ki = [self.sb([64, S], BF16, "gki", st) for _ in range(2)]
        kteT = [self.sb([128, ntile, 64], BF16, "gkteT", st) for _ in range(2)]
        dec = self.sb([64, nch], F32, "gdec", st)
        Sbf = [self.sb([64, nch, 128], BF16, "gSbf", st) for _ in range(2)]
        Sst = [self.sb([64, 128], F32, "gSst", st) for _ in range(2)]
        Vh = self.sb([128, ntile, 128], BF16, "gVh", st)
        wa2 = self.sb([16, 2, 256], F32, "gwa2", st)
        nba = self.sb([64, 2, 4], F32, "gnba", st)
        gg = self.sb([128, 1], F32, "ggn", st)
        mk = self.sb([128, 2, 128], F32, "gmk", st)
        gat = [self.sb([16, ch], F32, "ggat", st) for _ in range(2)]
        am = [self.sb([128, 128], BF16, "gam", st) for _ in range(4)]
        og = [self.sb([128, ch], BF16, "gog", st) for _ in range(2)]
        sg = self.sb([128, ch], F32, "gsg", st)
        osq = self.sb([128, ch], F32, "gosq", st)
        tmp = self.sb([128, ch], F32, "gtmp", st)
        rstd = self.sb([128, ch], F32, "grstd", st)
        t1 = self.sb([128, ch], F32, "gt1", st)
        yb = [self.sb([128, ch], BF16, "gyb", st) for _ in range(2)]
        ptr = self.ps([128, 512], BF16, "gptr", st)
        pcs = [self.ps([64, 4, 128], F32, "gpcs", st) for _ in range(2)]
        pa = [self.ps([128, 128], F32, "gpa", st) for _ in range(2)]
        po = self.ps([128, ch], F32, "gpo", st)
        pss = self.ps([128, ch], F32, "gpss", st)
        pz = [pss, po]
        for dd in range(2):
            self.dma(wa2[:, dd, :], I["gla_wa2"][l, dd], reads=[], pwrites=[wa2])
        self.dma(nba[:], I["gla_baT"][l], reads=[], writes=[nba])
        self.ts(nba[:], nba[:], -1.0, None, ALU.mult, None, reads=[nba], writes=[nba])
        self.dma(gg[:], I["glag"][l], reads=[], writes=[gg])
        self.dma(mk[:], I["gla_mask"].ap(), reads=[], writes=[mk])
        nz = 0
        import os
        cut = int(os.environ.get("GLA_CUT", "99"))

        def bail():
            self.barrier()
            st.close()
        for h in range(4):
            self.dma(Vh[:], d["gv"][:, h * 128:(h + 1) * 128].rearrange("(t p) v -> p t v", p=128), reads=[d["gv"]], writes=[Vh])
            for dd in range(2):
                for c in range(S // ch):
                    ga = gat[nz % 2]
                    p = pz[nz % 2]
                    nz += 1
                    self.dma(ga[:], d["gaT"][dd * 16:(dd + 1) * 16, c * ch:(c + 1) * ch], reads=[d["gaT"]], writes=[ga])
                    self.mm(p, p[0:64, :], wa2[:, dd, h * 64:(h + 1) * 64], ga[:], True, True, reads=[wa2, ga])
                    self.act(E[:, c * ch:(c + 1) * ch], p[0:64, :], AF.Exp, reads=[p, nba], pwrites=[E], scale=-1.0, bias=nba[:, dd, h:h + 1])
                    self.act(L[:, c * ch:(c + 1) * ch], E[:, c * ch:(c + 1) * ch], AF.Ln, reads=[E], pwrites=[L], bias=1.0)
                if cut == 2:
                    return bail()
                self.op("vector", lambda e: e.tensor_tensor_scan(out=CUM[:], data0=maskR[:], data1=L[:], initial=0.0, op0=ALU.mult, op1=ALU.add),
                        reads=[maskR, L], writes=[CUM])
                C3 = CUM[:].rearrange("p (c j) -> p c j", j=64)
                END = C3[:, :, 63:64]
                ENDb = END.to_broadcast([64, nch, 64])
                if dd == 0:
                    ARG = CUM
                else:
                    self.tt(L[:], L[:], CUM[:], ALU.subtract, reads=[L, CUM], writes=[L])
                    L3 = L[:].rearrange("p (c j) -> p c j", j=64)
                    self.tt(L3, L3, ENDb, ALU.add, reads=[L, CUM], writes=[L])
                    ARG = L
                A3 = ARG[:].rearrange("p (c j) -> p c j", j=64)
                self.dma(X[:], d["gqT"][h * 64:(h + 1) * 64, :], reads=[d["gqT"]], writes=[X])
                self.act(E[:], ARG[:], AF.Exp, reads=[ARG], writes=[E], scale=-1.0 / 16)
                self.stt(qd[dd][:], X[:], 0.125, E[:], ALU.mult, ALU.mult, reads=[X, E], writes=[qd[dd]])
                self.dma(X[:], d["gkT"][h * 64:(h + 1) * 64, :], reads=[d["gkT"]], writes=[X])
                self.act(E[:], ARG[:], AF.Exp, reads=[ARG], writes=[E], scale=1.0 / 16)
                self.tt(ki[dd][:], X[:], E[:], ALU.mult, reads=[X, E], writes=[ki[dd]])
                E3 = E[:].rearrange("p (c j) -> p c j", j=64)
                self.tt(E3, ENDb, A3, ALU.subtract, reads=[CUM, ARG], writes=[E])
                self.act(E[:], E[:], AF.Exp, reads=[E], writes=[E], scale=-1.0 / 16)
                self.tt(ktf[:], X[:], E[:], ALU.mult, reads=[X, E], writes=[ktf])
                if cut == 3:
                    return bail()
                for t0 in range(0, ntile, 8):
                    nt = min(8, ntile - t0)
                    for j in range(nt):
                        tt_ = t0 + j
                        self.tr(ptr, ptr[:, j * 64:(j + 1) * 64], ktf[:, tt_ * 128:(tt_ + 1) * 128], self.identB[0:64, 0:64], j == 0, reads=[ktf, self.identB])
                    self.cp(kteT[dd][:, t0:t0 + nt, :], ptr[:, 0:nt * 64].rearrange("p (t k) -> p t k", k=64), reads=[ptr],
                            writes=[kteT[dd]] if t0 == 0 else [], pwrites=[] if t0 == 0 else [kteT[dd]])
                if cut == 4:
                    return bail()
                self.act(dec[:].unsqueeze(2), END, AF.Exp, reads=[CUM], writes=[dec], scale=-1.0 / 16)
                cur = 0
                self.cp(Sst[0][:], sm.gin[:, h, dd, :], reads=[sm.gin], writes=[Sst[0]])
                order = list(range(nch)) if dd == 0 else list(range(nch - 1, -1, -1))
                for i0 in range(0, nch, 4):
                    grp = order[i0:i0 + 4]
                    gi = (i0 // 4) % 2
                    cnt = [0, 0]
                    slots = {}
                    for c in grp:
                        par = c % 2
                        slot = gi * 2 + cnt[par]
                        cnt[par] += 1
                        slots[c] = (pcs[par], slot)
                        tt_, pb = c // 2, par * 64
                        self.mm(pcs[par], pcs[par][:, slot, :], kteT[dd][pb:pb + 64, tt_, :], Vh[pb:pb + 64, tt_, :], True, True, reads=[kteT[dd], Vh])
                    for c in grp:
                        pc, slot = slots[c]
                        self.cp(Sbf[dd][:, c, :], Sst[cur][:], reads=[Sst[cur]], pwrites=[Sbf[dd]], eng="scalar")
                        self.stt(Sst[1 - cur][:], Sst[cur][:], dec[:, c:c + 1], pc[:, slot, :], ALU.mult, ALU.add,
                                 reads=[Sst[cur], dec, pc], writes=[Sst[1 - cur]])
                        cur = 1 - cur
                if sm.gout is not None:
                    self.cp(sm.gout[:, h, dd, :], Sst[cur][:], reads=[Sst[cur]], pwrites=[sm.gout])
            if cut == 5:
                return bail()
            npair = 0
            for c in range(S // ch):
                ntp = ch // 128
                for j in range(ntp):
                    tp = c * ntp + j
                    ams = []
                    for dd in range(2):
                        a = am[(npair % 2) * 2 + dd]
                        p = pa[dd]
                        self.mm(p, p[:], ki[dd][:, tp * 128:(tp + 1) * 128], qd[dd][:, tp * 128:(tp + 1) * 128], True, True, reads=[ki[dd], qd[dd]])
                        self.tt(a[:], p[:], mk[:, dd, :], ALU.mult, reads=[p, mk], writes=[a])
                        ams.append(a)
                    npair += 1
                    for cc in range(2):
                        cidx = tp * 2 + cc
                        reg = po[:, j * 128 + cc * 64:j * 128 + cc * 64 + 64]
                        for dd in range(2):
                            self.mm(po, reg, Vh[:, tp, :], ams[dd][:, cc * 64:(cc + 1) * 64], dd == 0, False, reads=[Vh, ams[dd]])
                            self.mm(po, reg, Sbf[dd][:, cidx, :], qd[dd][:, cidx * 64:(cidx + 1) * 64], False, dd == 1, reads=[Sbf[dd], qd[dd]])
                ogt = og[c % 2]
                y = yb[c % 2]
                self.dma(ogt[:], d["ogT"][h * 128:(h + 1) * 128, c * ch:(c + 1) * ch], reads=[d["ogT"]], writes=[ogt])
                self.act(sg[:], ogt[:], AF.Silu, reads=[ogt], writes=[sg])
                self.act(osq[:], po[:], AF.Square, reads=[po], writes=[osq])
                self.mm(pss, pss[:], self.onesF[:], osq[:], True, True, reads=[self.onesF, osq])
                self.rstd_from_ss(rstd[:], pss[:], 128.0, tmp[:], [pss], tmp, rstd)
                self.tt(t1[:], po[:], rstd[:], ALU.mult, reads=[po, rstd], writes=[t1])
                self.stt(y[:], t1[:], gg[:, 0:1], sg[:], ALU.mult, ALU.mult, reads=[t1, gg, sg], writes=[y])
                self.dma(d["yglaT"][h * 128:(h + 1) * 128, c * ch:(c + 1) * ch], y[:], reads=[y], pwrites=[d["yglaT"]])
        self.barrier()
        st.close()

    def phase_hyena(self, l, sm):
        I = self.inp
        S = sm.S
        n = S
        ntile = n // 128
        nkt = ntile
        d = sm.d
        sfx = "L" if n == SEQ else "C"
        TC, TS = I["dftC" + sfx], I["dftS" + sfx]
        N2 = 2 * n
        st = contextlib.ExitStack()
        ch = min(512, n)
        zemb = self.sb([33, n], F32, "hzemb", st)
        self.dma(zemb[:], I["zemb" + sfx].ap(), reads=[], writes=[zemb])
        w1 = self.sb([33, 64], F32, "hw1", st)
        w2 = self.sb([64, 64], F32, "hw2", st)
        w3 = self.sb([64, 2048], F32, "hw3", st)
        pv = self.sb([64, 4], F32, "hpv", st)
        self.dma(w1[:], I["hy_pos_w1"][l], reads=[], writes=[w1])
        self.dma(w2[:], I["hy_pos_w2"][l], reads=[], writes=[w2])
        self.dma(w3[:], I["hy_pos_w3"][l], reads=[], writes=[w3])
        self.dma(pv[:], I["hy_pv"][l], reads=[], writes=[pv])
        fb = self.sb([64, 2], F32, "hfb", st)
        self.tt(fb[:, 0:1], pv[:, 0:1], pv[:, 1:2], ALU.mult, reads=[pv], writes=[fb])
        self.tt(fb[:, 1:2], pv[:, 2:3], pv[:, 1:2], ALU.mult, reads=[pv], pwrites=[fb])
        hid = [self.sb([64, n], F32, "hhid", st) for _ in range(2)]
        u = self.sb([64, ch], F32, "hu", st)
        kf = self.sb([64, ch], F32, "hkf", st)
        kint = self.sb([64, ch], I32, "hki", st)
        php = [self.ps([64, ch], F32, "hph", st) for _ in range(2)]
        TWO_PI = 2.0 * math.pi
        for layer_i in range(2):
            wsb = w1 if layer_i == 0 else w2
            src = zemb if layer_i == 0 else hid[0]
            K = 33 if layer_i == 0 else 64
            for c in range(n // ch):
                p = php[c % 2]
                self.mm(p, p[:], wsb[0:K, :], src[0:K, c * ch:(c + 1) * ch], True, True, reads=[wsb, src])
                self.ts(u[:], p[:], pv[:, 1:2], fb[:, layer_i:layer_i + 1], ALU.mult, ALU.add, reads=[p, pv, fb], writes=[u])
                self.ts(kf[:], u[:], 1.0 / TWO_PI, None, ALU.mult, None, reads=[u], writes=[kf])
                self.cp(kint[:], kf[:], reads=[kf], writes=[kint])
                self.cp(kf[:], kint[:], reads=[kint], writes=[kf])
                self.stt(u[:], kf[:], -TWO_PI, u[:], ALU.mult, ALU.add, reads=[kf, u], writes=[u])
                self.ts(kf[:], u[:], math.pi, -TWO_PI, ALU.is_gt, ALU.mult, reads=[u], writes=[kf])
                self.tt(u[:], u[:], kf[:], ALU.add, reads=[u, kf], writes=[u])
                self.ts(kf[:], u[:], -math.pi, TWO_PI, ALU.is_lt, ALU.mult, reads=[u], writes=[kf])
                self.tt(u[:], u[:], kf[:], ALU.add, reads=[u, kf], writes=[u])
                self.ts(u[:], u[:], math.pi, -math.pi, ALU.min, ALU.max, reads=[u], writes=[u])
                self.act(hid[layer_i][:, c * ch:(c + 1) * ch], u[:], AF.Sin, reads=[u], pwrites=[hid[layer_i]])
        hid2 = hid[1]
        absd = self.sb([128, 2048], F32, "habsd", st)
        self.dma(absd[:], I["hy_decay"][l].partition_broadcast(128), reads=[], writes=[absd])
        self.act(absd[:], absd[:], AF.Abs, reads=[absd], writes=[absd])
        negtn = self.sb([128, ntile], F32, "hnegtn", st)
        self.dma(negtn[:], I["negtn" + sfx].ap(), reads=[], writes=[negtn])
        wins = [self.sb([128, 2048], F32, "hwin", st) for _ in range(2)]
        tapss = [self.sb([128, 2048], F32, "htaps", st) for _ in range(2)]
        ataps = [self.sb([128, 2048], F32, "hatap", st) for _ in range(2)]
        fs_t = [self.sb([128, 2, 512], BF16, "hfst", st) for _ in range(2)]
        fd_t = [self.sb([128, 2, 512], BF16, "hfdt", st) for _ in range(2)]
        pf = [self.ps([128, 512], F32, "hpf", st) for _ in range(2)]
        pl1 = [self.ps([128, 512], F32, "hpl1", st) for _ in range(4)]
        for tt_ in range(ntile):
            win, taps, atap = wins[tt_ % 2], tapss[tt_ % 2], ataps[tt_ % 2]
            self.act(win[:], absd[:], AF.Exp, reads=[absd, negtn], writes=[win], scale=negtn[:, tt_:tt_ + 1])
            for b in range(4):
                p = pf[b % 2]
                self.mm(p, p[:], hid2[:, tt_ * 128:(tt_ + 1) * 128], w3[:, b * 512:(b + 1) * 512], True, True, reads=[hid2, w3])
                self.tt(taps[:, b * 512:(b + 1) * 512], p[:], win[:, b * 512:(b + 1) * 512], ALU.mult, reads=[p, win],
                        writes=[taps] if b == 0 else [], pwrites=[] if b == 0 else [taps])
            if tt_ == 0:
                t4 = taps[0:1, :].rearrange("p (o d c) -> p o d c", o=2, d=2)
                self.op("vector", lambda e: e.memset(t4[:, :, 1, :], 0.0), reads=[taps], pwrites=[taps])
            self.act(atap[:], taps[:], AF.Abs, reads=[taps], writes=[atap])
            for b in range(4):
                self.mm(pl1[b], pl1[b][:], self.onesF[:], atap[:, b * 512:(b + 1) * 512], tt_ == 0, tt_ == ntile - 1, reads=[self.onesF, atap])
            T4 = taps[:].rearrange("p (o d c) -> p o d c", o=2, d=2)
            fs_ = fs_t[tt_ % 2]
            fd_ = fd_t[tt_ % 2]
            self.tt(fs_[:], T4[:, :, 0, :], T4[:, :, 1, :], ALU.add, reads=[taps], writes=[fs_])
            self.tt(fd_[:], T4[:, :, 0, :], T4[:, :, 1, :], ALU.subtract, reads=[taps], writes=[fd_], eng="gpsimd")
            self.dma(d["fsd"][tt_ * 128:(tt_ + 1) * 128, :], fs_[:].rearrange("p o c -> p (o c)"), reads=[fs_], pwrites=[d["fsd"]])
            self.dma(d["fdd"][tt_ * 128:(tt_ + 1) * 128, :], fd_[:].rearrange("p o c -> p (o c)"), reads=[fd_], pwrites=[d["fdd"]])
        linv = self.linv
        l1 = self.sb([128, 2048], F32, "hl1", st)
        for b in range(4):
            self.cp(l1[:, b * 512:(b + 1) * 512], pl1[b][:], reads=[pl1[b]], writes=[l1] if b == 0 else [], pwrites=[] if b == 0 else [l1])
        L4 = l1[:].rearrange("p (o d c) -> p o d c", o=2, d=2)
        self.tt(linv[:], L4[:, :, 0, :], L4[:, :, 1, :], ALU.add, reads=[l1], writes=[linv])
        self.ts(linv[:], linv[:], EPS, None, ALU.add, None, reads=[linv], writes=[linv])
        self.op("vector", lambda e: e.reciprocal(out=linv[:], in_=linv[:]), reads=[linv], writes=[linv])
        self.barrier()
        st.close()

        st = contextlib.ExitStack()
        wk = self.sb([128, nkt], F32, "hwk", st)
        self.dma(wk[:], I["wk" + sfx].ap(), reads=[], writes=[wk])
        alt = self.sb([128, 2], BF16, "halt", st)
        self.dma(alt[:], I["altcol"].ap(), reads=[], writes=[alt])
        altrow = self.sb([1, 128], BF16, "haltrow", st)
        self.dma(altrow[:], I["altrow"].ap(), reads=[], writes=[altrow])
        tcb = [self.sb([128, ntile, 128], BF16, "htc", st) for _ in range(2)]
        tsb = [self.sb([128, ntile, 128], BF16, "hts", st) for _ in range(2)]
        fsb = self.sb([128, ntile, 512], BF16, "hfsb", st)
        fdb = self.sb([128, ntile, 512], BF16, "hfdb", st)
        pre = [self.ps([128, 512], F32, "hpre", st) for _ in range(2)]
        pim = [self.ps([128, 512], F32, "hpim", st) for _ in range(2)]
        pny = self.ps([1, 512], F32, "hpny", st)
        fo = [self.sb([128, 2, 512], F32, "hfo", st) for _ in range(2)]
        fn = self.sb([1, 512], F32, "hfn", st)
        cw = self.sb([128, 4, 1536], F32, "hcw", st)
        for j in range(3):
            self.dma(cw[:, j, :], I["hy_conv_w"][l, j].partition_broadcast(128), reads=[], writes=[cw] if j == 0 else [], pwrites=[] if j == 0 else [cw])
        self.dma(cw[:, 3, :], I["hy_conv_b"][l].partition_broadcast(128), reads=[], pwrites=[cw])
        us = [self.sb([128, 3, 1536], BF16, "hus", st) for _ in range(2)]
        a1s = [self.sb([128, 1536], F32, "ha1", st) for _ in range(2)]
        a2s = [self.sb([128, 1536], F32, "ha2", st) for _ in range(2)]
        a3s = [self.sb([128, 1536], F32, "ha3", st) for _ in range(2)]
        ucb = [self.sb([128, 1536], BF16, "hucb", st) for _ in range(2)]

        def sc_step(tt_):
            u_ = us[tt_ % 2]
            uo = ucb[tt_ % 2]
            for j in range(3):
                self.dma(u_[:, j, :], d["hyu"][tt_ * 128 + j:tt_ * 128 + j + 128, :], reads=[d["hyu"]], writes=[u_] if j == 0 else [], pwrites=[] if j == 0 else [u_])
            a1, a2, a3 = a1s[tt_ % 2], a2s[tt_ % 2], a3s[tt_ % 2]
            self.tt(a1[:], u_[:, 0, :], cw[:, 0, :], ALU.mult, reads=[u_, cw], writes=[a1])
            self.tt(a2[:], u_[:, 1, :], cw[:, 1, :], ALU.mult, reads=[u_, cw], writes=[a2], eng="gpsimd")
            self.tt(a3[:], u_[:, 2, :], cw[:, 2, :], ALU.mult, reads=[u_, cw], writes=[a3], eng="gpsimd")
            self.tt(a1[:], a1[:], cw[:, 3, :], ALU.add, reads=[a1, cw], writes=[a1])
            self.tt(a2[:], a2[:], a3[:], ALU.add, reads=[a2, a3], writes=[a2], eng="gpsimd")
            self.tt(uo[:], a1[:], a2[:], ALU.add, reads=[a1, a2], writes=[uo])
            self.dma(d["ucd"][tt_ * 128:(tt_ + 1) * 128, :], uo[:], reads=[uo], pwrites=[d["ucd"]])
        sc_todo = list(range(ntile))
        sc_every = max(1, (2 * nkt) // ntile)
        sc_iter = 0
        nld = 0
        for o in range(2):
            self.dma(fsb[:], d["fsd"][:, o * 512:(o + 1) * 512].rearrange("(t p) c -> p t c", p=128), reads=[d["fsd"]], writes=[fsb])
            self.dma(fdb[:], d["fdd"][:, o * 512:(o + 1) * 512].rearrange("(t p) c -> p t c", p=128), reads=[d["fdd"]], writes=[fdb])
            for kt in range(nkt):
                sc_iter += 1
                if sc_todo and sc_iter % sc_every == 0:
                    sc_step(sc_todo.pop(0))
                tc_, ts_ = tcb[nld % 2], tsb[nld % 2]
                pr, pi = pre[nld % 2], pim[nld % 2]
                fo_ = fo[nld % 2]
                nld += 1
                self.dma(tc_[:], TC[kt], reads=[], writes=[tc_])
                self.dma(ts_[:], TS[kt], reads=[], writes=[ts_])
                for tt_ in range(ntile):
                    self.mm(pr, pr[:], tc_[:, tt_, :], fsb[:, tt_, :], tt_ == 0, tt_ == ntile - 1, reads=[tc_, fsb])
                for tt_ in range(ntile):
                    self.mm(pi, pi[:], ts_[:, tt_, :], fdb[:, tt_, :], tt_ == 0, tt_ == ntile - 1, reads=[ts_, fdb])
                self.stt(fo_[:, 0, :], pr[:], wk[:, kt:kt + 1], linv[:, o, :], ALU.mult, ALU.mult, reads=[pr, wk, linv], writes=[fo_])
                self.stt(fo_[:, 1, :], pi[:], wk[:, kt:kt + 1], linv[:, o, :], ALU.mult, ALU.mult, reads=[pi, wk, linv], pwrites=[fo_])
                self.dma(d["Fd"][o, :, kt * 128:(kt + 1) * 128, :].rearrange("r p c -> p r c"), fo_[:], reads=[fo_], pwrites=[d["Fd"]])
            for tt_ in range(ntile):
                self.mm(pny, pny[:], alt[:, 0:1], fsb[:, tt_, :], tt_ == 0, tt_ == ntile - 1, reads=[alt, fsb])
            self.stt(fn[:], pny[:], 1.0 / N2, linv[0:1, o, :], ALU.mult, ALU.mult, reads=[pny, linv], writes=[fn])
            self.dma(d["Fnyq"][o:o + 1, :], fn[:], reads=[fn], pwrites=[d["Fnyq"]])
        while sc_todo:
            sc_step(sc_todo.pop(0))
        self.barrier()
        st.close()

        st = contextlib.ExitStack()
        alt = self.sb([128, 2], BF16, "halt", st)
        self.dma(alt[:], I["altcol"].ap(), reads=[], writes=[alt])
        altrow = self.sb([1, 128], BF16, "haltrow", st)
        self.dma(altrow[:], I["altrow"].ap(), reads=[], writes=[altrow])
        skb = self.sb([128, 2, 512], F32, "hskb", st)
        self.dma(skb[:].rearrange("p o c -> p (o c)"), I["hy_skip"][l].partition_broadcast(128), reads=[], writes=[skb])
        zsb = self.sb([128, ntile, 512], BF16, "hzsb", st)
        Yre = self.sb([128, nkt, 512], BF16, "hYre", st)
        Ys = self.sb([128, nkt, 512], BF16, "hYs", st)
        Yn = self.sb([1, 512], BF16, "hYn", st)
        fn = self.sb([1, 512], F32, "hfn2", st)
        tcb = [self.sb([128, ntile, 128], BF16, "htc2", st) for _ in range(2)]
        tsb = [self.sb([128, ntile, 128], BF16, "hts2", st) for _ in range(2)]
        Ft = [self.sb([128, 2, 512], F32, "hFt", st) for _ in range(2)]
        m = [self.sb([128, 512], F32, "hm", st) for _ in range(4)]
        gt = [self.sb([128, 512], BF16, "hgt", st) for _ in range(2)]
        zo = [self.sb([128, 512], BF16, "hzo", st) for _ in range(2)]
        pre = [self.ps([128, 512], F32, "cpre", st) for _ in range(2)]
        pim = [self.ps([128, 512], F32, "cpim", st) for _ in range(2)]
        pny = self.ps([1, 512], F32, "cpny", st)
        py = [self.ps([128, 512], F32, "cpy", st) for _ in range(2)]
        ptr = self.ps([128, 512], BF16, "cptr", st)
        ytr = [self.sb([128, 4, 128], BF16, "hytr", st) for _ in range(2)]
        nld = 0
        for o in range(2):
            zsrc = d["ucd"][:, 1024:1536] if o == 0 else d["z2d"][:, :]
            zdep = d["ucd"] if o == 0 else d["z2d"]
            self.dma(zsb[:], zsrc.rearrange("(t p) c -> p t c", p=128), reads=[zdep], writes=[zsb])
            self.dma(fn[:], d["Fnyq"][o:o + 1, :], reads=[d["Fnyq"]], writes=[fn])
            for kt in range(nkt):
                tc_, ts_ = tcb[nld % 2], tsb[nld % 2]
                pr, pi = pre[nld % 2], pim[nld % 2]
                F_ = Ft[nld % 2]
                nld += 1
                self.dma(tc_[:], TC[kt], reads=[], writes=[tc_])
                self.dma(ts_[:], TS[kt], reads=[], writes=[ts_])
                self.dma(F_[:], d["Fd"][o, :, kt * 128:(kt + 1) * 128, :].rearrange("r p c -> p r c"), reads=[d["Fd"]], writes=[F_])
                for tt_ in range(ntile):
                    self.mm(pr, pr[:], tc_[:, tt_, :], zsb[:, tt_, :], tt_ == 0, tt_ == ntile - 1, reads=[tc_, zsb])
                for tt_ in range(ntile):
                    self.mm(pi, pi[:], ts_[:, tt_, :], zsb[:, tt_, :], tt_ == 0, tt_ == ntile - 1, reads=[ts_, zsb])
                self.tt(m[0][:], pr[:], F_[:, 0, :], ALU.mult, reads=[pr, F_], writes=[m[0]])
                self.tt(m[1][:], pi[:], F_[:, 1, :], ALU.mult, reads=[pi, F_], writes=[m[1]])
                self.tt(Yre[:, kt, :], m[0][:], m[1][:], ALU.subtract, reads=[m[0], m[1]], pwrites=[Yre], eng="gpsimd")
                self.tt(m[2][:], pr[:], F_[:, 1, :], ALU.mult, reads=[pr, F_], writes=[m[2]])
                self.tt(m[3][:], pi[:], F_[:, 0, :], ALU.mult, reads=[pi, F_], writes=[m[3]])
                self.tt(Ys[:, kt, :], m[2][:], m[3][:], ALU.add, reads=[m[2], m[3]], pwrites=[Ys], eng="gpsimd")
            for tt_ in range(ntile):
                self.mm(pny, pny[:], alt[:, 0:1], zsb[:, tt_, :], tt_ == 0, tt_ == ntile - 1, reads=[alt, zsb])
            self.tt(Yn[:], pny[:], fn[:], ALU.mult, reads=[pny, fn], writes=[Yn])
            for tt_ in range(ntile):
                tc_, ts_ = tcb[nld % 2], tsb[nld % 2]
                p = py[nld % 2]
                g_ = gt[nld % 2]
                z_ = zo[nld % 2]
                nld += 1
                self.dma(tc_[:], TC[tt_], reads=[], writes=[tc_])
                self.dma(ts_[:], TS[tt_], reads=[], writes=[ts_])
                self.dma(g_[:], d["ucd"][tt_ * 128:(tt_ + 1) * 128, o * 512:(o + 1) * 512], reads=[d["ucd"]], writes=[g_])
                for kt in range(nkt):
                    self.mm(p, p[:], tc_[:, kt, :], Yre[:, kt, :], kt == 0, False, reads=[tc_, Yre])
                    self.mm(p, p[:], ts_[:, kt, :], Ys[:, kt, :], False, False, reads=[ts_, Ys])
                self.mm(p, p[:], altrow[:, :], Yn[:], False, True, reads=[altrow, Yn])
                self.tt(m[0][:], zsb[:, tt_, :], skb[:, o, :], ALU.mult, reads=[zsb, skb], writes=[m[0]], eng="gpsimd")
                self.tt(m[1][:], p[:], m[0][:], ALU.add, reads=[p, m[0]], writes=[m[1]])
                self.tt(z_[:], m[1][:], g_[:], ALU.mult, reads=[m[1], g_], writes=[z_])
                if o == 0:
                    self.dma(d["z2d"][tt_ * 128:(tt_ + 1) * 128, :], z_[:], reads=[z_], pwrites=[d["z2d"]])
                else:
                    yt = ytr[tt_ % 2]
                    for j in range(4):
                        self.tr(ptr, ptr[:, j * 128:(j + 1) * 128], z_[:, j * 128:(j + 1) * 128], self.identB[:], j == 0, reads=[z_, self.identB])
                    self.cp(yt[:], ptr[:].rearrange("p (j t) -> p j t", j=4), reads=[ptr], writes=[yt], eng="scalar")
                    self.dma(d["yhyT"][:, tt_ * 128:(tt_ + 1) * 128].rearrange("(j p) t -> p j t", p=128), yt[:], reads=[yt], pwrites=[d["yhyT"]])
            self.barrier()
        self.barrier()
        st.close()

    def phase_merge(self, l, sm, xin, xout):
        I = self.inp
        S = sm.S
        ch = min(512, S)
        d = sm.d
        col = sm.col
        st = contextlib.ExitStack()
        wbh = self.sb([128, 4, 1024], BF16, "mwbh", st)
        wbg = self.sb([128, 4, 1024], BF16, "mwbg", st)
        wba = self.sb([128, 8, 1024], BF16, "mwba", st)
        wo = self.sb([128, 8, 1024], BF16, "mwo", st)
        self.wstage(st)
        for wt, nm, kk in ((wbh, "w_br_hy", 4), (wbg, "w_br_gla", 4), (wba, "w_br_att", 8), (wo, "w_out", 8)):
            self.wload(wt, I[nm][l], kk, 1024)
        yh = [self.sb([128, 4, ch], BF16, "myh", st) for _ in range(2)]
        yg = [self.sb([128, 4, ch], BF16, "myg", st) for _ in range(2)]
        ya = [self.sb([128, 8, ch], BF16, "mya", st) for _ in range(2)]
        mT = self.sb([128, 8, ch], BF16, "mmT", st)
        brg = [self.sb([128, 3, ch], BF16, "mbrg", st) for _ in range(2)]
        sigs = [self.sb([128, 3, ch], F32, "msig", st) for _ in range(2)]
        as_ = [self.sb([128, ch], F32, "ma", st) for _ in range(2)]
        bs_ = [self.sb([128, ch], F32, "mb", st) for _ in range(2)]
        cs_ = [self.sb([128, ch], F32, "mc", st) for _ in range(2)]
        xc = [self.sb([128, ch], F32, "mxc", st) for _ in range(2)]
        xn = [self.sb([128, ch], F32, "mxn", st) for _ in range(2)]
        p1l = [self.ps([128, ch], F32, "mp1", st) for _ in range(2)]
        p2l = [self.ps([128, ch], F32, "mp2", st) for _ in range(2)]
        p3l = [self.ps([128, ch], F32, "mp3", st) for _ in range(2)]
        py = [self.ps([128, ch], F32, "mpy", st) for _ in range(2)]
        brv = d["brT"]
        n = 0
        for c in range(S // ch):
            cs = slice(c * ch, (c + 1) * ch)
            yh_, yg_, ya_ = yh[c % 2], yg[c % 2], ya[c % 2]
            self.dma(yh_[:], d["yhyT"][:, cs].rearrange("(k p) t -> p k t", p=128), reads=[d["yhyT"]], writes=[yh_])
            self.dma(yg_[:], d["yglaT"][:, cs].rearrange("(k p) t -> p k t", p=128), reads=[d["yglaT"]], writes=[yg_])
            self.dma(ya_[:], d["yattT"][:, cs].rearrange("(k p) t -> p k t", p=128), reads=[d["yattT"]], writes=[ya_])
            for fc in range(8):
                fs_ = slice(fc * 128, (fc + 1) * 128)
                bg = brg[n % 2]
                p1, p2, p3 = p1l[n % 2], p2l[n % 2], p3l[n % 2]
                n += 1
                self.dma(bg[:], brv[:, cs].rearrange("(j k p) t -> p k j t", j=3, k=8, p=128)[:, fc], reads=[brv], writes=[bg])
                for k in range(4):
                    self.mm(p1, p1[:], wbh[:, k, fs_], yh_[:, k, :], k == 0, k == 3, reads=[wbh, yh_])
                for k in range(4):
                    self.mm(p2, p2[:], wbg[:, k, fs_], yg_[:, k, :], k == 0, k == 3, reads=[wbg, yg_])
                for k in range(8):
                    self.mm(p3, p3[:], wba[:, k, fs_], ya_[:, k, :], k == 0, k == 7, reads=[wba, ya_])
                sig, a, b, c_ = sigs[fc % 2], as_[fc % 2], bs_[fc % 2], cs_[fc % 2]
                self.act(sig[:], bg[:], AF.Sigmoid, reads=[bg], writes=[sig])
                self.tt(a[:], p1[:], sig[:, 0, :], ALU.mult, reads=[p1, sig], writes=[a])
                self.tt(b[:], p2[:], sig[:, 1, :], ALU.mult, reads=[p2, sig], writes=[b])
                self.tt(c_[:], p3[:], sig[:, 2, :], ALU.mult, reads=[p3, sig], writes=[c_])
                self.tt(a[:], a[:], b[:], ALU.add, reads=[a, b], writes=[a], eng="gpsimd")
                self.tt(mT[:, fc, :], a[:], c_[:], ALU.add, reads=[a, c_], writes=[mT] if fc == 0 else [], pwrites=[] if fc == 0 else [mT], eng="gpsimd")
            for fc in range(8):
                fs_ = slice(fc * 128, (fc + 1) * 128)
                p = py[fc % 2]
                x_ = xc[fc % 2]
                xo = xn[fc % 2]
                self.dma(x_[:], xin[fs_, cs], reads=[xin], writes=[x_])
                for k in range(8):
                    self.mm(p, p[:], wo[:, k, fs_], mT[:, k, :], k == 0, k == 7, reads=[wo, mT])
                self.stt(xo[:], p[:], self.modT[:, 16 + fc, col:col + 1], x_[:], ALU.mult, ALU.add, reads=[p, self.modT, x_], writes=[xo])
                self.dma(xout[fs_, cs], xo[:], reads=[xo], pwrites=[xout])
        self.barrier()
        st.close()

    def phase_moe(self, l, sm, xin, xout):
        I = self.inp
        S = sm.S
        col = sm.col
        half = min(2048, S)
        ch = min(512, half)
        ntt = half // 128
        for hb in range(S // half):
            hs = slice(hb * half, (hb + 1) * half)
            st = contextlib.ExitStack()
            h2T = self.sb([128, 8, half], BF16, "eh2T", st)
            lgT = self.sb([128, ntt, 16], F32, "elg", st)
            G = self.sb([128, ntt, 16], F32, "eG", st)
            acc = self.sb([128, 8, half], F32, "eacc", st)
            self.dma(acc[:], xin[:, hs].rearrange("(k p) t -> p k t", p=128), reads=[xin], writes=[acc])
            st2 = contextlib.ExitStack()
            self.phase_norm(_Slice(xin, hs), half, 1, col, h2T, st2, lgT=lgT, chmax=256)
            self.barrier()
            st2.close()
            st2 = contextlib.ExitStack()
            rb = self.sb([128, 16], F32, "erb", st2)
            self.dma(rb[:], I["router_b"].ap().partition_broadcast(128), reads=[], writes=[rb])
            sc = self.sb([128, ntt, 16], F32, "esc", st2)
            sv = self.sb([128, ntt, 16], F32, "esv", st2)
            t = self.sb([128, ntt, 16], F32, "et", st2)
            t2 = self.sb([128, ntt, 16], F32, "et2", st2)
            i1 = self.sb([128, ntt, 16], F32, "ei1", st2)
            i2 = self.sb([128, ntt, 16], F32, "ei2", st2)
            p6 = self.sb([128, ntt * 4, 6], F32, "ep6", st2)
            gs = self.sb([128, ntt, 4], F32, "egs", st2)
            gm = self.sb([128, ntt], F32, "egm", st2)
            ing = self.sb([128, ntt, 4], F32, "eing", st2)
            self.act(sc[:], lgT[:], AF.Sigmoid, reads=[lgT], writes=[sc])
            self.tt(sv[:], sc[:], rb[:].unsqueeze(1).to_broadcast([128, ntt, 16]), ALU.add, reads=[sc, rb], writes=[sv])
            s4 = sv[:].rearrange("p t (g e) -> p (t g) e", e=4)
            self.tt(p6[:, :, 0:3], s4[:, :, 0:3], s4[:, :, 1:4], ALU.add, reads=[sv], writes=[p6])
            self.tt(p6[:, :, 3:5], s4[:, :, 0:2], s4[:, :, 2:4], ALU.add, reads=[sv], pwrites=[p6])
            self.tt(p6[:, :, 5:6], s4[:, :, 0:1], s4[:, :, 3:4], ALU.add, reads=[sv], pwrites=[p6])
            self.op("vector", lambda e: e.tensor_reduce(out=gs[:].rearrange("p t g -> p (t g)"), in_=p6[:], axis=AX.X, op=ALU.max), reads=[p6], writes=[gs])
            self.op("vector", lambda e: e.tensor_reduce(out=gm[:], in_=gs[:], axis=AX.X, op=ALU.max), reads=[gs], writes=[gm])
            self.tt(ing[:], gs[:], gm[:].unsqueeze(2).to_broadcast([128, ntt, 4]), ALU.is_equal, reads=[gs, gm], writes=[ing])
            self.ts(t[:], sv[:], 2.0, None, ALU.add, None, reads=[sv], writes=[t])
            t4 = t[:].rearrange("p t (g e) -> p t g e", e=4)
            self.tt(t4, t4, ing[:].unsqueeze(3).to_broadcast([128, ntt, 4, 4]), ALU.mult, reads=[t, ing], writes=[t])
            self.ts(t[:], t[:], -2.0, None, ALU.add, None, reads=[t], writes=[t])
            self.op("vector", lambda e: e.tensor_reduce(out=gm[:], in_=t[:], axis=AX.X, op=ALU.max), reads=[t], writes=[gm])
            self.tt(i1[:], t[:], gm[:].unsqueeze(2).to_broadcast([128, ntt, 16]), ALU.is_equal, reads=[t, gm], writes=[i1])
            self.stt(t2[:], i1[:], -4.0, t[:], ALU.mult, ALU.add, reads=[i1, t], writes=[t2])
            self.op("vector", lambda e: e.tensor_reduce(out=gm[:], in_=t2[:], axis=AX.X, op=ALU.max), reads=[t2], writes=[gm])
            self.tt(i2[:], t2[:], gm[:].unsqueeze(2).to_broadcast([128, ntt, 16]), ALU.is_equal, reads=[t2, gm], writes=[i2])
            self.tt(i1[:], i1[:], i2[:], ALU.add, reads=[i1, i2], writes=[i1])
            self.tt(t[:], sc[:], i1[:], ALU.mult, reads=[sc, i1], writes=[t])
            self.op("vector", lambda e: e.tensor_reduce(out=gm[:], in_=t[:], axis=AX.X, op=ALU.add), reads=[t], writes=[gm])
            self.op("vector", lambda e: e.reciprocal(out=gm[:], in_=gm[:]), reads=[gm], writes=[gm])
            self.tt(G[:], t[:], gm[:].unsqueeze(2).to_broadcast([128, ntt, 16]), ALU.mult, reads=[t, gm], writes=[G])
            if self.dbg:
                self.dma(sm.d["gates"][hb * ntt * 128:(hb + 1) * ntt * 128, :].rearrange("(t p) e -> p t e", p=128), G[:], reads=[G], pwrites=[sm.d["gates"]])
            self.barrier()
            st2.close()
            self.wstage(st)
            Gx = [self.sb([128, ntt, 128], F32, "eGx", st) for _ in range(1)]
            wg = [self.sb([128, 8, 512], BF16, "ewg", st) for _ in range(2)]
            wu = [self.sb([128, 8, 512], BF16, "ewu", st) for _ in range(2)]
            wd = [self.sb([128, 4, 1024], BF16, "ewd", st) for _ in range(2)]
            gb = [self.sb([128, ch], F32, "egb", st) for _ in range(2)]
            sgl = [self.sb([128, ch], F32, "esgl", st) for _ in range(2)]
            tm = [self.sb([128, ch], F32, "etm", st) for _ in range(2)]
            hid = [self.sb([128, 4, ch], BF16, "ehid", st) for _ in range(2)]
            pgb = self.ps([128, ch], F32, "epgb", st)
            pg = [self.ps([128, ch], F32, "epg", st) for _ in range(2)]
            pu = [self.ps([128, ch], F32, "epu", st) for _ in range(2)]
            pd = [self.ps([128, ch], F32, "epd", st) for _ in range(2)]
            nn_ = 0
            nd = 0
            def esteps(ei):
                pcs_ = []
                for dstT, nm, K_, nc_ in ((wg[ei % 2], "moe_w_gate", 8, 512), (wu[ei % 2], "moe_w_up", 8, 512), (wd[ei % 2], "moe_w_down", 4, 1024)):
                    ksub = max(1, 2048 // nc_)
                    for k0 in range(0, K_, ksub):
                        pcs_.append((dstT, I[nm][l, ei], k0, min(ksub, K_ - k0), nc_))
                views = {}

                def do_dma(i):
                    dstT, src, k0, kn, nc_ = pcs_[i]
                    stg = self._wst[i % 2]
                    sv = stg[:, 0:kn * nc_].rearrange("p (k n) -> p k n", k=kn)
                    views[i] = (stg, sv)
                    self.dma(sv, src[k0 * 128:(k0 + kn) * 128, :].rearrange("(k p) n -> p k n", p=128), reads=[], writes=[stg])

                def do_cast(i):
                    dstT, src, k0, kn, nc_ = pcs_[i]
                    stg, sv = views[i]
                    self.cp(dstT[:, k0:k0 + kn, :nc_], sv, reads=[stg], pwrites=[dstT], eng="scalar")
                steps = []
                n_ = len(pcs_)
                for i in range(n_ + 2):
                    def st_(i=i):
                        if i >= 2:
                            do_cast(i - 2)
                        if i < n_:
                            do_dma(i)
                    steps.append(st_)
                return steps
            for f_ in esteps(0):
                f_()
            nslots = (half // ch) * 4
            for e_ in range(NEXP):
                g_, u_, d_ = wg[e_ % 2], wu[e_ % 2], wd[e_ % 2]
                nxt = esteps(e_ + 1) if e_ + 1 < NEXP else []
                per_slot = -(-len(nxt) // nslots) if nxt else 0
                gx = Gx[0]
                self.cp(gx[:], G[:, :, e_:e_ + 1].to_broadcast([128, ntt, 128]), reads=[G], writes=[gx])
                for c in range(half // ch):
                    cs = slice(c * ch, (c + 1) * ch)
                    gb_ = gb[c % 2]
                    hd = hid[c % 2]
                    for j in range(ch // 128):
                        self.mm(pgb, pgb[:, j * 128:(j + 1) * 128], gx[:, c * (ch // 128) + j, :], self.identF[:], True, True, reads=[gx, self.identF])
                    self.cp(gb_[:], pgb[:], reads=[pgb], writes=[gb_], eng="scalar")
                    for dc in range(4):
                        for _ in range(per_slot):
                            if nxt:
                                nxt.pop(0)()
                        ds_ = slice(dc * 128, (dc + 1) * 128)
                        a_, b_ = pg[nn_ % 2], pu[nn_ % 2]
                        s_, t_ = sgl[nn_ % 2], tm[nn_ % 2]
                        nn_ += 1
                        for k in range(8):
                            self.mm(a_, a_[:], g_[:, k, ds_], h2T[:, k, cs], k == 0, k == 7, reads=[g_, h2T])
                        for k in range(8):
                            self.mm(b_, b_[:], u_[:, k, ds_], h2T[:, k, cs], k == 0, k == 7, reads=[u_, h2T])
                        self.act(s_[:], a_[:], AF.Silu, reads=[a_], writes=[s_])
                        self.tt(t_[:], b_[:], s_[:], ALU.mult, reads=[b_, s_], writes=[t_])
                        self.tt(hd[:, dc, :], t_[:], gb_[:], ALU.mult, reads=[t_, gb_], writes=[hd] if dc == 0 else [], pwrites=[] if dc == 0 else [hd], eng="gpsimd")
                    for fc in range(8):
                        p_ = pd[nd % 2]
                        nd += 1
                        for dc in range(4):
                            self.mm(p_, p_[:], d_[:, dc, fc * 128:(fc + 1) * 128], hd[:, dc, :], dc == 0, dc == 3, reads=[d_, hd])
                        self.stt(acc[:, fc, cs], p_[:], self.modT[:, 40 + fc, col:col + 1], acc[:, fc, cs], ALU.mult, ALU.add,
                                 reads=[p_, self.modT, acc], pwrites=[acc])
                while nxt:
                    nxt.pop(0)()
            self.dma(xout[:, hs].rearrange("(k p) t -> p k t", p=128), acc[:], reads=[acc], pwrites=[xout])
            self.barrier()
            st.close()

    def phase_xpose_out(self, xT, out, S):
        st = contextlib.ExitStack()
        xs = [self.sb([128, 8, 128], F32, "oxs", st) for _ in range(2)]
        stg = [self.sb([128, 1024], F32, "ostg", st) for _ in range(2)]
        pt = [self.ps([128, 512], F32, "opt", st) for _ in range(4)]
        for tt in range(S // 128):
            x = xs[tt % 2]
            sg = stg[tt % 2]
            self.dma(x[:], xT[:, tt * 128:(tt + 1) * 128].rearrange("(k p) t -> p k t", p=128), reads=[xT], writes=[x])
            for half in range(2):
                p = pt[(tt * 2 + half) % 4]
                for k in range(4):
                    self.tr(p, p[:, k * 128:(k + 1) * 128], x[:, half * 4 + k, :], self.identF[:], k == 0, reads=[x, self.identF])
                self.cp(sg[:, half * 512:(half + 1) * 512], p[:], reads=[p], writes=[sg] if half == 0 else [], pwrites=[] if half == 0 else [sg],
                        eng="vector" if half == 0 else "scalar")
            self.dma(out[tt * 128:(tt + 1) * 128, :], sg[:], reads=[sg], pwrites=[out])
        self.barrier()
        st.close()


class _Slice:
    def __init__(self, t, cols):
        self.t = t
        self.buf = t.buf
        self.cols = cols

    def __getitem__(self, idx):
        r, c = idx
        base = self.cols.start
        c2 = slice(base + (c.start or 0), base + c.stop)
        return self.t[r, c2]


class Stream:
    pass


def build_program(dbg=False, stop_after=None, layers=(0, 1)):
    nc = bass.Bass("TRN2", target_bir_lowering=False)
    P = MK(nc, dbg=dbg)
    din = P.din
    din("x", [SEQ, D]); din("ctx", [CTX, D]); din("cT", [128, 8, 2])
    din("w_mod", [2, D, 6 * D]); din("b_modT", [2, 128, 48]); din("g1T", [2, 128, 8]); din("g2T", [2, 128, 8])
    din("w_in", [2, D, NIN]); din("qkg", [2, 128, 2])
    din("gla_wa2", [2, 2, 16, 256]); din("gla_baT", [2, 64, 2, 4]); din("glag", [2, 128, 1]); din("gla_mask", [128, 2, 128])
    din("ropeC", [128, SEQ]); din("ropeS", [128, SEQ]); din("ropeRT", [128, 128])
    din("hy_pos_w1", [2, 33, 64]); din("hy_pos_w2", [2, 64, 64]); din("hy_pos_w3", [2, 64, 2048]); din("hy_pv", [2, 64, 4])
    din("hy_decay", [2, 2048]); din("hy_skip", [2, 1024]); din("hy_conv_w", [2, 3, 1536]); din("hy_conv_b", [2, 1536])
    din("zembL", [33, SEQ]); din("zembC", [33, CTX]); din("negtnL", [128, 32]); din("negtnC", [128, 2])
    din("wkL", [128, 32]); din("wkC", [128, 2])
    din("dftCL", [32, 128, 32, 128], BF16); din("dftSL", [32, 128, 32, 128], BF16)
    din("dftCC", [2, 128, 2, 128], BF16); din("dftSC", [2, 128, 2, 128], BF16)
    din("altcol", [128, 2], BF16); din("altrow", [1, 128], BF16)
    din("w_br_hy", [2, 512, D]); din("w_br_gla", [2, 512, D]); din("w_br_att", [2, D, D]); din("w_out", [2, D, D])
    din("router_w", [D, 16]); din("router_b", [16]); din("moe_sel", [16, 16, 128])
    din("moe_w_gate", [2, 16, D, DE]); din("moe_w_up", [2, 16, D, DE]); din("moe_w_down", [2, 16, DE, D])
    out = P.dram("out", [SEQ, D], F32, kind="ExternalOutput")

    P.setup_consts()
    P.modT = P.sb([128, 48, 2], F32, "modT")
    P.AA = P.sb([128, 2, 8, 2], F32, "AA")
    P.linv = P.sb([128, 2, 512], F32, "linv")
    g0 = P.sb([64, 4, 2, 128], F32, "gstate0")
    gc = P.sb([64, 4, 2, 128], F32, "gstatec")
    P.op("vector", lambda e: e.memset(g0[:], 0.0), writes=[g0])
    P.krT = P.dscr("krT", [256, SEQ + CTX], BF16)
    P.av = P.dscr("av", [SEQ + CTX, 256], BF16)
    streams = []
    for nm, S, col in (("c", CTX, 1), ("l", SEQ, 0)):
        sm = Stream()
        sm.S, sm.col, sm.nm = S, col, nm
        sm.rope = nm == "l"
        sm.koff = 0 if nm == "l" else SEQ
        sm.keys = (0, SEQ + CTX) if nm == "l" else (SEQ, SEQ + CTX)
        sm.gin = gc if nm == "l" else g0
        sm.gout = None if nm == "l" else gc
        dd = {}
        for k, shp, dt in (("xTa", [D, S], F32), ("xTb", [D, S], F32), ("kTraw", [256, S], F32), ("qTraw", [1024, S], F32),
                           ("gkT", [256, S], F32), ("gqT", [256, S], F32), ("gaT", [32, S], F32), ("ogT", [512, S], BF16),
                           ("brT", [3072, S], BF16), ("gv", [S, 512], BF16), ("hyu", [S + 2, 1536], BF16), ("qrT", [1024, S], BF16),
                           ("yattT", [1024, S], BF16), ("yglaT", [512, S], BF16), ("yhyT", [512, S], BF16),
                           ("fsd", [S, 1024], BF16), ("fdd", [S, 1024], BF16), ("Fd", [2, 2, S, 512], F32), ("Fnyq", [2, 512], F32),
                           ("ucd", [S, 1536], BF16), ("z2d", [S, 512], BF16), ("gates", [S, 16], F32)):
            dd[k] = P.dscr(f"{nm}_{k}", shp, dt)
        sm.d = dd
        streams.append(sm)
    ctxs, lat = streams

    def done(tag):
        return stop_after == tag

    def finish():
        P.finish()
        return nc, P

    P.phase_xpose_in(P.inp["ctx"], ctxs.d["xTa"], CTX)
    P.phase_xpose_in(P.inp["x"], lat.d["xTa"], SEQ)
    if done("xpose"):
        return finish()
    for l in layers:
        last = l == DEPTH - 1
        P.phase_mods(l)
        for sm in (ctxs, lat):
            S = sm.S
            d = sm.d
            st = contextlib.ExitStack()
            hT = P.sb([128, 8, S], BF16, "hT", st)
            st2 = contextlib.ExitStack()
            P.phase_norm(d["xTa"], S, 0, sm.col, hT, st2)
            P.barrier()
            st2.close()
            if P.dbg and l == layers[0]:
                hdbg = P.dscr(f"{sm.nm}_hT", [D, S], BF16)
                P.dma(hdbg.ap().rearrange("(k p) t -> p k t", p=128), hT[:], reads=[hT], writes=[hdbg])
            win = P.inp["w_in"][l]
            zr = P.sb([1, 1536], BF16, "zr", st)
            P.op("vector", lambda e: e.memset(zr[:], 0.0), writes=[zr])
            P.dma(d["hyu"][0:1, :], zr[:], reads=[zr], pwrites=[d["hyu"]])
            P.dma(d["hyu"][S + 1:S + 2, :], zr[:], reads=[zr], pwrites=[d["hyu"]])
            kv_only = last and sm is ctxs
            for (c0, ncol, key, dt) in ((0, 256, "kTraw", F32), (512, 256, "gkT", F32), (1280, 32, "gaT", F32), (1312, 1024, "qTraw", F32),
                                        (2336, 256, "gqT", F32), (2592, 512, "ogT", BF16), (4640, 3072, "brT", BF16)):
                if kv_only and key in ("qTraw", "gqT", "ogT", "brT"):
                    continue
                P.linear_fm(win, 8, c0, ncol, hT, S, d[key], 0, dt)
            P.linear_tm(win, 8, 256, 256, hT, S, P.av, sm.koff, 0, BF16)
            P.linear_tm(win, 8, 768, 512, hT, S, d["gv"], 0, 0, BF16)
            if not kv_only:
                P.linear_tm(win, 8, 3104, 1536, hT, S, d["hyu"], 1, 0, BF16)
            P.barrier()
            st.close()
            if done(sm.nm + ":proj"):
                return finish()
            P.phase_qkprep(l, sm)
            if done(sm.nm + ":qkprep"):
                return finish()
            P.phase_gla(l, sm)
            if done(sm.nm + ":gla"):
                return finish()
            if last and sm is ctxs:
                continue
            P.phase_attn(sm)
            if done(sm.nm + ":attn"):
                return finish()
            P.phase_hyena(l, sm)
            if done(sm.nm + ":hyena"):
                return finish()
            P.phase_merge(l, sm, d["xTa"], d["xTb"])
            if done(sm.nm + ":merge"):
                return finish()
            P.phase_moe(l, sm, d["xTb"], d["xTa"])
            if done(sm.nm + ":moe"):
                return finish()
    P.phase_xpose_out(lat.d["xTa"], out, SEQ)
    return finish()


def host_consts():
    c = {}
    f32 = np.float32
    bf = ml_dtypes.bfloat16
    t = np.arange(SEQ)
    row = (t // 64).astype(f32)
    colv = (t % 64).astype(f32)
    inv = (np.float32(10000.0) ** (-np.arange(32, dtype=f32) / np.float32(32))).astype(f32)
    ang = np.concatenate([row[:, None] * inv[None, :], colv[:, None] * inv[None, :]], axis=-1).astype(f32)
    pidx = np.arange(128) // 2
    c["ropeC"] = np.ascontiguousarray(np.cos(ang)[:, pidx].T.astype(f32))
    c["ropeS"] = np.ascontiguousarray(np.sin(ang)[:, pidx].T.astype(f32))
    RT = np.zeros((128, 128), f32)
    for i in range(64):
        RT[2 * i + 1, 2 * i] = -1.0
        RT[2 * i, 2 * i + 1] = 1.0
    c["ropeRT"] = RT
    s = np.arange(128)[:, None]
    q = np.arange(128)[None, :]
    same = (s // 64) == (q // 64)
    mk = np.zeros((128, 2, 128), f32)
    mk[:, 0, :] = (same & (s <= q)).astype(f32)
    mk[:, 1, :] = (same & (s >= q)).astype(f32)
    c["gla_mask"] = mk
    for sfx, n in (("L", SEQ), ("C", CTX)):
        tt = np.arange(n, dtype=f32)
        tn = (tt / np.float32(n)).astype(f32)
        bands = np.linspace(1e-4, 15, 16, dtype=f32)
        phase = (np.float32(2 * math.pi / n) * tt[:, None] * bands[None, :]).astype(f32)
        z = np.concatenate([tn[:, None], np.cos(phase), -np.sin(phase)], axis=-1).astype(f32)
        c["zemb" + sfx] = np.ascontiguousarray(z.T)
        nt = n // 128
        c["negtn" + sfx] = np.ascontiguousarray((-tn).reshape(nt, 128).T)
        N2 = 2 * n
        wk = np.full(n, 2.0 / N2, f32)
        wk[0] = 1.0 / N2
        c["wk" + sfx] = np.ascontiguousarray(wk.reshape(nt, 128).T)
        idx = np.arange(n, dtype=np.int64)
        prod = (idx[:, None] * idx[None, :]) % N2
        angd = prod.astype(np.float64) * (2 * math.pi / N2)
        for nm, fn in (("dftC", np.cos), ("dftS", np.sin)):
            M = fn(angd).astype(f32)
            T4 = M.reshape(nt, 128, nt, 128).transpose(2, 1, 0, 3)
            c[nm + sfx] = np.ascontiguousarray(T4).astype(bf)
    alt = np.where(np.arange(128) % 2 == 0, 1.0, -1.0).astype(f32)
    c["altcol"] = np.stack([alt, alt], axis=1).astype(bf)
    c["altrow"] = alt[None, :].astype(bf)
    sel = np.zeros((16, 16, 128), f32)
    for e in range(16):
        sel[e, e, :] = 1.0
    c["moe_sel"] = sel
    return c


_CONSTS = None


def host_inputs(inp):
    global _CONSTS
    if _CONSTS is None:
        _CONSTS = host_consts()
    f32 = np.float32
    g = {k: np.asarray(v) for k, v in inp.items()}
    sh = dict(_CONSTS)
    for k in ("w_mod", "w_in", "gla_wa2", "hy_pos_w1", "hy_pos_w2", "hy_pos_w3", "hy_conv_w", "hy_conv_b", "w_br_hy", "w_br_gla",
              "w_br_att", "w_out", "router_w", "router_b", "moe_w_gate", "moe_w_up", "moe_w_down"):
        sh[k] = np.ascontiguousarray(g[k], dtype=f32)
    sh["b_modT"] = np.ascontiguousarray(g["b_mod"].reshape(2, 48, 128).transpose(0, 2, 1))
    sh["g1T"] = np.ascontiguousarray(g["norm1_g"].reshape(2, 8, 128).transpose(0, 2, 1))
    sh["g2T"] = np.ascontiguousarray(g["norm2_g"].reshape(2, 8, 128).transpose(0, 2, 1))
    sh["qkg"] = np.ascontiguousarray(np.stack([g["q_norm_g"], g["k_norm_g"]], axis=-1))
    sh["gla_baT"] = np.ascontiguousarray(g["gla_ba"].reshape(2, 2, 4, 64).transpose(0, 3, 1, 2))
    sh["glag"] = np.ascontiguousarray(g["gla_norm_g"].reshape(2, 128, 1))
    pv = np.zeros((2, 64, 4), f32)
    pv[:, :, 0] = g["hy_pos_b1"]
    pv[:, :, 1] = g["hy_sin_freq"]
    pv[:, :, 2] = g["hy_pos_b2"]
    sh["hy_pv"] = pv
    sh["hy_decay"] = np.ascontiguousarray(g["hy_decay"].reshape(2, 2048))
    sh["hy_skip"] = np.ascontiguousarray(g["hy_skip"].reshape(2, 1024))
    return sh, g


def core_inputs(sh, g, b):
    m = dict(sh)
    m["x"] = np.ascontiguousarray(g["x"][b], dtype=np.float32)
    m["ctx"] = np.ascontiguousarray(g["ctx"][b], dtype=np.float32)
    cT = np.stack([g["c"][b].reshape(8, 128).T, g["c_ctx"].reshape(8, 128).T], axis=-1)
    m["cT"] = np.ascontiguousarray(cT, dtype=np.float32)
    return m


def kernel(**inputs):
    sh, g = host_inputs(inputs)
    nc, P = build_program()
    in_maps = [core_inputs(sh, g, b) for b in range(8)]
    res = run_bass_kernel_spmd(nc, in_maps, core_ids=list(range(8)))
    return np.stack([np.asarray(r["out"]) for r in res.results], axis=0).astype(np.float32)
```

```python
import contextlib
import math
import numpy as np
import ml_dtypes
import concourse.bass as bass
import concourse.mybir as mybir
from concourse.bass_utils import run_bass_kernel_spmd

F32 = mybir.dt.float32
BF16 = mybir.dt.bfloat16
I32 = mybir.dt.int32
AF = mybir.ActivationFunctionType
ALU = mybir.AluOpType
AX = mybir.AxisListType

D = 1024
SEQ = 4096
CTX = 256
DEPTH = 2
NIN = 7712
NKV = 1312
EPS = 1e-6
NEXP = 16
DE = 512
HYW = 512

SEM_LIMIT = 30000


class Buf:
    def __init__(self, name):
        self.name = name
        self.writers = {}
        self.readers = {}
        self.prev = {}


class T:
    def __init__(self, t, name, dram=False):
        self.t = t
        self.buf = Buf(name)
        self.name = name
        self.dram = dram
        self.view = None

    def __getitem__(self, idx):
        if self.dram:
            return self.t.ap()[idx]
        if self.view is not None:
            return self.view[idx]
        return self.t[idx]

    def ap(self):
        return self.t.ap() if self.dram else self.t[:]


class Eng:
    def __init__(self, P, name, handle):
        self.P = P
        self.name = name
        self.h = handle
        self.sem = None
        self.count = 0
        self.seen = {}
        self.nsem = 0

    def new_sem(self):
        self.sem = self.P.alloc_sem(f"{self.name}{self.nsem}")
        self.nsem += 1
        self.count = 0


class Prog:
    def __init__(self, nc):
        self.nc = nc
        self.stack = contextlib.ExitStack()
        self.eng = {}
        for n in ("tensor", "vector", "scalar", "gpsimd", "sync"):
            e = Eng(self, n, getattr(nc, n))
            self.eng[n] = e
        self.nsems = 0
        for e in self.eng.values():
            e.new_sem()
        self.dma_sems = []
        self.dma_rr = 0
        for i in range(24):
            self.dma_sems.append([self.alloc_sem(f"dma{i}"), 0, i])
        self.ndma_gen = 24
        self.all_tokens = {}
        self.uid = 0
        self.pending = []
        self.max_pending = 2

    def alloc_sem(self, name):
        self.nsems += 1
        return self.stack.enter_context(self.nc.semaphore(name))

    def sb(self, shape, dtype, name=None, stack=None):
        self.uid += 1
        nm = f"{name or 't'}_{self.uid}"
        t = (stack or self.stack).enter_context(self.nc.sbuf_tensor(nm, list(shape), dtype))
        return T(t, nm)

    def ps(self, shape, dtype=F32, name=None, stack=None):
        self.uid += 1
        nm = f"{name or 'p'}_{self.uid}"
        full = 512 if dtype == F32 else 1024
        t = (stack or self.stack).enter_context(self.nc.psum_tensor(nm, [128, full], dtype))
        free = 1
        for d_ in shape[1:]:
            free *= d_
        assert free <= full
        v = t[0:shape[0], 0:free]
        if len(shape) == 3:
            v = v.rearrange("p (a b) -> p a b", a=shape[1])
        r = T(t, nm)
        r.view = v
        return r

    def dram(self, name, shape, dtype, kind="Internal"):
        t = self.nc.dram_tensor(name, list(shape), dtype, kind=kind)
        return T(t, name, dram=True)

    def _need(self, E, toks):
        for key, (sem, val) in toks.items():
            if E.seen.get(key, 0) < val:
                E.h.wait_ge(sem, val)
                E.seen[key] = val

    def _deps(self, E, reads, writes, pwrites, skip_same=False):
        need = {}

        def add(d):
            for k, (s, v) in d.items():
                if skip_same and k == id(E.sem):
                    continue
                if k not in need or need[k][1] < v:
                    need[k] = (s, v)
        for b in reads:
            add(b.writers)
        for b in writes:
            add(b.writers)
            add(b.readers)
        for b in pwrites:
            add(b.readers)
            add(b.prev)
        self._need(E, need)

    def _commit(self, tok, reads, writes, pwrites):
        k = id(tok[0])
        for b in writes:
            pv = dict(b.writers)
            for kk, vv in b.readers.items():
                if kk not in pv or pv[kk][1] < vv[1]:
                    pv[kk] = vv
            b.prev = pv
            b.writers = {k: tok}
            b.readers = {}
        for b in pwrites:
            b.writers[k] = tok
        for b in reads:
            b.readers[k] = tok
        self.all_tokens[k] = tok

    def op(self, en, fn, reads=(), writes=(), pwrites=()):
        E = self.eng[en]
        reads = [getattr(r, "buf", r) for r in reads]
        writes = [getattr(r, "buf", r) for r in writes]
        pwrites = [getattr(r, "buf", r) for r in pwrites]
        if E.count >= SEM_LIMIT:
            E.new_sem()
        if self.pending and self._pending_conflict(writes + pwrites, ()):
            self.flush_stores()
        self._deps(E, reads, writes, pwrites, skip_same=(en == "tensor"))
        ins = fn(E.h)
        E.count += 1
        ins.then_inc(E.sem, 1)
        tok = (E.sem, E.count)
        E.seen[id(E.sem)] = max(E.seen.get(id(E.sem), 0), 0)
        self._commit(tok, reads, writes, pwrites)
        return ins

    def _pending_conflict(self, bufs_w, bufs_r):
        if not self.pending:
            return False
        for ent in self.pending:
            src, dst = ent[7], ent[8]
            for b in bufs_w:
                if id(b) in src or id(b) in dst:
                    return True
            for b in bufs_r:
                if id(b) in dst:
                    return True
        return False

    def flush_stores(self, keep=0):
        while len(self.pending) > keep:
            ent = self.pending.pop(0)
            self._dma_emit(*ent[:7])

    def dma(self, out, in_, reads=(), writes=(), pwrites=(), q="sync", **kw):
        is_store = any(getattr(r, "dram", False) for r in list(writes) + list(pwrites)) and not any(getattr(r, "dram", False) for r in reads)
        reads = [getattr(r, "buf", r) for r in reads]
        writes = [getattr(r, "buf", r) for r in writes]
        pwrites = [getattr(r, "buf", r) for r in pwrites]
        if is_store:
            src = {id(b) for b in reads}
            dst = {id(b) for b in writes + pwrites}
            self.pending.append((out, in_, reads, writes, pwrites, q, kw, src, dst))
            self.flush_stores(keep=self.max_pending)
            return None
        if self._pending_conflict(writes + pwrites, reads):
            self.flush_stores()
        return self._dma_emit(out, in_, reads, writes, pwrites, q, kw)

    def _dma_emit(self, out, in_, reads, writes, pwrites, q, kw):
        E = self.eng[q]
        slot = self.dma_sems[self.dma_rr]
        self.dma_rr = (self.dma_rr + 1) % len(self.dma_sems)
        if slot[1] + 16 > SEM_LIMIT:
            self._need(E, {id(slot[0]): (slot[0], slot[1])})
            slot[0] = self.alloc_sem(f"dma{self.ndma_gen}")
            self.ndma_gen += 1
            slot[1] = 0
        sem = slot[0]
        if slot[1] > 0:
            self._need(E, {id(sem): (sem, slot[1])})
        self._deps(E, reads, writes, pwrites)
        ins = E.h.dma_start(out=out, in_=in_, **kw)
        slot[1] += 16
        ins.then_inc(sem, 16)
        tok = (sem, slot[1])
        self._commit(tok, reads, writes, pwrites)
        return ins

    def barrier(self):
        self.flush_stores()
        for E in self.eng.values():
            self._need(E, dict(self.all_tokens))

    def finish(self):
        self.barrier()
        self.stack.close()


def _rr(lst, i):
    return lst[i % len(lst)]


class MK(Prog):
    def __init__(self, nc, dbg=False, layers=(0, 1), phases=None):
        super().__init__(nc)
        self.dbg = dbg
        self.layers = layers
        self.phases = phases
        self.inp = {}
        self.scr = {}

    def din(self, name, shape, dtype=F32):
        t = self.dram(name, shape, dtype, kind="ExternalInput")
        self.inp[name] = t
        return t

    def dscr(self, name, shape, dtype):
        t = self.dram(name, shape, dtype, kind="ExternalOutput" if self.dbg else "Internal")
        self.scr[name] = t
        return t

    def mm(self, ps, out, lhsT, rhs, first, last, reads):
        self.op("tensor", lambda e: e.matmul(out, lhsT, rhs, start=first, stop=last), reads=reads,
                writes=[ps] if first else [], pwrites=[] if first else [ps])

    def tr(self, ps, out, in_, ident, first, reads):
        self.op("tensor", lambda e: e.transpose(out, in_, ident), reads=reads,
                writes=[ps] if first else [], pwrites=[] if first else [ps])

    def act(self, out, in_, func, reads, writes=(), pwrites=(), **kw):
        self.op("scalar", lambda e: e.activation(out=out, in_=in_, func=func, **kw), reads=reads, writes=writes, pwrites=pwrites)

    def tt(self, out, in0, in1, op, reads, writes=(), pwrites=(), eng="vector"):
        self.op(eng, lambda e: e.tensor_tensor(out=out, in0=in0, in1=in1, op=op), reads=reads, writes=writes, pwrites=pwrites)

    def ts(self, out, in0, s1, s2, op0, op1, reads, writes=(), pwrites=(), eng="vector"):
        if op1 is None:
            self.op(eng, lambda e: e.tensor_scalar(out=out, in0=in0, scalar1=s1, scalar2=None, op0=op0), reads=reads, writes=writes, pwrites=pwrites)
        else:
            self.op(eng, lambda e: e.tensor_scalar(out=out, in0=in0, scalar1=s1, scalar2=s2, op0=op0, op1=op1), reads=reads, writes=writes, pwrites=pwrites)

    def stt(self, out, in0, scalar, in1, op0, op1, reads, writes=(), pwrites=()):
        self.op("vector", lambda e: e.scalar_tensor_tensor(out=out, in0=in0, scalar=scalar, in1=in1, op0=op0, op1=op1), reads=reads, writes=writes, pwrites=pwrites)

    def cp(self, out, in_, reads, writes=(), pwrites=(), eng="vector"):
        if eng == "scalar":
            self.op("scalar", lambda e: e.copy(out=out, in_=in_), reads=reads, writes=writes, pwrites=pwrites)
        else:
            self.op(eng, lambda e: e.tensor_copy(out=out, in_=in_), reads=reads, writes=writes, pwrites=pwrites)

    def wstage(self, st):
        self._wst = [self.sb([128, 2048], F32, "wst", st) for _ in range(2)]
        self._wsti = 0

    def wload(self, dstT, src, K, ncols):
        ksub = max(1, 2048 // ncols)
        for k0 in range(0, K, ksub):
            kn = min(ksub, K - k0)
            stg = self._wst[self._wsti % 2]
            self._wsti += 1
            sv = stg[:, 0:kn * ncols].rearrange("p (k n) -> p k n", k=kn)
            self.dma(sv, src[k0 * 128:(k0 + kn) * 128, :].rearrange("(k p) n -> p k n", p=128), reads=[], writes=[stg])
            first = k0 == 0
            self.cp(dstT[:, k0:k0 + kn, :ncols], sv, reads=[stg], writes=[dstT] if first else [], pwrites=[] if first else [dstT], eng="gpsimd")

    def rstd_from_ss(self, out, ps_ap, n, tmp_ap, reads, tmpT, outT):
        self.act(tmp_ap, ps_ap, AF.Sqrt, reads=reads, writes=[tmpT], scale=1.0 / n, bias=self.epsc[:, 0:1])
        self.op("vector", lambda e: e.reciprocal(out=out, in_=tmp_ap), reads=[tmpT], writes=[outT])

    def setup_consts(self):
        c = {}
        self.identF = self.sb([128, 128], F32, "identF")
        self.identB = self.sb([128, 128], BF16, "identB")
        self.onesF = self.sb([128, 128], F32, "onesF")
        self.onesB = self.sb([128, 128], BF16, "onesB")
        self.epsc = self.sb([128, 1], F32, "epsc")
        self.op("vector", lambda e: e.memset(self.epsc[:], EPS), writes=[self.epsc])
        self.op("gpsimd", lambda e: e.memset(self.identF[:], 1.0), writes=[self.identF])
        self.op("gpsimd", lambda e: e.affine_select(out=self.identF[:], in_=self.identF[:], pattern=[[-1, 128]],
                                                     compare_op=ALU.is_equal, fill=0.0, base=0, channel_multiplier=1),
                reads=[self.identF], writes=[self.identF])
        self.cp(self.identB[:], self.identF[:], reads=[self.identF], writes=[self.identB])
        self.op("vector", lambda e: e.memset(self.onesF[:], 1.0), writes=[self.onesF])
        self.op("vector", lambda e: e.memset(self.onesB[:], 1.0), writes=[self.onesB])

    def phase_mods(self, l):
        I = self.inp
        st = contextlib.ExitStack()
        scT = self.sb([128, 8, 2], F32, "scT", st)
        self.dma(scT[:], I["cT"].ap(), reads=[I["cT"]], writes=[scT])
        self.act(scT[:], scT[:], AF.Silu, reads=[scT], writes=[scT])
        wm = [self.sb([128, 8, 512], F32, "wm", st) for _ in range(2)]
        pm = self.ps([128, 96], F32, "pm", st)
        bm = self.sb([128, 48], F32, "bm", st)
        gg = self.sb([128, 2, 8], F32, "gg", st)
        self.dma(bm[:], I["b_modT"][l], reads=[I["b_modT"]], writes=[bm])
        self.dma(gg[:, 0, :], I["g1T"][l], reads=[I["g1T"]], pwrites=[gg])
        self.dma(gg[:, 1, :], I["g2T"][l], reads=[I["g2T"]], pwrites=[gg])
        first = True
        for ob in range(12):
            w = wm[ob % 2]
            self.dma(w[:], I["w_mod"][l, :, ob * 512:(ob + 1) * 512].rearrange("(k p) n -> p k n", p=128),
                     reads=[I["w_mod"]], writes=[w])
            for j in range(4):
                oc = ob * 4 + j
                for kc in range(8):
                    self.mm(pm, pm[:, oc * 2:oc * 2 + 2], w[:, kc, j * 128:(j + 1) * 128], scT[:, kc, :],
                            kc == 0, kc == 7, reads=[w, scT])
        modT = self.modT
        self.tt(modT[:], pm[:].rearrange("p (c t) -> p c t", t=2), bm[:].unsqueeze(2).to_broadcast([128, 48, 2]), ALU.add,
                reads=[pm, bm], writes=[modT])
        AA = self.AA
        for i, sc0 in ((0, 8), (1, 32)):
            self.ts(AA[:, i], modT[:, sc0:sc0 + 8, :], 1.0, None, ALU.add, None, reads=[modT], pwrites=[AA])
            self.tt(AA[:, i], AA[:, i], gg[:, i, :].unsqueeze(2).to_broadcast([128, 8, 2]), ALU.mult, reads=[AA, gg], pwrites=[AA])
        self.barrier()
        st.close()

    def phase_xpose_in(self, src, dstT, S):
        st = contextlib.ExitStack()
        xs = [self.sb([128, 1024], F32, "xs", st) for _ in range(2)]
        stg = [self.sb([128, 8, 128], F32, "stg", st) for _ in range(2)]
        pt = [self.ps([128, 512], F32, "pt", st) for _ in range(4)]
        for tt in range(S // 128):
            x = xs[tt % 2]
            sg = stg[tt % 2]
            self.dma(x[:], src[tt * 128:(tt + 1) * 128, :], reads=[src], writes=[x])
            for half in range(2):
                p = pt[(tt * 2 + half) % 4]
                for k in range(4):
                    kk = half * 4 + k
                    self.tr(p, p[:, k * 128:(k + 1) * 128], x[:, kk * 128:(kk + 1) * 128], self.identF[:], k == 0, reads=[x, self.identF])
                self.cp(sg[:, half * 4:(half + 1) * 4, :], p[:].rearrange("p (k n) -> p k n", k=4), reads=[p],
                        writes=[sg] if half == 0 else [], pwrites=[] if half == 0 else [sg], eng="vector" if half == 0 else "scalar")
            self.dma(dstT[:, tt * 128:(tt + 1) * 128].rearrange("(k p) t -> p k t", p=128), sg[:], reads=[sg], pwrites=[dstT])
        self.barrier()
        st.close()

    def phase_norm(self, xT, S, which, col, hT, st, lgT=None, chmax=512):
        A = self.AA
        B0 = 0 if which == 0 else 24
        ch = min(chmax, S)
        xc = [self.sb([128, 8, ch], F32, "xc", st) for _ in range(2)]
        sq = self.sb([128, 8, ch], F32, "sq", st)
        hf = [self.sb([128, 8, ch], F32, "hf", st) for _ in range(2)]
        tmp = self.sb([128, ch], F32, "ntmp", st)
        rstd = self.sb([128, ch], F32, "rstd", st)
        pss = [self.ps([128, ch], F32, "pss", st) for _ in range(2)]
        if lgT is not None:
            rw = self.sb([128, 8, 16], F32, "rw", st)
            self.dma(rw[:], self.inp["router_w"].ap().rearrange("(k p) e -> p k e", p=128), reads=[self.inp["router_w"]], writes=[rw])
            psr = [self.ps([128, (ch // 128) * 16], F32, "psr", st) for _ in range(2)]
        for c in range(S // ch):
            x = xc[c % 2]
            h = hf[c % 2]
            p = pss[c % 2]
            self.dma(x[:], xT[:, c * ch:(c + 1) * ch].rearrange("(k p) t -> p k t", p=128), reads=[xT], writes=[x])
            self.act(sq[:], x[:], AF.Square, reads=[x], writes=[sq])
            for kc in range(8):
                self.mm(p, p[:], self.onesF[:], sq[:, kc, :], kc == 0, kc == 7, reads=[sq, self.onesF])
            self.rstd_from_ss(rstd[:], p[:], 1024.0, tmp[:], [p], tmp, rstd)
            for kc in range(8):
                self.tt(h[:, kc, :], x[:, kc, :], rstd[:], ALU.mult, reads=[x, rstd], writes=[h] if kc == 0 else [], pwrites=[] if kc == 0 else [h])
                self.act(h[:, kc, :], h[:, kc, :], AF.Identity, reads=[h, A, self.modT], pwrites=[h],
                         scale=A[:, which, kc, col:col + 1], bias=self.modT[:, B0 + kc, col:col + 1])
            self.cp(hT[:, :, c * ch:(c + 1) * ch], h[:], reads=[h], pwrites=[hT], eng="gpsimd")
            if lgT is not None:
                pr = psr[c % 2]
                nj = ch // 128
                for j in range(nj):
                    for kc in range(8):
                        self.mm(pr, pr[:, j * 16:(j + 1) * 16], h[:, kc, j * 128:(j + 1) * 128], rw[:, kc, :], kc == 0, kc == 7, reads=[rw, h])
                self.cp(lgT[:, c * nj:(c + 1) * nj, :], pr[:].rearrange("p (j e) -> p j e", e=16), reads=[pr], pwrites=[lgT], eng="scalar")

    def linear_fm(self, wsrc, wrow_chunks, col0, ncols, acts, S, dst, drow0, evac_dt, st_outer=None, wtiles=None):
        st = contextlib.ExitStack()
        K = wrow_chunks
        ch = min(512, S)
        wb = [self.sb([128, K, 512], BF16, "wb", st) for _ in range(2)]
        self.wstage(st)
        stg = [self.sb([128, ch], evac_dt, "lstg", st) for _ in range(3)]
        pp = [self.ps([128, ch], F32, "lps", st) for _ in range(3)]
        n = 0
        blocks = list(range(0, ncols, 512))
        self.wload(wb[0], wsrc[:, col0:col0 + min(512, ncols)], K, min(512, ncols))
        for bi, b0 in enumerate(blocks):
            bw = min(512, ncols - b0)
            w = wb[bi % 2]
            if bi + 1 < len(blocks):
                nb0 = blocks[bi + 1]
                nbw = min(512, ncols - nb0)
                self.wload(wb[(bi + 1) % 2], wsrc[:, col0 + nb0:col0 + nb0 + nbw], K, nbw)
            for m0 in range(0, bw, 128):
                msz = min(128, bw - m0)
                for c in range(S // ch):
                    p = pp[n % 3]
                    sg = stg[n % 3]
                    for kc in range(K):
                        self.mm(p, p[:msz, :], w[:, kc, m0:m0 + msz], acts[:, kc, c * ch:(c + 1) * ch], kc == 0, kc == K - 1, reads=[w, acts])
                    self.cp(sg[:msz, :], p[:msz, :], reads=[p], writes=[sg], eng="vector" if n % 2 == 0 else "scalar")
                    r0 = drow0 + b0 + m0
                    self.dma(dst[r0:r0 + msz, c * ch:(c + 1) * ch], sg[:msz, :], reads=[sg], pwrites=[dst])
                    n += 1
        self.barrier()
        st.close()

    def linear_tm(self, wsrc, K, col0, ncols, acts, S, dst, drow0, dcol0, evac_dt):
        st = contextlib.ExitStack()
        wb = [self.sb([128, K, 512], BF16, "wbt", st) for _ in range(2)]
        self.wstage(st)
        stg = [self.sb([128, 512], evac_dt, "tstg", st) for _ in range(3)]
        pp = [self.ps([128, 512], F32, "tps", st) for _ in range(3)]
        n = 0
        blocks = list(range(0, ncols, 512))
        self.wload(wb[0], wsrc[:, col0:col0 + min(512, ncols)], K, min(512, ncols))
        for bi, b0 in enumerate(blocks):
            bw = min(512, ncols - b0)
            w = wb[bi % 2]
            if bi + 1 < len(blocks):
                nb0 = blocks[bi + 1]
                nbw = min(512, ncols - nb0)
                self.wload(wb[(bi + 1) % 2], wsrc[:, col0 + nb0:col0 + nb0 + nbw], K, nbw)
            for tt in range(S // 128):
                p = pp[n % 3]
                sg = stg[n % 3]
                for kc in range(K):
                    self.mm(p, p[:, :bw], acts[:, kc, tt * 128:(tt + 1) * 128], w[:, kc, :bw], kc == 0, kc == K - 1, reads=[w, acts])
                self.cp(sg[:, :bw], p[:, :bw], reads=[p], writes=[sg], eng="vector" if n % 2 == 0 else "scalar")
                self.dma(dst[drow0 + tt * 128:drow0 + (tt + 1) * 128, dcol0 + b0:dcol0 + b0 + bw], sg[:, :bw], reads=[sg], pwrites=[dst])
                n += 1
        self.barrier()
        st.close()

    def phase_qkprep(self, l, sm):
        I = self.inp
        S = sm.S
        ch = min(512, S)
        st = contextlib.ExitStack()
        qkg = self.sb([128, 2], F32, "qkg", st)
        self.dma(qkg[:], I["qkg"][l], reads=[I["qkg"]], writes=[qkg])
        RT = self.sb([128, 128], F32, "RT", st)
        self.dma(RT[:], I["ropeRT"].ap(), reads=[I["ropeRT"]], writes=[RT])
        xs = [self.sb([128, ch], F32, "qx", st) for _ in range(3)]
        sqs = [self.sb([128, ch], F32, "qsq", st) for _ in range(3)]
        tmps = [self.sb([128, ch], F32, "qtmp", st) for _ in range(3)]
        rstds = [self.sb([128, ch], F32, "qrstd", st) for _ in range(3)]
        nn = [self.sb([128, ch], F32, "qn", st) for _ in range(3)]
        t1s = [self.sb([128, ch], F32, "qt1", st) for _ in range(3)]
        t2s = [self.sb([128, ch], F32, "qt2", st) for _ in range(3)]
        ob = [self.sb([128, ch], BF16, "qo", st) for _ in range(3)]
        cs = [self.sb([128, 2, ch], F32, "qcs", st) for _ in range(2)]
        p1 = [self.ps([128, ch], F32, "qp1", st) for _ in range(2)]
        p2 = [self.ps([128, ch], F32, "qp2", st) for _ in range(2)]
        n = 0
        for c in range(S // ch):
            if sm.rope:
                cst = cs[c % 2]
                self.dma(cst[:, 0, :], I["ropeC"][:, c * ch:(c + 1) * ch], reads=[], writes=[cst])
                self.dma(cst[:, 1, :], I["ropeS"][:, c * ch:(c + 1) * ch], reads=[], pwrites=[cst])
            for hh in range(10):
                isq = hh < 8
                src = sm.d["qTraw"] if isq else sm.d["kTraw"]
                r0 = hh * 128 if isq else (hh - 8) * 128
                x = xs[n % 3]
                nb = nn[n % 3]
                o = ob[n % 3]
                sq, tmp, rstd, t1, t2 = sqs[n % 3], tmps[n % 3], rstds[n % 3], t1s[n % 3], t2s[n % 3]
                pa = p1[n % 2]
                pb = p2[n % 2]
                self.dma(x[:], src[r0:r0 + 128, c * ch:(c + 1) * ch], reads=[src], writes=[x])
                self.act(sq[:], x[:], AF.Square, reads=[x], writes=[sq])
                self.mm(pa, pa[:], self.onesF[:], sq[:], True, True, reads=[sq, self.onesF])
                self.rstd_from_ss(rstd[:], pa[:], 128.0, tmp[:], [pa], tmp, rstd)
                g = qkg[:, 0:1] if isq else qkg[:, 1:2]
                self.stt(nb[:], x[:], g, rstd[:], ALU.mult, ALU.mult, reads=[x, qkg, rstd], writes=[nb])
                if sm.rope:
                    self.mm(pb, pb[:], RT[:], nb[:], True, True, reads=[RT, nb])
                    self.tt(t1[:], nb[:], cst[:, 0, :], ALU.mult, reads=[nb, cst], writes=[t1], eng="gpsimd")
                    self.tt(t2[:], pb[:], cst[:, 1, :], ALU.mult, reads=[pb, cst], writes=[t2])
                    self.tt(o[:], t1[:], t2[:], ALU.add, reads=[t1, t2], writes=[o])
                else:
                    self.cp(o[:], nb[:], reads=[nb], writes=[o])
                if isq:
                    self.dma(sm.d["qrT"][r0:r0 + 128, c * ch:(c + 1) * ch], o[:], reads=[o], pwrites=[sm.d["qrT"]])
                else:
                    k0 = sm.koff + c * ch
                    self.dma(self.krT[r0:r0 + 128, k0:k0 + ch], o[:], reads=[o], pwrites=[self.krT])
                n += 1
        self.barrier()
        st.close()

    def phase_attn(self, sm):
        S = sm.S
        ch = min(512, S)
        k0, k1 = sm.keys
        nk = k1 - k0
        nkt = nk // 128
        st = contextlib.ExitStack()
        KT = self.sb([128, nk], BF16, "KT", st)
        V = self.sb([128, nkt, 128], BF16, "V", st)
        Qc = [self.sb([128, ch], BF16, "Qc", st) for _ in range(2)]
        pT = [self.sb([128, ch], BF16, "pT", st) for _ in range(3)]
        rden = self.sb([128, ch], F32, "rden", st)
        yo = [self.sb([128, ch], BF16, "yo", st) for _ in range(2)]
        ps_s = [self.ps([128, ch], F32, "ps_s", st) for _ in range(3)]
        ps_o = [self.ps([128, ch], F32, "ps_o", st) for _ in range(2)]
        ps_d = [self.ps([128, ch], F32, "ps_d", st) for _ in range(2)]
        scale = 128.0 ** -0.5
        nq = 0
        ns = 0
        for kv in range(2):
            self.dma(KT[:], self.krT[kv * 128:(kv + 1) * 128, k0:k1], reads=[self.krT], writes=[KT])
            self.dma(V[:], self.av[k0:k1, kv * 128:(kv + 1) * 128].rearrange("(t p) d -> p t d", p=128), reads=[self.av], writes=[V])
            for g in range(4):
                h = kv * 4 + g
                for c in range(S // ch):
                    q = Qc[nq % 2]
                    po = ps_o[nq % 2]
                    pd = ps_d[nq % 2]
                    y = yo[nq % 2]
                    self.dma(q[:], sm.d["qrT"][h * 128:(h + 1) * 128, c * ch:(c + 1) * ch], reads=[sm.d["qrT"]], writes=[q])
                    prev = None
                    for kt in range(nkt + 1):
                        cur_pt = None
                        if kt < nkt:
                            psx = ps_s[ns % 3]
                            cur_pt = pT[ns % 3]
                            ns += 1
                            self.mm(psx, psx[:], KT[:, kt * 128:(kt + 1) * 128], q[:], True, True, reads=[KT, q])
                            self.act(cur_pt[:], psx[:], AF.Exp, reads=[psx], writes=[cur_pt], scale=scale)
                        if prev is not None:
                            pk, ppt = prev
                            self.mm(po, po[:], V[:, pk, :], ppt[:], pk == 0, pk == nkt - 1, reads=[V, ppt])
                            self.mm(pd, pd[:], self.onesB[:], ppt[:], pk == 0, pk == nkt - 1, reads=[self.onesB, ppt])
                        prev = (kt, cur_pt) if kt < nkt else None
                    self.op("vector", lambda e: e.reciprocal(out=rden[:], in_=pd[:]), reads=[pd], writes=[rden])
                    self.tt(y[:], po[:], rden[:], ALU.mult, reads=[po, rden], writes=[y])
                    self.dma(sm.d["yattT"][h * 128:(h + 1) * 128, c * ch:(c + 1) * ch], y[:], reads=[y], pwrites=[sm.d["yattT"]])
                    nq += 1
        self.barrier()
        st.close()

    def phase_gla(self, l, sm):
        I = self.inp
        S = sm.S
        ch = min(512, S)
        nch = S // 64
        ntile = S // 128
        st = contextlib.ExitStack()
        d = sm.d
        X = self.sb([64, S], F32, "gX", st)
        L = self.sb([64, S], F32, "gL", st)
        CUM = self.sb([64, S], F32, "gCUM", st)
        E = self.sb([64, S], F32, "gE", st)
        maskR = self.sb([64, S], F32, "gmaskR", st)
        ktf = self.sb([64, S], BF16, "gktf", st)
        self.op("gpsimd", lambda e: e.memset(maskR[:], 1.0), writes=[maskR])
        self.op("gpsimd", lambda e: e.memset(maskR[:].rearrange("p (c j) -> p c j", j=64)[:, :, 0:1], 0.0), reads=[maskR], writes=[maskR])
        qd = [self.sb([64, S], BF16, "gqd", st) for _ in range(2)]
        ki = [self.sb([64, S], BF16, "gki", st) for _ in range(2)]
        kteT = [self.sb([128, ntile, 64], BF16, "gkteT", st) for _ in range(2)]
        dec = self.sb([64, nch], F32, "gdec", st)
        Sbf = [self.sb([64, nch, 128], BF16, "gSbf", st) for _ in range(2)]
        Sst = [self.sb([64, 128], F32, "gSst", st) for _ in range(2)]
        Vh = self.sb([128, ntile, 128], BF16, "gVh", st)
        wa2 = self.sb([16, 2, 256], F32, "gwa2", st)
        nba = self.sb([64, 2, 4], F32, "gnba", st)
        gg = self.sb([128, 1], F32, "ggn", st)
        mk = self.sb([128, 2, 128], F32, "gmk", st)
        gat = [self.sb([16, ch], F32, "ggat", st) for _ in range(2)]
        am = [self.sb([128, 128], BF16, "gam", st) for _ in range(4)]
        og = [self.sb([128, ch], BF16, "gog", st) for _ in range(2)]
        sg = self.sb([128, ch], F32, "gsg", st)
        osq = self.sb([128, ch], F32, "gosq", st)
        tmp = self.sb([128, ch], F32, "gtmp", st)
        rstd = self.sb([128, ch], F32, "grstd", st)
        t1 = self.sb([128, ch], F32, "gt1", st)
        yb = [self.sb([128, ch], BF16, "gyb", st) for _ in range(2)]
        ptr = self.ps([128, 512], BF16, "gptr", st)
        pcs = [self.ps([64, 4, 128], F32, "gpcs", st) for _ in range(2)]
        pa = [self.ps([128, 128], F32, "gpa", st) for _ in range(2)]
        po = self.ps([128, ch], F32, "gpo", st)
        pss = self.ps([128, ch], F32, "gpss", st)
        pz = [pss, po]
        for dd in range(2):
            self.dma(wa2[:, dd, :], I["gla_wa2"][l, dd], reads=[], pwrites=[wa2])
        self.dma(nba[:], I["gla_baT"][l], reads=[], writes=[nba])
        self.ts(nba[:], nba[:], -1.0, None, ALU.mult, None, reads=[nba], writes=[nba])
        self.dma(gg[:], I["glag"][l], reads=[], writes=[gg])
        self.dma(mk[:], I["gla_mask"].ap(), reads=[], writes=[mk])
        nz = 0
        import os
        cut = int(os.environ.get("GLA_CUT", "99"))

        def bail():
            self.barrier()
            st.close()
        for h in range(4):
            self.dma(Vh[:], d["gv"][:, h * 128:(h + 1) * 128].rearrange("(t p) v -> p t v", p=128), reads=[d["gv"]], writes=[Vh])
            for dd in range(2):
                for c in range(S // ch):
                    ga = gat[nz % 2]
                    p = pz[nz % 2]
                    nz += 1
                    self.dma(ga[:], d["gaT"][dd * 16:(dd + 1) * 16, c * ch:(c + 1) * ch], reads=[d["gaT"]], writes=[ga])
                    self.mm(p, p[0:64, :], wa2[:, dd, h * 64:(h + 1) * 64], ga[:], True, True, reads=[wa2, ga])
                    self.act(E[:, c * ch:(c + 1) * ch], p[0:64, :], AF.Exp, reads=[p, nba], pwrites=[E], scale=-1.0, bias=nba[:, dd, h:h + 1])
                    self.act(L[:, c * ch:(c + 1) * ch], E[:, c * ch:(c + 1) * ch], AF.Ln, reads=[E], pwrites=[L], bias=1.0)
                if cut == 2:
                    return bail()
                self.op("vector", lambda e: e.tensor_tensor_scan(out=CUM[:], data0=maskR[:], data1=L[:], initial=0.0, op0=ALU.mult, op1=ALU.add),
                        reads=[maskR, L], writes=[CUM])
                C3 = CUM[:].rearrange("p (c j) -> p c j", j=64)
                END = C3[:, :, 63:64]
                ENDb = END.to_broadcast([64, nch, 64])
                if dd == 0:
                    ARG = CUM
                else:
                    self.tt(L[:], L[:], CUM[:], ALU.subtract, reads=[L, CUM], writes=[L])
                    L3 = L[:].rearrange("p (c j) -> p c j", j=64)
                    self.tt(L3, L3, ENDb, ALU.add, reads=[L, CUM], writes=[L])
                    ARG = L
                A3 = ARG[:].rearrange("p (c j) -> p c j", j=64)
                self.dma(X[:], d["gqT"][h * 64:(h + 1) * 64, :], reads=[d["gqT"]], writes=[X])
                self.act(E[:], ARG[:], AF.Exp, reads=[ARG], writes=[E], scale=-1.0 / 16)
                self.stt(qd[dd][:], X[:], 0.125, E[:], ALU.mult, ALU.mult, reads=[X, E], writes=[qd[dd]])
                self.dma(X[:], d["gkT"][h * 64:(h + 1) * 64, :], reads=[d["gkT"]], writes=[X])
                self.act(E[:], ARG[:], AF.Exp, reads=[ARG], writes=[E], scale=1.0 / 16)
                self.tt(ki[dd][:], X[:], E[:], ALU.mult, reads=[X, E], writes=[ki[dd]])
                E3 = E[:].rearrange("p (c j) -> p c j", j=64)
                self.tt(E3, ENDb, A3, ALU.subtract, reads=[CUM, ARG], writes=[E])
                self.act(E[:], E[:], AF.Exp, reads=[E], writes=[E], scale=-1.0 / 16)
                self.tt(ktf[:], X[:], E[:], ALU.mult, reads=[X, E], writes=[ktf])
                if cut == 3:
                    return bail()
                for t0 in range(0, ntile, 8):
                    nt = min(8, ntile - t0)
                    for j in range(nt):
                        tt_ = t0 + j
                        self.tr(ptr, ptr[:, j * 64:(j + 1) * 64], ktf[:, tt_ * 128:(tt_ + 1) * 128], self.identB[0:64, 0:64], j == 0, reads=[ktf, self.identB])
                    self.cp(kteT[dd][:, t0:t0 + nt, :], ptr[:, 0:nt * 64].rearrange("p (t k) -> p t k", k=64), reads=[ptr],
                            writes=[kteT[dd]] if t0 == 0 else [], pwrites=[] if t0 == 0 else [kteT[dd]])
                if cut == 4:
                    return bail()
                self.act(dec[:].unsqueeze(2), END, AF.Exp, reads=[CUM], writes=[dec], scale=-1.0 / 16)
                cur = 0
                self.cp(Sst[0][:], sm.gin[:, h, dd, :], reads=[sm.gin], writes=[Sst[0]])
                order = list(range(nch)) if dd == 0 else list(range(nch - 1, -1, -1))
                for i0 in range(0, nch, 4):
                    grp = order[i0:i0 + 4]
                    gi = (i0 // 4) % 2
                    cnt = [0, 0]
                    slots = {}
                    for c in grp:
                        par = c % 2
                        slot = gi * 2 + cnt[par]
                        cnt[par] += 1
                        slots[c] = (pcs[par], slot)
                        tt_, pb = c // 2, par * 64
                        self.mm(pcs[par], pcs[par][:, slot, :], kteT[dd][pb:pb + 64, tt_, :], Vh[pb:pb + 64, tt_, :], True, True, reads=[kteT[dd], Vh])
                    for c in grp:
                        pc, slot = slots[c]
                        self.cp(Sbf[dd][:, c, :], Sst[cur][:], reads=[Sst[cur]], pwrites=[Sbf[dd]], eng="scalar")
                        self.stt(Sst[1 - cur][:], Sst[cur][:], dec[:, c:c + 1], pc[:, slot, :], ALU.mult, ALU.add,
                                 reads=[Sst[cur], dec, pc], writes=[Sst[1 - cur]])
                        cur = 1 - cur
                if sm.gout is not None:
                    self.cp(sm.gout[:, h, dd, :], Sst[cur][:], reads=[Sst[cur]], pwrites=[sm.gout])
            if cut == 5:
                return bail()
            npair = 0
            for c in range(S // ch):
                ntp = ch // 128
                for j in range(ntp):
                    tp = c * ntp + j
                    ams = []
                    for dd in range(2):
                        a = am[(npair % 2) * 2 + dd]
                        p = pa[dd]
                        self.mm(p, p[:], ki[dd][:, tp * 128:(tp + 1) * 128], qd[dd][:, tp * 128:(tp + 1) * 128], True, True, reads=[ki[dd], qd[dd]])
                        self.tt(a[:], p[:], mk[:, dd, :], ALU.mult, reads=[p, mk], writes=[a])
                        ams.append(a)
                    npair += 1
                    for cc in range(2):
                        cidx = tp * 2 + cc
                        reg = po[:, j * 128 + cc * 64:j * 128 + cc * 64 + 64]
                        for dd in range(2):
                            self.mm(po, reg, Vh[:, tp, :], ams[dd][:, cc * 64:(cc + 1) * 64], dd == 0, False, reads=[Vh, ams[dd]])
                            self.mm(po, reg, Sbf[dd][:, cidx, :], qd[dd][:, cidx * 64:(cidx + 1) * 64], False, dd == 1, reads=[Sbf[dd], qd[dd]])
                ogt = og[c % 2]
                y = yb[c % 2]
                self.dma(ogt[:], d["ogT"][h * 128:(h + 1) * 128, c * ch:(c + 1) * ch], reads=[d["ogT"]], writes=[ogt])
                self.act(sg[:], ogt[:], AF.Silu, reads=[ogt], writes=[sg])
                self.act(osq[:], po[:], AF.Square, reads=[po], writes=[osq])
                self.mm(pss, pss[:], self.onesF[:], osq[:], True, True, reads=[self.onesF, osq])
                self.rstd_from_ss(rstd[:], pss[:], 128.0, tmp[:], [pss], tmp, rstd)
                self.tt(t1[:], po[:], rstd[:], ALU.mult, reads=[po, rstd], writes=[t1])
                self.stt(y[:], t1[:], gg[:, 0:1], sg[:], ALU.mult, ALU.mult, reads=[t1, gg, sg], writes=[y])
                self.dma(d["yglaT"][h * 128:(h + 1) * 128, c * ch:(c + 1) * ch], y[:], reads=[y], pwrites=[d["yglaT"]])
        self.barrier()
        st.close()

    def phase_hyena(self, l, sm):
        I = self.inp
        S = sm.S
        n = S
        ntile = n // 128
        nkt = ntile
        d = sm.d
        sfx = "L" if n == SEQ else "C"
        TC, TS = I["dftC" + sfx], I["dftS" + sfx]
        N2 = 2 * n
        st = contextlib.ExitStack()
        ch = min(512, n)
        zemb = self.sb([33, n], F32, "hzemb", st)
        self.dma(zemb[:], I["zemb" + sfx].ap(), reads=[], writes=[zemb])
        w1 = self.sb([33, 64], F32, "hw1", st)
        w2 = self.sb([64, 64], F32, "hw2", st)
        w3 = self.sb([64, 2048], F32, "hw3", st)
        pv = self.sb([64, 4], F32, "hpv", st)
        self.dma(w1[:], I["hy_pos_w1"][l], reads=[], writes=[w1])
        self.dma(w2[:], I["hy_pos_w2"][l], reads=[], writes=[w2])
        self.dma(w3[:], I["hy_pos_w3"][l], reads=[], writes=[w3])
        self.dma(pv[:], I["hy_pv"][l], reads=[], writes=[pv])
        fb = self.sb([64, 2], F32, "hfb", st)
        self.tt(fb[:, 0:1], pv[:, 0:1], pv[:, 1:2], ALU.mult, reads=[pv], writes=[fb])
        self.tt(fb[:, 1:2], pv[:, 2:3], pv[:, 1:2], ALU.mult, reads=[pv], pwrites=[fb])
        hid = [self.sb([64, n], F32, "hhid", st) for _ in range(2)]
        u = self.sb([64, ch], F32, "hu", st)
        kf = self.sb([64, ch], F32, "hkf", st)
        kint = self.sb([64, ch], I32, "hki", st)
        php = [self.ps([64, ch], F32, "hph", st) for _ in range(2)]
        TWO_PI = 2.0 * math.pi
        for layer_i in range(2):
            wsb = w1 if layer_i == 0 else w2
            src = zemb if layer_i == 0 else hid[0]
            K = 33 if layer_i == 0 else 64
            for c in range(n // ch):
                p = php[c % 2]
                self.mm(p, p[:], wsb[0:K, :], src[0:K, c * ch:(c + 1) * ch], True, True, reads=[wsb, src])
                self.ts(u[:], p[:], pv[:, 1:2], fb[:, layer_i:layer_i + 1], ALU.mult, ALU.add, reads=[p, pv, fb], writes=[u])
                self.ts(kf[:], u[:], 1.0 / TWO_PI, None, ALU.mult, None, reads=[u], writes=[kf])
                self.cp(kint[:], kf[:], reads=[kf], writes=[kint])
                self.cp(kf[:], kint[:], reads=[kint], writes=[kf])
                self.stt(u[:], kf[:], -TWO_PI, u[:], ALU.mult, ALU.add, reads=[kf, u], writes=[u])
                self.ts(kf[:], u[:], math.pi, -TWO_PI, ALU.is_gt, ALU.mult, reads=[u], writes=[kf])
                self.tt(u[:], u[:], kf[:], ALU.add, reads=[u, kf], writes=[u])
                self.ts(kf[:], u[:], -math.pi, TWO_PI, ALU.is_lt, ALU.mult, reads=[u], writes=[kf])
                self.tt(u[:], u[:], kf[:], ALU.add, reads=[u, kf], writes=[u])
                self.ts(u[:], u[:], math.pi, -math.pi, ALU.min, ALU.max, reads=[u], writes=[u])
                self.act(hid[layer_i][:, c * ch:(c + 1) * ch], u[:], AF.Sin, reads=[u], pwrites=[hid[layer_i]])
        hid2 = hid[1]
        absd = self.sb([128, 2048], F32, "habsd", st)
        self.dma(absd[:], I["hy_decay"][l].partition_broadcast(128), reads=[], writes=[absd])
        self.act(absd[:], absd[:], AF.Abs, reads=[absd], writes=[absd])
        negtn = self.sb([128, ntile], F32, "hnegtn", st)
        self.dma(negtn[:], I["negtn" + sfx].ap(), reads=[], writes=[negtn])
        win = self.sb([128, 2048], F32, "hwin", st)
        taps = self.sb([128, 2048], F32, "htaps", st)
        atap = self.sb([128, 2048], F32, "hatap", st)
        fs_t = [self.sb([128, 2, 512], BF16, "hfst", st) for _ in range(2)]
        fd_t = [self.sb([128, 2, 512], BF16, "hfdt", st) for _ in range(2)]
        pf = [self.ps([128, 512], F32, "hpf", st) for _ in range(2)]
        pl1 = [self.ps([128, 512], F32, "hpl1", st) for _ in range(4)]
        for tt_ in range(ntile):
            self.act(win[:], absd[:], AF.Exp, reads=[absd, negtn], writes=[win], scale=negtn[:, tt_:tt_ + 1])
            for b in range(4):
                p = pf[b % 2]
                self.mm(p, p[:], hid2[:, tt_ * 128:(tt_ + 1) * 128], w3[:, b * 512:(b + 1) * 512], True, True, reads=[hid2, w3])
                self.tt(taps[:, b * 512:(b + 1) * 512], p[:], win[:, b * 512:(b + 1) * 512], ALU.mult, reads=[p, win],
                        writes=[taps] if b == 0 else [], pwrites=[] if b == 0 else [taps])
            if tt_ == 0:
                t4 = taps[0:1, :].rearrange("p (o d c) -> p o d c", o=2, d=2)
                self.op("vector", lambda e: e.memset(t4[:, :, 1, :], 0.0), reads=[taps], pwrites=[taps])
            self.act(atap[:], taps[:], AF.Abs, reads=[taps], writes=[atap])
            for b in range(4):
                self.mm(pl1[b], pl1[b][:], self.onesF[:], atap[:, b * 512:(b + 1) * 512], tt_ == 0, tt_ == ntile - 1, reads=[self.onesF, atap])
            T4 = taps[:].rearrange("p (o d c) -> p o d c", o=2, d=2)
            fs_ = fs_t[tt_ % 2]
            fd_ = fd_t[tt_ % 2]
            self.tt(fs_[:], T4[:, :, 0, :], T4[:, :, 1, :], ALU.add, reads=[taps], writes=[fs_])
            self.tt(fd_[:], T4[:, :, 0, :], T4[:, :, 1, :], ALU.subtract, reads=[taps], writes=[fd_], eng="gpsimd")
            self.dma(d["fsd"][tt_ * 128:(tt_ + 1) * 128, :], fs_[:].rearrange("p o c -> p (o c)"), reads=[fs_], pwrites=[d["fsd"]])
            self.dma(d["fdd"][tt_ * 128:(tt_ + 1) * 128, :], fd_[:].rearrange("p o c -> p (o c)"), reads=[fd_], pwrites=[d["fdd"]])
        linv = self.linv
        l1 = self.sb([128, 2048], F32, "hl1", st)
        for b in range(4):
            self.cp(l1[:, b * 512:(b + 1) * 512], pl1[b][:], reads=[pl1[b]], writes=[l1] if b == 0 else [], pwrites=[] if b == 0 else [l1])
        L4 = l1[:].rearrange("p (o d c) -> p o d c", o=2, d=2)
        self.tt(linv[:], L4[:, :, 0, :], L4[:, :, 1, :], ALU.add, reads=[l1], writes=[linv])
        self.ts(linv[:], linv[:], EPS, None, ALU.add, None, reads=[linv], writes=[linv])
        self.op("vector", lambda e: e.reciprocal(out=linv[:], in_=linv[:]), reads=[linv], writes=[linv])
        self.barrier()
        st.close()

        st = contextlib.ExitStack()
        wk = self.sb([128, nkt], F32, "hwk", st)
        self.dma(wk[:], I["wk" + sfx].ap(), reads=[], writes=[wk])
        alt = self.sb([128, 2], BF16, "halt", st)
        self.dma(alt[:], I["altcol"].ap(), reads=[], writes=[alt])
        altrow = self.sb([1, 128], BF16, "haltrow", st)
        self.dma(altrow[:], I["altrow"].ap(), reads=[], writes=[altrow])
        tcb = [self.sb([128, ntile, 128], BF16, "htc", st) for _ in range(2)]
        tsb = [self.sb([128, ntile, 128], BF16, "hts", st) for _ in range(2)]
        fsb = self.sb([128, ntile, 512], BF16, "hfsb", st)
        fdb = self.sb([128, ntile, 512], BF16, "hfdb", st)
        pre = [self.ps([128, 512], F32, "hpre", st) for _ in range(2)]
        pim = [self.ps([128, 512], F32, "hpim", st) for _ in range(2)]
        pny = self.ps([1, 512], F32, "hpny", st)
        fo = [self.sb([128, 2, 512], F32, "hfo", st) for _ in range(2)]
        fn = self.sb([1, 512], F32, "hfn", st)
        cw = self.sb([128, 4, 1536], F32, "hcw", st)
        for j in range(3):
            self.dma(cw[:, j, :], I["hy_conv_w"][l, j].partition_broadcast(128), reads=[], writes=[cw] if j == 0 else [], pwrites=[] if j == 0 else [cw])
        self.dma(cw[:, 3, :], I["hy_conv_b"][l].partition_broadcast(128), reads=[], pwrites=[cw])
        us = [self.sb([128, 3, 1536], BF16, "hus", st) for _ in range(2)]
        a1s = [self.sb([128, 1536], F32, "ha1", st) for _ in range(2)]
        a2s = [self.sb([128, 1536], F32, "ha2", st) for _ in range(2)]
        a3s = [self.sb([128, 1536], F32, "ha3", st) for _ in range(2)]
        ucb = [self.sb([128, 1536], BF16, "hucb", st) for _ in range(2)]

        def sc_step(tt_):
            u_ = us[tt_ % 2]
            uo = ucb[tt_ % 2]
            for j in range(3):
                self.dma(u_[:, j, :], d["hyu"][tt_ * 128 + j:tt_ * 128 + j + 128, :], reads=[d["hyu"]], writes=[u_] if j == 0 else [], pwrites=[] if j == 0 else [u_])
            a1, a2, a3 = a1s[tt_ % 2], a2s[tt_ % 2], a3s[tt_ % 2]
            self.tt(a1[:], u_[:, 0, :], cw[:, 0, :], ALU.mult, reads=[u_, cw], writes=[a1])
            self.tt(a2[:], u_[:, 1, :], cw[:, 1, :], ALU.mult, reads=[u_, cw], writes=[a2], eng="gpsimd")
            self.tt(a3[:], u_[:, 2, :], cw[:, 2, :], ALU.mult, reads=[u_, cw], writes=[a3], eng="gpsimd")
            self.tt(a1[:], a1[:], cw[:, 3, :], ALU.add, reads=[a1, cw], writes=[a1])
            self.tt(a2[:], a2[:], a3[:], ALU.add, reads=[a2, a3], writes=[a2], eng="gpsimd")
            self.tt(uo[:], a1[:], a2[:], ALU.add, reads=[a1, a2], writes=[uo])
            self.dma(d["ucd"][tt_ * 128:(tt_ + 1) * 128, :], uo[:], reads=[uo], pwrites=[d["ucd"]])
        sc_todo = list(range(ntile))
        sc_every = max(1, (2 * nkt) // ntile)
        sc_iter = 0
        nld = 0
        for o in range(2):
            self.dma(fsb[:], d["fsd"][:, o * 512:(o + 1) * 512].rearrange("(t p) c -> p t c", p=128), reads=[d["fsd"]], writes=[fsb])
            self.dma(fdb[:], d["fdd"][:, o * 512:(o + 1) * 512].rearrange("(t p) c -> p t c", p=128), reads=[d["fdd"]], writes=[fdb])
            for kt in range(nkt):
                sc_iter += 1
                if sc_todo and sc_iter % sc_every == 0:
                    sc_step(sc_todo.pop(0))
                tc_, ts_ = tcb[nld % 2], tsb[nld % 2]
                pr, pi = pre[nld % 2], pim[nld % 2]
                fo_ = fo[nld % 2]
                nld += 1
                self.dma(tc_[:], TC[kt], reads=[], writes=[tc_])
                self.dma(ts_[:], TS[kt], reads=[], writes=[ts_])
                for tt_ in range(ntile):
                    self.mm(pr, pr[:], tc_[:, tt_, :], fsb[:, tt_, :], tt_ == 0, tt_ == ntile - 1, reads=[tc_, fsb])
                for tt_ in range(ntile):
                    self.mm(pi, pi[:], ts_[:, tt_, :], fdb[:, tt_, :], tt_ == 0, tt_ == ntile - 1, reads=[ts_, fdb])
                self.stt(fo_[:, 0, :], pr[:], wk[:, kt:kt + 1], linv[:, o, :], ALU.mult, ALU.mult, reads=[pr, wk, linv], writes=[fo_])
                self.stt(fo_[:, 1, :], pi[:], wk[:, kt:kt + 1], linv[:, o, :], ALU.mult, ALU.mult, reads=[pi, wk, linv], pwrites=[fo_])
                self.dma(d["Fd"][o, :, kt * 128:(kt + 1) * 128, :].rearrange("r p c -> p r c"), fo_[:], reads=[fo_], pwrites=[d["Fd"]])
            for tt_ in range(ntile):
                self.mm(pny, pny[:], alt[:, 0:1], fsb[:, tt_, :], tt_ == 0, tt_ == ntile - 1, reads=[alt, fsb])
            self.stt(fn[:], pny[:], 1.0 / N2, linv[0:1, o, :], ALU.mult, ALU.mult, reads=[pny, linv], writes=[fn])
            self.dma(d["Fnyq"][o:o + 1, :], fn[:], reads=[fn], pwrites=[d["Fnyq"]])
        while sc_todo:
            sc_step(sc_todo.pop(0))
        self.barrier()
        st.close()

        st = contextlib.ExitStack()
        alt = self.sb([128, 2], BF16, "halt", st)
        self.dma(alt[:], I["altcol"].ap(), reads=[], writes=[alt])
        altrow = self.sb([1, 128], BF16, "haltrow", st)
        self.dma(altrow[:], I["altrow"].ap(), reads=[], writes=[altrow])
        skb = self.sb([128, 2, 512], F32, "hskb", st)
        self.dma(skb[:].rearrange("p o c -> p (o c)"), I["hy_skip"][l].partition_broadcast(128), reads=[], writes=[skb])
        zsb = self.sb([128, ntile, 512], BF16, "hzsb", st)
        Yre = self.sb([128, nkt, 512], BF16, "hYre", st)
        Ys = self.sb([128, nkt, 512], BF16, "hYs", st)
        Yn = self.sb([1, 512], BF16, "hYn", st)
        fn = self.sb([1, 512], F32, "hfn2", st)
        tcb = [self.sb([128, ntile, 128], BF16, "htc2", st) for _ in range(2)]
        tsb = [self.sb([128, ntile, 128], BF16, "hts2", st) for _ in range(2)]
        Ft = [self.sb([128, 2, 512], F32, "hFt", st) for _ in range(2)]
        m = [self.sb([128, 512], F32, "hm", st) for _ in range(4)]
        gt = [self.sb([128, 512], BF16, "hgt", st) for _ in range(2)]
        zo = [self.sb([128, 512], BF16, "hzo", st) for _ in range(2)]
        pre = [self.ps([128, 512], F32, "cpre", st) for _ in range(2)]
        pim = [self.ps([128, 512], F32, "cpim", st) for _ in range(2)]
        pny = self.ps([1, 512], F32, "cpny", st)
        py = [self.ps([128, 512], F32, "cpy", st) for _ in range(2)]
        ptr = self.ps([128, 512], BF16, "cptr", st)
        ytr = [self.sb([128, 4, 128], BF16, "hytr", st) for _ in range(2)]
        nld = 0
        for o in range(2):
            zsrc = d["ucd"][:, 1024:1536] if o == 0 else d["z2d"][:, :]
            zdep = d["ucd"] if o == 0 else d["z2d"]
            self.dma(zsb[:], zsrc.rearrange("(t p) c -> p t c", p=128), reads=[zdep], writes=[zsb])
            self.dma(fn[:], d["Fnyq"][o:o + 1, :], reads=[d["Fnyq"]], writes=[fn])
            for kt in range(nkt):
                tc_, ts_ = tcb[nld % 2], tsb[nld % 2]
                pr, pi = pre[nld % 2], pim[nld % 2]
                F_ = Ft[nld % 2]
                nld += 1
                self.dma(tc_[:], TC[kt], reads=[], writes=[tc_])
                self.dma(ts_[:], TS[kt], reads=[], writes=[ts_])
                self.dma(F_[:], d["Fd"][o, :, kt * 128:(kt + 1) * 128, :].rearrange("r p c -> p r c"), reads=[d["Fd"]], writes=[F_])
                for tt_ in range(ntile):
                    self.mm(pr, pr[:], tc_[:, tt_, :], zsb[:, tt_, :], tt_ == 0, tt_ == ntile - 1, reads=[tc_, zsb])
                for tt_ in range(ntile):
                    self.mm(pi, pi[:], ts_[:, tt_, :], zsb[:, tt_, :], tt_ == 0, tt_ == ntile - 1, reads=[ts_, zsb])
                self.tt(m[0][:], pr[:], F_[:, 0, :], ALU.mult, reads=[pr, F_], writes=[m[0]])
                self.tt(m[1][:], pi[:], F_[:, 1, :], ALU.mult, reads=[pi, F_], writes=[m[1]])
                self.tt(Yre[:, kt, :], m[0][:], m[1][:], ALU.subtract, reads=[m[0], m[1]], pwrites=[Yre], eng="gpsimd")
                self.tt(m[2][:], pr[:], F_[:, 1, :], ALU.mult, reads=[pr, F_], writes=[m[2]])
                self.tt(m[3][:], pi[:], F_[:, 0, :], ALU.mult, reads=[pi, F_], writes=[m[3]])
                self.tt(Ys[:, kt, :], m[2][:], m[3][:], ALU.add, reads=[m[2], m[3]], pwrites=[Ys], eng="gpsimd")
            for tt_ in range(ntile):
                self.mm(pny, pny[:], alt[:, 0:1], zsb[:, tt_, :], tt_ == 0, tt_ == ntile - 1, reads=[alt, zsb])
            self.tt(Yn[:], pny[:], fn[:], ALU.mult, reads=[pny, fn], writes=[Yn])
            for tt_ in range(ntile):
                tc_, ts_ = tcb[nld % 2], tsb[nld % 2]
                p = py[nld % 2]
                g_ = gt[nld % 2]
                z_ = zo[nld % 2]
                nld += 1
                self.dma(tc_[:], TC[tt_], reads=[], writes=[tc_])
                self.dma(ts_[:], TS[tt_], reads=[], writes=[ts_])
                self.dma(g_[:], d["ucd"][tt_ * 128:(tt_ + 1) * 128, o * 512:(o + 1) * 512], reads=[d["ucd"]], writes=[g_])
                for kt in range(nkt):
                    self.mm(p, p[:], tc_[:, kt, :], Yre[:, kt, :], kt == 0, False, reads=[tc_, Yre])
                    self.mm(p, p[:], ts_[:, kt, :], Ys[:, kt, :], False, False, reads=[ts_, Ys])
                self.mm(p, p[:], altrow[:, :], Yn[:], False, True, reads=[altrow, Yn])
                self.tt(m[0][:], zsb[:, tt_, :], skb[:, o, :], ALU.mult, reads=[zsb, skb], writes=[m[0]], eng="gpsimd")
                self.tt(m[1][:], p[:], m[0][:], ALU.add, reads=[p, m[0]], writes=[m[1]])
                self.tt(z_[:], m[1][:], g_[:], ALU.mult, reads=[m[1], g_], writes=[z_])
                if o == 0:
                    self.dma(d["z2d"][tt_ * 128:(tt_ + 1) * 128, :], z_[:], reads=[z_], pwrites=[d["z2d"]])
                else:
                    yt = ytr[tt_ % 2]
                    for j in range(4):
                        self.tr(ptr, ptr[:, j * 128:(j + 1) * 128], z_[:, j * 128:(j + 1) * 128], self.identB[:], j == 0, reads=[z_, self.identB])
                    self.cp(yt[:], ptr[:].rearrange("p (j t) -> p j t", j=4), reads=[ptr], writes=[yt], eng="scalar")
                    self.dma(d["yhyT"][:, tt_ * 128:(tt_ + 1) * 128].rearrange("(j p) t -> p j t", p=128), yt[:], reads=[yt], pwrites=[d["yhyT"]])
            self.barrier()
        self.barrier()
        st.close()

    def phase_merge(self, l, sm, xin, xout):
        I = self.inp
        S = sm.S
        ch = min(512, S)
        d = sm.d
        col = sm.col
        st = contextlib.ExitStack()
        wbh = self.sb([128, 4, 1024], BF16, "mwbh", st)
        wbg = self.sb([128, 4, 1024], BF16, "mwbg", st)
        wba = self.sb([128, 8, 1024], BF16, "mwba", st)
        wo = self.sb([128, 8, 1024], BF16, "mwo", st)
        self.wstage(st)
        for wt, nm, kk in ((wbh, "w_br_hy", 4), (wbg, "w_br_gla", 4), (wba, "w_br_att", 8), (wo, "w_out", 8)):
            self.wload(wt, I[nm][l], kk, 1024)
        yh = [self.sb([128, 4, ch], BF16, "myh", st) for _ in range(2)]
        yg = [self.sb([128, 4, ch], BF16, "myg", st) for _ in range(2)]
        ya = [self.sb([128, 8, ch], BF16, "mya", st) for _ in range(2)]
        mT = self.sb([128, 8, ch], BF16, "mmT", st)
        brg = [self.sb([128, 3, ch], BF16, "mbrg", st) for _ in range(2)]
        sigs = [self.sb([128, 3, ch], F32, "msig", st) for _ in range(2)]
        as_ = [self.sb([128, ch], F32, "ma", st) for _ in range(2)]
        bs_ = [self.sb([128, ch], F32, "mb", st) for _ in range(2)]
        cs_ = [self.sb([128, ch], F32, "mc", st) for _ in range(2)]
        xc = [self.sb([128, ch], F32, "mxc", st) for _ in range(2)]
        xn = [self.sb([128, ch], F32, "mxn", st) for _ in range(2)]
        p1 = self.ps([128, ch], F32, "mp1", st)
        p2 = self.ps([128, ch], F32, "mp2", st)
        p3 = self.ps([128, ch], F32, "mp3", st)
        py = [self.ps([128, ch], F32, "mpy", st) for _ in range(2)]
        brv = d["brT"]
        n = 0
        for c in range(S // ch):
            cs = slice(c * ch, (c + 1) * ch)
            yh_, yg_, ya_ = yh[c % 2], yg[c % 2], ya[c % 2]
            self.dma(yh_[:], d["yhyT"][:, cs].rearrange("(k p) t -> p k t", p=128), reads=[d["yhyT"]], writes=[yh_])
            self.dma(yg_[:], d["yglaT"][:, cs].rearrange("(k p) t -> p k t", p=128), reads=[d["yglaT"]], writes=[yg_])
            self.dma(ya_[:], d["yattT"][:, cs].rearrange("(k p) t -> p k t", p=128), reads=[d["yattT"]], writes=[ya_])
            for fc in range(8):
                fs_ = slice(fc * 128, (fc + 1) * 128)
                bg = brg[n % 2]
                n += 1
                self.dma(bg[:], brv[:, cs].rearrange("(j k p) t -> p k j t", j=3, k=8, p=128)[:, fc], reads=[brv], writes=[bg])
                for k in range(4):
                    self.mm(p1, p1[:], wbh[:, k, fs_], yh_[:, k, :], k == 0, k == 3, reads=[wbh, yh_])
                for k in range(4):
                    self.mm(p2, p2[:], wbg[:, k, fs_], yg_[:, k, :], k == 0, k == 3, reads=[wbg, yg_])
                for k in range(8):
                    self.mm(p3, p3[:], wba[:, k, fs_], ya_[:, k, :], k == 0, k == 7, reads=[wba, ya_])
                sig, a, b, c_ = sigs[fc % 2], as_[fc % 2], bs_[fc % 2], cs_[fc % 2]
                self.act(sig[:], bg[:], AF.Sigmoid, reads=[bg], writes=[sig])
                self.tt(a[:], p1[:], sig[:, 0, :], ALU.mult, reads=[p1, sig], writes=[a])
                self.tt(b[:], p2[:], sig[:, 1, :], ALU.mult, reads=[p2, sig], writes=[b])
                self.tt(c_[:], p3[:], sig[:, 2, :], ALU.mult, reads=[p3, sig], writes=[c_])
                self.tt(a[:], a[:], b[:], ALU.add, reads=[a, b], writes=[a], eng="gpsimd")
                self.tt(mT[:, fc, :], a[:], c_[:], ALU.add, reads=[a, c_], writes=[mT] if fc == 0 else [], pwrites=[] if fc == 0 else [mT], eng="gpsimd")
            for fc in range(8):
                fs_ = slice(fc * 128, (fc + 1) * 128)
                p = py[fc % 2]
                x_ = xc[fc % 2]
                xo = xn[fc % 2]
                self.dma(x_[:], xin[fs_, cs], reads=[xin], writes=[x_])
                for k in range(8):
                    self.mm(p, p[:], wo[:, k, fs_], mT[:, k, :], k == 0, k == 7, reads=[wo, mT])
                self.stt(xo[:], p[:], self.modT[:, 16 + fc, col:col + 1], x_[:], ALU.mult, ALU.add, reads=[p, self.modT, x_], writes=[xo])
                self.dma(xout[fs_, cs], xo[:], reads=[xo], pwrites=[xout])
        self.barrier()
        st.close()

    def phase_moe(self, l, sm, xin, xout):
        I = self.inp
        S = sm.S
        col = sm.col
        half = min(2048, S)
        ch = min(512, half)
        ntt = half // 128
        for hb in range(S // half):
            hs = slice(hb * half, (hb + 1) * half)
            st = contextlib.ExitStack()
            h2T = self.sb([128, 8, half], BF16, "eh2T", st)
            lgT = self.sb([128, ntt, 16], F32, "elg", st)
            G = self.sb([128, ntt, 16], F32, "eG", st)
            acc = self.sb([128, 8, half], F32, "eacc", st)
            self.dma(acc[:], xin[:, hs].rearrange("(k p) t -> p k t", p=128), reads=[xin], writes=[acc])
            st2 = contextlib.ExitStack()
            self.phase_norm(_Slice(xin, hs), half, 1, col, h2T, st2, lgT=lgT, chmax=256)
            self.barrier()
            st2.close()
            st2 = contextlib.ExitStack()
            rb = self.sb([128, 16], F32, "erb", st2)
            self.dma(rb[:], I["router_b"].ap().partition_broadcast(128), reads=[], writes=[rb])
            sc = self.sb([128, ntt, 16], F32, "esc", st2)
            sv = self.sb([128, ntt, 16], F32, "esv", st2)
            t = self.sb([128, ntt, 16], F32, "et", st2)
            t2 = self.sb([128, ntt, 16], F32, "et2", st2)
            i1 = self.sb([128, ntt, 16], F32, "ei1", st2)
            i2 = self.sb([128, ntt, 16], F32, "ei2", st2)
            p6 = self.sb([128, ntt * 4, 6], F32, "ep6", st2)
            gs = self.sb([128, ntt, 4], F32, "egs", st2)
            gm = self.sb([128, ntt], F32, "egm", st2)
            ing = self.sb([128, ntt, 4], F32, "eing", st2)
            self.act(sc[:], lgT[:], AF.Sigmoid, reads=[lgT], writes=[sc])
            self.tt(sv[:], sc[:], rb[:].unsqueeze(1).to_broadcast([128, ntt, 16]), ALU.add, reads=[sc, rb], writes=[sv])
            s4 = sv[:].rearrange("p t (g e) -> p (t g) e", e=4)
            self.tt(p6[:, :, 0:3], s4[:, :, 0:3], s4[:, :, 1:4], ALU.add, reads=[sv], writes=[p6])
            self.tt(p6[:, :, 3:5], s4[:, :, 0:2], s4[:, :, 2:4], ALU.add, reads=[sv], pwrites=[p6])
            self.tt(p6[:, :, 5:6], s4[:, :, 0:1], s4[:, :, 3:4], ALU.add, reads=[sv], pwrites=[p6])
            self.op("vector", lambda e: e.tensor_reduce(out=gs[:].rearrange("p t g -> p (t g)"), in_=p6[:], axis=AX.X, op=ALU.max), reads=[p6], writes=[gs])
            self.op("vector", lambda e: e.tensor_reduce(out=gm[:], in_=gs[:], axis=AX.X, op=ALU.max), reads=[gs], writes=[gm])
            self.tt(ing[:], gs[:], gm[:].unsqueeze(2).to_broadcast([128, ntt, 4]), ALU.is_equal, reads=[gs, gm], writes=[ing])
            self.ts(t[:], sv[:], 2.0, None, ALU.add, None, reads=[sv], writes=[t])
            t4 = t[:].rearrange("p t (g e) -> p t g e", e=4)
            self.tt(t4, t4, ing[:].unsqueeze(3).to_broadcast([128, ntt, 4, 4]), ALU.mult, reads=[t, ing], writes=[t])
            self.ts(t[:], t[:], -2.0, None, ALU.add, None, reads=[t], writes=[t])
            self.op("vector", lambda e: e.tensor_reduce(out=gm[:], in_=t[:], axis=AX.X, op=ALU.max), reads=[t], writes=[gm])
            self.tt(i1[:], t[:], gm[:].unsqueeze(2).to_broadcast([128, ntt, 16]), ALU.is_equal, reads=[t, gm], writes=[i1])
            self.stt(t2[:], i1[:], -4.0, t[:], ALU.mult, ALU.add, reads=[i1, t], writes=[t2])
            self.op("vector", lambda e: e.tensor_reduce(out=gm[:], in_=t2[:], axis=AX.X, op=ALU.max), reads=[t2], writes=[gm])
            self.tt(i2[:], t2[:], gm[:].unsqueeze(2).to_broadcast([128, ntt, 16]), ALU.is_equal, reads=[t2, gm], writes=[i2])
            self.tt(i1[:], i1[:], i2[:], ALU.add, reads=[i1, i2], writes=[i1])
            self.tt(t[:], sc[:], i1[:], ALU.mult, reads=[sc, i1], writes=[t])
            self.op("vector", lambda e: e.tensor_reduce(out=gm[:], in_=t[:], axis=AX.X, op=ALU.add), reads=[t], writes=[gm])
            self.op("vector", lambda e: e.reciprocal(out=gm[:], in_=gm[:]), reads=[gm], writes=[gm])
            self.tt(G[:], t[:], gm[:].unsqueeze(2).to_broadcast([128, ntt, 16]), ALU.mult, reads=[t, gm], writes=[G])
            if self.dbg:
                self.dma(sm.d["gates"][hb * ntt * 128:(hb + 1) * ntt * 128, :].rearrange("(t p) e -> p t e", p=128), G[:], reads=[G], pwrites=[sm.d["gates"]])
            self.barrier()
            st2.close()
            self.wstage(st)
            Gh = self.sb([128, ntt, 16], BF16, "eGh", st)
            Gl = self.sb([128, ntt, 16], BF16, "eGl", st)
            Gt = self.sb([128, ntt, 16], F32, "eGt", st)
            self.cp(Gh[:], G[:], reads=[G], writes=[Gh])
            self.cp(Gt[:], Gh[:], reads=[Gh], writes=[Gt])
            self.tt(Gt[:], G[:], Gt[:], ALU.subtract, reads=[G, Gt], writes=[Gt])
            self.cp(Gl[:], Gt[:], reads=[Gt], writes=[Gl])
            Gxh = self.sb([128, ntt, 128], BF16, "eGxh", st)
            Gxl = self.sb([128, ntt, 128], BF16, "eGxl", st)
            wg = [self.sb([128, 8, 512], BF16, "ewg", st) for _ in range(2)]
            wu = [self.sb([128, 8, 512], BF16, "ewu", st) for _ in range(2)]
            wd = [self.sb([128, 4, 1024], BF16, "ewd", st) for _ in range(2)]
            gb = [self.sb([128, ch], F32, "egb", st) for _ in range(2)]
            sgl = [self.sb([128, ch], F32, "esgl", st) for _ in range(2)]
            tm = [self.sb([128, ch], F32, "etm", st) for _ in range(2)]
            hid = [self.sb([128, 4, ch], BF16, "ehid", st) for _ in range(2)]
            pgb = self.ps([128, ch], F32, "epgb", st)
            pg = [self.ps([128, ch], F32, "epg", st) for _ in range(2)]
            pu = [self.ps([128, ch], F32, "epu", st) for _ in range(2)]
            pd = [self.ps([128, ch], F32, "epd", st) for _ in range(2)]
            nn_ = 0
            nd = 0
            def esteps(ei):
                pcs_ = []
                for dstT, nm, K_, nc_ in ((wg[ei % 2], "moe_w_gate", 8, 512), (wu[ei % 2], "moe_w_up", 8, 512), (wd[ei % 2], "moe_w_down", 4, 1024)):
                    ksub = max(1, 2048 // nc_)
                    for k0 in range(0, K_, ksub):
                        pcs_.append((dstT, I[nm][l, ei], k0, min(ksub, K_ - k0), nc_))
                views = {}

                def do_dma(i):
                    dstT, src, k0, kn, nc_ = pcs_[i]
                    stg = self._wst[i % 2]
                    sv = stg[:, 0:kn * nc_].rearrange("p (k n) -> p k n", k=kn)
                    views[i] = (stg, sv)
                    self.dma(sv, src[k0 * 128:(k0 + kn) * 128, :].rearrange("(k p) n -> p k n", p=128), reads=[], writes=[stg])

                def do_cast(i):
                    dstT, src, k0, kn, nc_ = pcs_[i]
                    stg, sv = views[i]
                    self.cp(dstT[:, k0:k0 + kn, :nc_], sv, reads=[stg], pwrites=[dstT], eng="scalar")
                steps = []
                n_ = len(pcs_)
                for i in range(n_ + 2):
                    def st_(i=i):
                        if i >= 2:
                            do_cast(i - 2)
                        if i < n_:
                            do_dma(i)
                    steps.append(st_)
                return steps
            for f_ in esteps(0):
                f_()
            nslots = (half // ch) * 4
            for e_ in range(NEXP):
                g_, u_, d_ = wg[e_ % 2], wu[e_ % 2], wd[e_ % 2]
                nxt = esteps(e_ + 1) if e_ + 1 < NEXP else []
                per_slot = -(-len(nxt) // nslots) if nxt else 0
                self.cp(Gxh[:], Gh[:, :, e_:e_ + 1].to_broadcast([128, ntt, 128]), reads=[Gh], writes=[Gxh])
                self.cp(Gxl[:], Gl[:, :, e_:e_ + 1].to_broadcast([128, ntt, 128]), reads=[Gl], writes=[Gxl], eng="gpsimd")
                for c in range(half // ch):
                    cs = slice(c * ch, (c + 1) * ch)
                    gb_ = gb[c % 2]
                    hd = hid[c % 2]
                    for j in range(ch // 128):
                        self.mm(pgb, pgb[:, j * 128:(j + 1) * 128], Gxh[:, c * (ch // 128) + j, :], self.identB[:], True, False, reads=[Gxh, self.identB])
                        self.mm(pgb, pgb[:, j * 128:(j + 1) * 128], Gxl[:, c * (ch // 128) + j, :], self.identB[:], False, True, reads=[Gxl, self.identB])
                    self.cp(gb_[:], pgb[:], reads=[pgb], writes=[gb_], eng="scalar")
                    for dc in range(4):
                        for _ in range(per_slot):
                            if nxt:
                                nxt.pop(0)()
                        ds_ = slice(dc * 128, (dc + 1) * 128)
                        a_, b_ = pg[nn_ % 2], pu[nn_ % 2]
                        s_, t_ = sgl[nn_ % 2], tm[nn_ % 2]
                        nn_ += 1
                        for k in range(8):
                            self.mm(a_, a_[:], g_[:, k, ds_], h2T[:, k, cs], k == 0, k == 7, reads=[g_, h2T])
                        for k in range(8):
                            self.mm(b_, b_[:], u_[:, k, ds_], h2T[:, k, cs], k == 0, k == 7, reads=[u_, h2T])
                        self.act(s_[:], a_[:], AF.Silu, reads=[a_], writes=[s_])
                        self.tt(t_[:], b_[:], s_[:], ALU.mult, reads=[b_, s_], writes=[t_])
                        self.tt(hd[:, dc, :], t_[:], gb_[:], ALU.mult, reads=[t_, gb_], writes=[hd] if dc == 0 else [], pwrites=[] if dc == 0 else [hd], eng="gpsimd")
                    for fc in range(8):
                        p_ = pd[nd % 2]
                        nd += 1
                        for dc in range(4):
                            self.mm(p_, p_[:], d_[:, dc, fc * 128:(fc + 1) * 128], hd[:, dc, :], dc == 0, dc == 3, reads=[d_, hd])
                        self.stt(acc[:, fc, cs], p_[:], self.modT[:, 40 + fc, col:col + 1], acc[:, fc, cs], ALU.mult, ALU.add,
                                 reads=[p_, self.modT, acc], pwrites=[acc])
                while nxt:
                    nxt.pop(0)()
            self.dma(xout[:, hs].rearrange("(k p) t -> p k t", p=128), acc[:], reads=[acc], pwrites=[xout])
            self.barrier()
            st.close()

    def phase_xpose_out(self, xT, out, S):
        st = contextlib.ExitStack()
        xs = [self.sb([128, 8, 128], F32, "oxs", st) for _ in range(2)]
        stg = [self.sb([128, 1024], F32, "ostg", st) for _ in range(2)]
        pt = [self.ps([128, 512], F32, "opt", st) for _ in range(4)]
        for tt in range(S // 128):
            x = xs[tt % 2]
            sg = stg[tt % 2]
            self.dma(x[:], xT[:, tt * 128:(tt + 1) * 128].rearrange("(k p) t -> p k t", p=128), reads=[xT], writes=[x])
            for half in range(2):
                p = pt[(tt * 2 + half) % 4]
                for k in range(4):
                    self.tr(p, p[:, k * 128:(k + 1) * 128], x[:, half * 4 + k, :], self.identF[:], k == 0, reads=[x, self.identF])
                self.cp(sg[:, half * 512:(half + 1) * 512], p[:], reads=[p], writes=[sg] if half == 0 else [], pwrites=[] if half == 0 else [sg],
                        eng="vector" if half == 0 else "scalar")
            self.dma(out[tt * 128:(tt + 1) * 128, :], sg[:], reads=[sg], pwrites=[out])
        self.barrier()
        st.close()


class _Slice:
    def __init__(self, t, cols):
        self.t = t
        self.buf = t.buf
        self.cols = cols

    def __getitem__(self, idx):
        r, c = idx
        base = self.cols.start
        c2 = slice(base + (c.start or 0), base + c.stop)
        return self.t[r, c2]


class Stream:
    pass


def build_program(dbg=False, stop_after=None, layers=(0, 1)):
    nc = bass.Bass("TRN2", target_bir_lowering=False)
    P = MK(nc, dbg=dbg)
    din = P.din
    din("x", [SEQ, D]); din("ctx", [CTX, D]); din("cT", [128, 8, 2])
    din("w_mod", [2, D, 6 * D]); din("b_modT", [2, 128, 48]); din("g1T", [2, 128, 8]); din("g2T", [2, 128, 8])
    din("w_in", [2, D, NIN]); din("qkg", [2, 128, 2])
    din("gla_wa2", [2, 2, 16, 256]); din("gla_baT", [2, 64, 2, 4]); din("glag", [2, 128, 1]); din("gla_mask", [128, 2, 128])
    din("ropeC", [128, SEQ]); din("ropeS", [128, SEQ]); din("ropeRT", [128, 128])
    din("hy_pos_w1", [2, 33, 64]); din("hy_pos_w2", [2, 64, 64]); din("hy_pos_w3", [2, 64, 2048]); din("hy_pv", [2, 64, 4])
    din("hy_decay", [2, 2048]); din("hy_skip", [2, 1024]); din("hy_conv_w", [2, 3, 1536]); din("hy_conv_b", [2, 1536])
    din("zembL", [33, SEQ]); din("zembC", [33, CTX]); din("negtnL", [128, 32]); din("negtnC", [128, 2])
    din("wkL", [128, 32]); din("wkC", [128, 2])
    din("dftCL", [32, 128, 32, 128], BF16); din("dftSL", [32, 128, 32, 128], BF16)
    din("dftCC", [2, 128, 2, 128], BF16); din("dftSC", [2, 128, 2, 128], BF16)
    din("altcol", [128, 2], BF16); din("altrow", [1, 128], BF16)
    din("w_br_hy", [2, 512, D]); din("w_br_gla", [2, 512, D]); din("w_br_att", [2, D, D]); din("w_out", [2, D, D])
    din("router_w", [D, 16]); din("router_b", [16]); din("moe_sel", [16, 16, 128])
    din("moe_w_gate", [2, 16, D, DE]); din("moe_w_up", [2, 16, D, DE]); din("moe_w_down", [2, 16, DE, D])
    out = P.dram("out", [SEQ, D], F32, kind="ExternalOutput")

    P.setup_consts()
    P.modT = P.sb([128, 48, 2], F32, "modT")
    P.AA = P.sb([128, 2, 8, 2], F32, "AA")
    P.linv = P.sb([128, 2, 512], F32, "linv")
    g0 = P.sb([64, 4, 2, 128], F32, "gstate0")
    gc = P.sb([64, 4, 2, 128], F32, "gstatec")
    P.op("vector", lambda e: e.memset(g0[:], 0.0), writes=[g0])
    P.krT = P.dscr("krT", [256, SEQ + CTX], BF16)
    P.av = P.dscr("av", [SEQ + CTX, 256], BF16)
    streams = []
    for nm, S, col in (("c", CTX, 1), ("l", SEQ, 0)):
        sm = Stream()
        sm.S, sm.col, sm.nm = S, col, nm
        sm.rope = nm == "l"
        sm.koff = 0 if nm == "l" else SEQ
        sm.keys = (0, SEQ + CTX) if nm == "l" else (SEQ, SEQ + CTX)
        sm.gin = gc if nm == "l" else g0
        sm.gout = None if nm == "l" else gc
        dd = {}
        for k, shp, dt in (("xTa", [D, S], F32), ("xTb", [D, S], F32), ("kTraw", [256, S], F32), ("qTraw", [1024, S], F32),
                           ("gkT", [256, S], F32), ("gqT", [256, S], F32), ("gaT", [32, S], F32), ("ogT", [512, S], BF16),
                           ("brT", [3072, S], BF16), ("gv", [S, 512], BF16), ("hyu", [S + 2, 1536], BF16), ("qrT", [1024, S], BF16),
                           ("yattT", [1024, S], BF16), ("yglaT", [512, S], BF16), ("yhyT", [512, S], BF16),
                           ("fsd", [S, 1024], BF16), ("fdd", [S, 1024], BF16), ("Fd", [2, 2, S, 512], F32), ("Fnyq", [2, 512], F32),
                           ("ucd", [S, 1536], BF16), ("z2d", [S, 512], BF16), ("gates", [S, 16], F32)):
            dd[k] = P.dscr(f"{nm}_{k}", shp, dt)
        sm.d = dd
        streams.append(sm)
    ctxs, lat = streams

    def done(tag):
        return stop_after == tag

    def finish():
        P.finish()
        return nc, P

    P.phase_xpose_in(P.inp["ctx"], ctxs.d["xTa"], CTX)
    P.phase_xpose_in(P.inp["x"], lat.d["xTa"], SEQ)
    if done("xpose"):
        return finish()
    for l in layers:
        last = l == DEPTH - 1
        P.phase_mods(l)
        for sm in (ctxs, lat):
            S = sm.S
            d = sm.d
            st = contextlib.ExitStack()
            hT = P.sb([128, 8, S], BF16, "hT", st)
            st2 = contextlib.ExitStack()
            P.phase_norm(d["xTa"], S, 0, sm.col, hT, st2)
            P.barrier()
            st2.close()
            if P.dbg and l == layers[0]:
                hdbg = P.dscr(f"{sm.nm}_hT", [D, S], BF16)
                P.dma(hdbg.ap().rearrange("(k p) t -> p k t", p=128), hT[:], reads=[hT], writes=[hdbg])
            win = P.inp["w_in"][l]
            zr = P.sb([1, 1536], BF16, "zr", st)
            P.op("vector", lambda e: e.memset(zr[:], 0.0), writes=[zr])
            P.dma(d["hyu"][0:1, :], zr[:], reads=[zr], pwrites=[d["hyu"]])
            P.dma(d["hyu"][S + 1:S + 2, :], zr[:], reads=[zr], pwrites=[d["hyu"]])
            for (c0, ncol, key, dt) in ((0, 256, "kTraw", F32), (512, 256, "gkT", F32), (1280, 32, "gaT", F32), (1312, 1024, "qTraw", F32),
                                        (2336, 256, "gqT", F32), (2592, 512, "ogT", BF16), (4640, 3072, "brT", BF16)):
                P.linear_fm(win, 8, c0, ncol, hT, S, d[key], 0, dt)
            P.linear_tm(win, 8, 256, 256, hT, S, P.av, sm.koff, 0, BF16)
            P.linear_tm(win, 8, 768, 512, hT, S, d["gv"], 0, 0, BF16)
            P.linear_tm(win, 8, 3104, 1536, hT, S, d["hyu"], 1, 0, BF16)
            P.barrier()
            st.close()
            if done(sm.nm + ":proj"):
                return finish()
            P.phase_qkprep(l, sm)
            if done(sm.nm + ":qkprep"):
                return finish()
            P.phase_gla(l, sm)
            if done(sm.nm + ":gla"):
                return finish()
            if last and sm is ctxs:
                continue
            P.phase_attn(sm)
            if done(sm.nm + ":attn"):
                return finish()
            P.phase_hyena(l, sm)
            if done(sm.nm + ":hyena"):
                return finish()
            P.phase_merge(l, sm, d["xTa"], d["xTb"])
            if done(sm.nm + ":merge"):
                return finish()
            P.phase_moe(l, sm, d["xTb"], d["xTa"])
            if done(sm.nm + ":moe"):
                return finish()
    P.phase_xpose_out(lat.d["xTa"], out, SEQ)
    return finish()


def host_consts():
    c = {}
    f32 = np.float32
    bf = ml_dtypes.bfloat16
    t = np.arange(SEQ)
    row = (t // 64).astype(f32)
    colv = (t % 64).astype(f32)
    inv = (np.float32(10000.0) ** (-np.arange(32, dtype=f32) / np.float32(32))).astype(f32)
    ang = np.concatenate([row[:, None] * inv[None, :], colv[:, None] * inv[None, :]], axis=-1).astype(f32)
    pidx = np.arange(128) // 2
    c["ropeC"] = np.ascontiguousarray(np.cos(ang)[:, pidx].T.astype(f32))
    c["ropeS"] = np.ascontiguousarray(np.sin(ang)[:, pidx].T.astype(f32))
    RT = np.zeros((128, 128), f32)
    for i in range(64):
        RT[2 * i + 1, 2 * i] = -1.0
        RT[2 * i, 2 * i + 1] = 1.0
    c["ropeRT"] = RT
    s = np.arange(128)[:, None]
    q = np.arange(128)[None, :]
    same = (s // 64) == (q // 64)
    mk = np.zeros((128, 2, 128), f32)
    mk[:, 0, :] = (same & (s <= q)).astype(f32)
    mk[:, 1, :] = (same & (s >= q)).astype(f32)
    c["gla_mask"] = mk
    for sfx, n in (("L", SEQ), ("C", CTX)):
        tt = np.arange(n, dtype=f32)
        tn = (tt / np.float32(n)).astype(f32)
        bands = np.linspace(1e-4, 15, 16, dtype=f32)
        phase = (np.float32(2 * math.pi / n) * tt[:, None] * bands[None, :]).astype(f32)
        z = np.concatenate([tn[:, None], np.cos(phase), -np.sin(phase)], axis=-1).astype(f32)
        c["zemb" + sfx] = np.ascontiguousarray(z.T)
        nt = n // 128
        c["negtn" + sfx] = np.ascontiguousarray((-tn).reshape(nt, 128).T)
        N2 = 2 * n
        wk = np.full(n, 2.0 / N2, f32)
        wk[0] = 1.0 / N2
        c["wk" + sfx] = np.ascontiguousarray(wk.reshape(nt, 128).T)
        idx = np.arange(n, dtype=np.int64)
        prod = (idx[:, None] * idx[None, :]) % N2
        angd = prod.astype(np.float64) * (2 * math.pi / N2)
        for nm, fn in (("dftC", np.cos), ("dftS", np.sin)):
            M = fn(angd).astype(f32)
            T4 = M.reshape(nt, 128, nt, 128).transpose(2, 1, 0, 3)
            c[nm + sfx] = np.ascontiguousarray(T4).astype(bf)
    alt = np.where(np.arange(128) % 2 == 0, 1.0, -1.0).astype(f32)
    c["altcol"] = np.stack([alt, alt], axis=1).astype(bf)
    c["altrow"] = alt[None, :].astype(bf)
    sel = np.zeros((16, 16, 128), f32)
    for e in range(16):
        sel[e, e, :] = 1.0
    c["moe_sel"] = sel
    return c


_CONSTS = None


def host_inputs(inp):
    global _CONSTS
    if _CONSTS is None:
        _CONSTS = host_consts()
    f32 = np.float32
    g = {k: np.asarray(v) for k, v in inp.items()}
    sh = dict(_CONSTS)
    for k in ("w_mod", "w_in", "gla_wa2", "hy_pos_w1", "hy_pos_w2", "hy_pos_w3", "hy_conv_w", "hy_conv_b", "w_br_hy", "w_br_gla",
              "w_br_att", "w_out", "router_w", "router_b", "moe_w_gate", "moe_w_up", "moe_w_down"):
        sh[k] = np.ascontiguousarray(g[k], dtype=f32)
    sh["b_modT"] = np.ascontiguousarray(g["b_mod"].reshape(2, 48, 128).transpose(0, 2, 1))
    sh["g1T"] = np.ascontiguousarray(g["norm1_g"].reshape(2, 8, 128).transpose(0, 2, 1))
    sh["g2T"] = np.ascontiguousarray(g["norm2_g"].reshape(2, 8, 128).transpose(0, 2, 1))
    sh["qkg"] = np.ascontiguousarray(np.stack([g["q_norm_g"], g["k_norm_g"]], axis=-1))
    sh["gla_baT"] = np.ascontiguousarray(g["gla_ba"].reshape(2, 2, 4, 64).transpose(0, 3, 1, 2))
    sh["glag"] = np.ascontiguousarray(g["gla_norm_g"].reshape(2, 128, 1))
    pv = np.zeros((2, 64, 4), f32)
    pv[:, :, 0] = g["hy_pos_b1"]
    pv[:, :, 1] = g["hy_sin_freq"]
    pv[:, :, 2] = g["hy_pos_b2"]
    sh["hy_pv"] = pv
    sh["hy_decay"] = np.ascontiguousarray(g["hy_decay"].reshape(2, 2048))
    sh["hy_skip"] = np.ascontiguousarray(g["hy_skip"].reshape(2, 1024))
    return sh, g


def core_inputs(sh, g, b):
    m = dict(sh)
    m["x"] = np.ascontiguousarray(g["x"][b], dtype=np.float32)
    m["ctx"] = np.ascontiguousarray(g["ctx"][b], dtype=np.float32)
    cT = np.stack([g["c"][b].reshape(8, 128).T, g["c_ctx"].reshape(8, 128).T], axis=-1)
    m["cT"] = np.ascontiguousarray(cT, dtype=np.float32)
    return m


def kernel(**inputs):
    sh, g = host_inputs(inputs)
    nc, P = build_program()
    in_maps = [core_inputs(sh, g, b) for b in range(8)]
    res = run_bass_kernel_spmd(nc, in_maps, core_ids=list(range(8)))
    return np.stack([np.asarray(r["out"]) for r in res.results], axis=0).astype(np.float32)
```
